# Optimizing a Trainium2 kernel written in Bass

```python
import jax, jax.numpy as jnp
from jax import lax
import numpy as np

D_MODEL = 2048
BATCH = 2
SEQ = 16384
DEPTH = 2

HEAD_DIM = 128
ROPE_THETA = 10000.0
EPS = 1e-6
Q_BLOCK = 128
D_FF = 4 * D_MODEL
NEG_INF = -1e30
POS_INF = 1e30

NSA_HEADS = 8
NSA_KV_GROUPS = 2
NSA_CMP_LEN = 32
NSA_CMP_STRIDE = 16
NSA_SEL_LEN = 64
NSA_SEL_TOPK = 16
NSA_WINDOW = 512
NSA_CMP_HIDDEN = HEAD_DIM

MLA_HEADS = 8
MLA_Q_RANK = 512
MLA_KV_RANK = 256
MLA_NOPE = 128
MLA_ROPE = 64
MLA_V = 128

DIL_PATTERNS = ((128, 1), (512, 4), (2048, 16))
DIL_HEADS = 8

A_Q_COLS = NSA_HEADS * HEAD_DIM
A_KV_COLS = 3 * 2 * NSA_KV_GROUPS * HEAD_DIM
A_GATE_COLS = 3 * NSA_HEADS
B_COLS = MLA_Q_RANK + MLA_KV_RANK + MLA_ROPE
EVEN_IN = A_Q_COLS + A_KV_COLS + A_GATE_COLS + B_COLS
EVEN_OUT = NSA_HEADS * HEAD_DIM + MLA_HEADS * MLA_V
ODD_IN = len(DIL_PATTERNS) * 3 * DIL_HEADS * HEAD_DIM
ODD_OUT = DIL_HEADS * HEAD_DIM

kernel_name = "hybrid_nsa_mla_dilated_trunk"


def rms_norm(x, g):
    xf = x.astype(jnp.float32)
    y = xf * lax.rsqrt(jnp.mean(xf * xf, axis=-1, keepdims=True) + EPS)
    return (y * g.astype(jnp.float32)).astype(x.dtype)


def rope_tables(seq, dim):
    inv = 1.0 / (ROPE_THETA ** (jnp.arange(0, dim, 2, dtype=jnp.float32) / dim))
    ang = jnp.arange(seq, dtype=jnp.float32)[:, None] * inv[None, :]
    return jnp.cos(ang), jnp.sin(ang)


def apply_rope(x, cos, sin):
    half = x.shape[-1] // 2
    c = cos[None, :, None, :].astype(x.dtype)
    s = sin[None, :, None, :].astype(x.dtype)
    x1, x2 = x[..., :half], x[..., half:]
    return jnp.concatenate([x1 * c - x2 * s, x1 * s + x2 * c], axis=-1)


def masked_softmax(s, mask):
    return jax.nn.softmax(jnp.where(mask, s, NEG_INF), axis=-1) * mask


def nsa_compress(k, pe, w1, w2):
    B, S, G, d = k.shape
    n_cmp = (S - NSA_CMP_LEN) // NSA_CMP_STRIDE + 1
    idx = jnp.arange(n_cmp)[:, None] * NSA_CMP_STRIDE + jnp.arange(NSA_CMP_LEN)[None, :]
    blocks = k[:, idx] + pe[None, None, :, None, :].astype(k.dtype)
    flat = blocks.transpose(0, 1, 3, 2, 4).reshape(B, n_cmp, G, NSA_CMP_LEN * d)
    return jax.nn.gelu(flat @ w1) @ w2


def nsa_attention(q, k_cmp, v_cmp, k_slc, v_slc, k_win, v_win, gate):
    B, S, H, d = q.shape
    G = k_slc.shape[2]
    Hg = H // G
    n_cmp = k_cmp.shape[1]
    n_slc = S // NSA_SEL_LEN
    top_k = min(NSA_SEL_TOPK, n_slc)
    scale = d ** -0.5
    cmp_last = jnp.arange(n_cmp) * NSA_CMP_STRIDE + (NSA_CMP_LEN - 1)
    ratio = NSA_SEL_LEN // NSA_CMP_STRIDE
    offs = np.arange(1 - NSA_CMP_LEN // NSA_CMP_STRIDE, ratio)
    ov = np.arange(n_slc)[:, None] * ratio + offs[None, :]
    ov_ok = jnp.asarray((ov >= 0) & (ov < n_cmp))
    ov = jnp.asarray(np.clip(ov, 0, n_cmp - 1))
    ks_b = k_slc.reshape(B, n_slc, NSA_SEL_LEN, G, d).transpose(0, 3, 1, 2, 4)
    vs_b = v_slc.reshape(B, n_slc, NSA_SEL_LEN, G, d).transpose(0, 3, 1, 2, 4)
    kw_p = jnp.pad(k_win, ((0, 0), (NSA_WINDOW, 0), (0, 0), (0, 0)))
    vw_p = jnp.pad(v_win, ((0, 0), (NSA_WINDOW, 0), (0, 0), (0, 0)))
    b_ix = jnp.arange(B)[:, None, None, None]
    g_ix = jnp.arange(G)[None, :, None, None]
    blk = jnp.arange(n_slc)
    sel_tok = jnp.arange(NSA_SEL_LEN)
    win_off = jnp.arange(Q_BLOCK + NSA_WINDOW) - NSA_WINDOW

    def one_block(s0):
        t = s0 + jnp.arange(Q_BLOCK)
        qb = lax.dynamic_slice_in_dim(q, s0, Q_BLOCK, 1).reshape(B, Q_BLOCK, G, Hg, d)
        gb = lax.dynamic_slice_in_dim(gate, s0, Q_BLOCK, 1).reshape(B, Q_BLOCK, G, Hg, 3)
        s_c = jnp.einsum('bqghd,bngd->bghqn', qb, k_cmp).astype(jnp.float32) * scale
        p_c = masked_softmax(s_c, cmp_last[None, :] <= t[:, None])
        o_c = jnp.einsum('bghqn,bngd->bqghd', p_c.astype(v_cmp.dtype), v_cmp)
        imp = jnp.sum(jnp.where(ov_ok, p_c[..., ov], 0.0), axis=(2, 5))
        cur = (t // NSA_SEL_LEN)[:, None]
        forced = (blk[None, :] == 0) | (blk[None, :] == cur) | (blk[None, :] == cur - 1)
        causal = blk[None, :] * NSA_SEL_LEN <= t[:, None]
        score = jnp.where(forced, POS_INF, jnp.where(causal, imp, NEG_INF))
        top_val, top_idx = lax.top_k(score, top_k)
        sel_ok = top_val > 0.5 * NEG_INF
        kg = ks_b[b_ix, g_ix, top_idx]
        vg = vs_b[b_ix, g_ix, top_idx]
        s_s = jnp.einsum('bqghd,bgqkld->bghqkl', qb, kg).astype(jnp.float32) * scale
        tok = top_idx[..., None] * NSA_SEL_LEN + sel_tok
        m_s = (tok <= t[None, None, :, None, None]) & sel_ok[..., None]
        p_s = masked_softmax(s_s.reshape(B, G, Hg, Q_BLOCK, -1), m_s.reshape(B, G, 1, Q_BLOCK, -1))
        o_s = jnp.einsum('bghqm,bgqmd->bqghd', p_s.astype(vg.dtype), vg.reshape(B, G, Q_BLOCK, -1, d))
        kw = lax.dynamic_slice_in_dim(kw_p, s0, Q_BLOCK + NSA_WINDOW, 1)
        vw = lax.dynamic_slice_in_dim(vw_p, s0, Q_BLOCK + NSA_WINDOW, 1)
        pos = s0 + win_off
        rel = t[:, None] - pos[None, :]
        m_w = (rel >= 0) & (rel < NSA_WINDOW) & (pos[None, :] >= 0)
        s_w = jnp.einsum('bqghd,bkgd->bghqk', qb, kw).astype(jnp.float32) * scale
        p_w = masked_softmax(s_w, m_w)
        o_w = jnp.einsum('bghqk,bkgd->bqghd', p_w.astype(vw.dtype), vw)
        o = gb[..., 0:1] * o_c + gb[..., 1:2] * o_s + gb[..., 2:3] * o_w
        return o.reshape(B, Q_BLOCK, H, d)

    out = lax.map(one_block, jnp.arange(S // Q_BLOCK) * Q_BLOCK)
    return out.transpose(1, 0, 2, 3, 4).reshape(B, S, H, d)


def mla_attention(c_q, c_kv, k_rope, q_norm, w_uq, kv_norm, w_ukv):
    B, S, _ = c_q.shape
    cos_r, sin_r = rope_tables(S, MLA_ROPE)
    q = (rms_norm(c_q, q_norm) @ w_uq).reshape(B, S, MLA_HEADS, MLA_NOPE + MLA_ROPE)
    q_nope = q[..., :MLA_NOPE]
    q_pe = apply_rope(q[..., MLA_NOPE:], cos_r, sin_r)
    kv = (rms_norm(c_kv, kv_norm) @ w_ukv).reshape(B, S, MLA_HEADS, MLA_NOPE + MLA_V)
    k_nope, v = kv[..., :MLA_NOPE], kv[..., MLA_NOPE:]
    k_pe = apply_rope(k_rope[:, :, None, :], cos_r, sin_r)[:, :, 0]
    scale = (MLA_NOPE + MLA_ROPE) ** -0.5
    key_pos = jnp.arange(S)

    def one_block(s0):
        t = s0 + jnp.arange(Q_BLOCK)
        qn = lax.dynamic_slice_in_dim(q_nope, s0, Q_BLOCK, 1)
        qp = lax.dynamic_slice_in_dim(q_pe, s0, Q_BLOCK, 1)
        s = (jnp.einsum('bqhd,bkhd->bhqk', qn, k_nope)
             + jnp.einsum('bqhd,bkd->bhqk', qp, k_pe)).astype(jnp.float32) * scale
        p = masked_softmax(s, key_pos[None, :] <= t[:, None])
        return jnp.einsum('bhqk,bkhd->bqhd', p.astype(v.dtype), v)

    out = lax.map(one_block, jnp.arange(S // Q_BLOCK) * Q_BLOCK)
    return out.transpose(1, 0, 2, 3, 4).reshape(B, S, MLA_HEADS, MLA_V)


def dilated_attention(qs, ks, vs):
    B, S, H, d = qs[0].shape
    scale = d ** -0.5
    kps = [jnp.pad(k, ((0, 0), (w, 0), (0, 0), (0, 0))) for k, (w, r) in zip(ks, DIL_PATTERNS)]
    vps = [jnp.pad(v, ((0, 0), (w, 0), (0, 0), (0, 0))) for v, (w, r) in zip(vs, DIL_PATTERNS)]

    def one_block(s0):
        t = s0 + jnp.arange(Q_BLOCK)
        outs, lses = [], []
        for (w, r), q, kp, vp in zip(DIL_PATTERNS, qs, kps, vps):
            dist = jnp.arange(w // r + 1) * r
            idx = t[:, None] + w - dist[None, :]
            kg = kp[:, idx]
            vg = vp[:, idx]
            qb = lax.dynamic_slice_in_dim(q, s0, Q_BLOCK, 1)
            s = jnp.einsum('bqhd,bqnhd->bhqn', qb, kg).astype(jnp.float32) * scale
            s = jnp.where((t[:, None] - dist[None, :]) >= 0, s, NEG_INF)
            m = jnp.max(s, axis=-1, keepdims=True)
            e = jnp.exp(s - m)
            den = jnp.sum(e, axis=-1, keepdims=True)
            outs.append(jnp.einsum('bhqn,bqnhd->bqhd', (e / den).astype(vg.dtype), vg))
            lses.append((m + jnp.log(den))[..., 0])
        alpha = jax.nn.softmax(jnp.stack(lses), axis=0).transpose(0, 1, 3, 2)[..., None]
        o = jnp.stack(outs)
        return jnp.sum(alpha.astype(o.dtype) * o, axis=0)

    out = lax.map(one_block, jnp.arange(S // Q_BLOCK) * Q_BLOCK)
    return out.transpose(1, 0, 2, 3, 4).reshape(B, S, H, d)


def even_mixer(h, w_in, cmp_pe_k, cmp_w1_k, cmp_w2_k, cmp_pe_v, cmp_w1_v, cmp_w2_v,
               mla_q_norm, mla_w_uq, mla_kv_norm, mla_w_ukv, w_out):
    B, S, _ = h.shape
    cos, sin = rope_tables(S, HEAD_DIM)
    z = h @ w_in
    o1 = A_Q_COLS
    o2 = o1 + A_KV_COLS
    o3 = o2 + A_GATE_COLS
    o4 = o3 + MLA_Q_RANK
    o5 = o4 + MLA_KV_RANK
    q_a = apply_rope(z[..., :o1].reshape(B, S, NSA_HEADS, HEAD_DIM), cos, sin)
    kv_a = z[..., o1:o2].reshape(B, S, 3, 2, NSA_KV_GROUPS, HEAD_DIM)
    gate = jax.nn.sigmoid(z[..., o2:o3]).reshape(B, S, NSA_HEADS, 3)
    k_cmp = nsa_compress(apply_rope(kv_a[:, :, 0, 0], cos, sin), cmp_pe_k, cmp_w1_k, cmp_w2_k)
    v_cmp = nsa_compress(kv_a[:, :, 0, 1], cmp_pe_v, cmp_w1_v, cmp_w2_v)
    k_slc = apply_rope(kv_a[:, :, 1, 0], cos, sin)
    k_win = apply_rope(kv_a[:, :, 2, 0], cos, sin)
    o_a = nsa_attention(q_a, k_cmp, v_cmp, k_slc, kv_a[:, :, 1, 1], k_win, kv_a[:, :, 2, 1], gate)
    o_b = mla_attention(z[..., o3:o4], z[..., o4:o5], z[..., o5:],
                        mla_q_norm, mla_w_uq, mla_kv_norm, mla_w_ukv)
    o = jnp.concatenate([o_a.reshape(B, S, -1), o_b.reshape(B, S, -1)], axis=-1)
    return o @ w_out


def odd_mixer(h, w_in, w_out):
    B, S, _ = h.shape
    cos, sin = rope_tables(S, HEAD_DIM)
    z = (h @ w_in).reshape(B, S, len(DIL_PATTERNS), 3, DIL_HEADS, HEAD_DIM)
    qs = [apply_rope(z[:, :, g, 0], cos, sin) for g in range(len(DIL_PATTERNS))]
    ks = [apply_rope(z[:, :, g, 1], cos, sin) for g in range(len(DIL_PATTERNS))]
    vs = [z[:, :, g, 2] for g in range(len(DIL_PATTERNS))]
    o = dilated_attention(qs, ks, vs)
    return o.reshape(B, S, ODD_OUT) @ w_out


def sq_relu_mlp(h, w1, w2):
    return jnp.square(jax.nn.relu(h @ w1)) @ w2


def setup_inputs(seed: int = 0) -> dict:
    key = jax.random.key(seed)
    it = iter(list(jax.random.split(key, 40)))

    def dense(shape, fan_in):
        return jax.random.normal(next(it), shape, jnp.float32) * fan_in ** -0.5

    def gain(n):
        return 1.0 + 0.05 * jax.random.normal(next(it), (n,), jnp.float32)

    def small(shape):
        return 0.1 * jax.random.normal(next(it), shape, jnp.float32)

    x = jax.random.normal(next(it), (BATCH, SEQ, D_MODEL), jnp.float32)
    cmp_in = NSA_CMP_LEN * HEAD_DIM
    return {
        "x": x,
        "l0_norm_mix_pre": gain(D_MODEL),
        "l0_w_in": dense((D_MODEL, EVEN_IN), D_MODEL),
        "l0_cmp_pe_k": small((NSA_CMP_LEN, HEAD_DIM)),
        "l0_cmp_w1_k": dense((cmp_in, NSA_CMP_HIDDEN), cmp_in),
        "l0_cmp_w2_k": dense((NSA_CMP_HIDDEN, HEAD_DIM), NSA_CMP_HIDDEN),
        "l0_cmp_pe_v": small((NSA_CMP_LEN, HEAD_DIM)),
        "l0_cmp_w1_v": dense((cmp_in, NSA_CMP_HIDDEN), cmp_in),
        "l0_cmp_w2_v": dense((NSA_CMP_HIDDEN, HEAD_DIM), NSA_CMP_HIDDEN),
        "l0_mla_q_norm": gain(MLA_Q_RANK),
        "l0_mla_w_uq": dense((MLA_Q_RANK, MLA_HEADS * (MLA_NOPE + MLA_ROPE)), MLA_Q_RANK),
        "l0_mla_kv_norm": gain(MLA_KV_RANK),
        "l0_mla_w_ukv": dense((MLA_KV_RANK, MLA_HEADS * (MLA_NOPE + MLA_V)), MLA_KV_RANK),
        "l0_w_out": dense((EVEN_OUT, D_MODEL), EVEN_OUT),
        "l0_norm_mix_post": gain(D_MODEL),
        "l0_norm_ffn_pre": gain(D_MODEL),
        "l0_w_ff1": dense((D_MODEL, D_FF), D_MODEL),
        "l0_w_ff2": dense((D_FF, D_MODEL), D_FF),
        "l0_norm_ffn_post": gain(D_MODEL),
        "l1_norm_mix_pre": gain(D_MODEL),
        "l1_w_in": dense((D_MODEL, ODD_IN), D_MODEL),
        "l1_w_out": dense((ODD_OUT, D_MODEL), ODD_OUT),
        "l1_norm_mix_post": gain(D_MODEL),
        "l1_norm_ffn_pre": gain(D_MODEL),
        "l1_w_ff1": dense((D_MODEL, D_FF), D_MODEL),
        "l1_w_ff2": dense((D_FF, D_MODEL), D_FF),
        "l1_norm_ffn_post": gain(D_MODEL),
    }


def reference(x, l0_norm_mix_pre, l0_w_in, l0_cmp_pe_k, l0_cmp_w1_k, l0_cmp_w2_k,
              l0_cmp_pe_v, l0_cmp_w1_v, l0_cmp_w2_v, l0_mla_q_norm, l0_mla_w_uq,
              l0_mla_kv_norm, l0_mla_w_ukv, l0_w_out, l0_norm_mix_post, l0_norm_ffn_pre,
              l0_w_ff1, l0_w_ff2, l0_norm_ffn_post, l1_norm_mix_pre, l1_w_in, l1_w_out,
              l1_norm_mix_post, l1_norm_ffn_pre, l1_w_ff1, l1_w_ff2, l1_norm_ffn_post):
    even_mix = (l0_w_in, l0_cmp_pe_k, l0_cmp_w1_k, l0_cmp_w2_k, l0_cmp_pe_v, l0_cmp_w1_v,
                l0_cmp_w2_v, l0_mla_q_norm, l0_mla_w_uq, l0_mla_kv_norm, l0_mla_w_ukv, l0_w_out)
    odd_mix = (l1_w_in, l1_w_out)
    norms = ((l0_norm_mix_pre, l0_norm_mix_post, l0_norm_ffn_pre, l0_norm_ffn_post),
             (l1_norm_mix_pre, l1_norm_mix_post, l1_norm_ffn_pre, l1_norm_ffn_post))
    ffns = ((l0_w_ff1, l0_w_ff2), (l1_w_ff1, l1_w_ff2))
    h = x
    for layer in range(DEPTH):
        g_mix_pre, g_mix_post, g_ffn_pre, g_ffn_post = norms[layer]
        u = rms_norm(h, g_mix_pre)
        if layer % 2 == 0:
            m = even_mixer(u, *even_mix)
        else:
            m = odd_mixer(u, *odd_mix)
        h = h + rms_norm(m, g_mix_post)
        f = sq_relu_mlp(rms_norm(h, g_ffn_pre), *ffns[layer])
        h = h + rms_norm(f, g_ffn_post)
    return h
```

```python
import contextlib
import numpy as np
import concourse.bass as bass
import concourse.mybir as mybir
from concourse.bass_utils import run_bass_kernel_spmd

F32 = mybir.dt.float32
BF16 = mybir.dt.bfloat16
I32 = mybir.dt.int32
AF = mybir.ActivationFunctionType
ALU = mybir.AluOpType
AX = mybir.AxisListType

NDMA_SEM = 6


class Prog:
    def __init__(self, nc, stack):
        self.nc = nc
        self.stack = stack
        self.names = ['pe', 'act', 'dve', 'pool', 'sp']
        self.ops = {k: [] for k in self.names}
        self.cnt = {k: 0 for k in self.names}
        self.dcnt = {k: 0 for k in self.names}
        self.csem = {k: stack.enter_context(nc.semaphore("c_" + k)) for k in self.names}
        self.dsem = {k: [stack.enter_context(nc.semaphore("d_%s%d" % (k, i))) for i in range(NDMA_SEM)]
                     for k in ('sp', 'pool', 'act')}
        self.seen = {k: {} for k in self.names}
        self.lastw = {}
        self.readers = {}
        self.ntile = 0

    def sb(self, shape, dt, name=None):
        self.ntile += 1
        return self.stack.enter_context(self.nc.sbuf_tensor(name or ("t%d" % self.ntile), list(shape), dt))

    def ps(self, shape, dt, name=None):
        self.ntile += 1
        return self.stack.enter_context(self.nc.psum_tensor(name or ("p%d" % self.ntile), list(shape), dt))

    def _need(self, eng, ev, waits):
        semkey, val, peng, isdma = ev
        if (not isdma) and peng == eng and eng == 'pe':
            return
        if self.seen[eng].get(semkey, 0) >= val:
            return
        self.seen[eng][semkey] = val
        waits.append((semkey, val))

    def op(self, eng, fn, reads=(), writes=(), dma=False):
        waits = []
        for b in reads:
            if b in self.lastw:
                self._need(eng, self.lastw[b], waits)
        for b in writes:
            if b in self.lastw:
                self._need(eng, self.lastw[b], waits)
            for ev in self.readers.get(b, ()):
                self._need(eng, ev, waits)
        if dma:
            i = self.dcnt[eng]
            self.dcnt[eng] += 1
            j = i % NDMA_SEM
            tgt = 16 * (i // NDMA_SEM + 1)
            semkey = ('d', eng, j)
            if tgt > 16:
                self._need(eng, (semkey, tgt - 16, eng, True), waits)
            ev = (semkey, tgt, eng, True)
            inc = (semkey, 16)
        else:
            self.cnt[eng] += 1
            semkey = ('c', eng)
            ev = (semkey, self.cnt[eng], eng, False)
            inc = (semkey, 1)
        for b in reads:
            self.readers.setdefault(b, []).append(ev)
        for b in writes:
            self.lastw[b] = ev
            self.readers[b] = []
        self.ops[eng].append((fn, waits, inc))
        return ev

    def fence(self):
        allev = []
        for k in self.names:
            if self.cnt[k]:
                allev.append((('c', k), self.cnt[k], k, False))
        for k in ('sp', 'pool', 'act'):
            for j in range(NDMA_SEM):
                n = (self.dcnt[k] - j + NDMA_SEM - 1) // NDMA_SEM
                if n > 0:
                    allev.append((('d', k, j), 16 * n, k, True))
        for eng in self.names:
            waits = []
            for semkey, val, k, isdma in allev:
                if self.seen[eng].get(semkey, 0) >= val:
                    continue
                if (not isdma) and k == eng:
                    continue
                self.seen[eng][semkey] = val
                waits.append((semkey, val))
            self.cnt[eng] += 1
            self.ops[eng].append((lambda e: e.nop(), waits, (('c', eng), 1)))

    def _sem(self, semkey):
        if semkey[0] == 'c':
            return self.csem[semkey[1]]
        return self.dsem[semkey[1]][semkey[2]]

    def finish(self):
        waits = []
        for k in self.names:
            if self.cnt[k]:
                waits.append((('c', k), self.cnt[k]))
        for k in ('sp', 'pool', 'act'):
            for j in range(NDMA_SEM):
                n = (self.dcnt[k] - j + NDMA_SEM - 1) // NDMA_SEM
                if n > 0:
                    waits.append((('d', k, j), 16 * n))
        self.final_waits = waits

    def emit(self):
        nc = self.nc
        self.finish()
        with nc.Block() as block:
            def run(name):
                def f(e):
                    for fn, waits, inc in self.ops[name]:
                        for semkey, val in waits:
                            e.wait_ge(self._sem(semkey), val)
                        ins = fn(e)
                        ins.then_inc(self._sem(inc[0]), inc[1])
                    if name == 'sp':
                        for semkey, val in self.final_waits:
                            e.wait_ge(self._sem(semkey), val)
                return f
            block.tensor(run('pe'))
            block.scalar(run('act'))
            block.vector(run('dve'))
            block.gpsimd(run('pool'))
            block.sync(run('sp'))

    def dma(self, q, out, in_, reads=(), writes=(), slow=False):
        if slow:
            return self.op(q, lambda e: e.dma_start(out=out, in_=in_, allow_slow_non_contiguous=True), reads, writes, dma=True)
        return self.op(q, lambda e: e.dma_start(out=out, in_=in_), reads, writes, dma=True)

    def mm(self, out, lhsT, rhs, start, stop, reads=(), writes=()):
        return self.op('pe', lambda e: e.matmul(out, lhsT=lhsT, rhs=rhs, start=start, stop=stop), reads, writes)

    def transpose(self, out, in_, ident, reads=(), writes=()):
        return self.op('pe', lambda e: e.transpose(out, in_, ident), reads, writes)

    def act(self, out, in_, func, reads=(), writes=(), **kw):
        return self.op('act', lambda e: e.activation(out, in_, func, **kw), reads, writes)

    def tt(self, eng, out, in0, in1, op, reads=(), writes=()):
        return self.op(eng, lambda e: e.tensor_tensor(out, in0, in1, op), reads, writes)

    def ts(self, eng, out, in0, s1, s2, op0, op1=None, reads=(), writes=(), **kw):
        if op1 is None:
            return self.op(eng, lambda e: e.tensor_scalar(out, in0, s1, s2, op0, **kw), reads, writes)
        return self.op(eng, lambda e: e.tensor_scalar(out, in0, s1, s2, op0, op1, **kw), reads, writes)

    def stt(self, eng, out, in0, scalar, in1, op0, op1, reads=(), writes=()):
        return self.op(eng, lambda e: e.scalar_tensor_tensor(out, in0, scalar, in1, op0, op1), reads, writes)

    def copy(self, eng, out, in_, reads=(), writes=()):
        if eng == 'act':
            return self.op(eng, lambda e: e.copy(out, in_), reads, writes)
        return self.op(eng, lambda e: e.tensor_copy(out, in_), reads, writes)

    def memset(self, eng, ap, val, writes=()):
        return self.op(eng, lambda e: e.memset(ap, val), (), writes)

import math, contextlib


D = 2048
EPS = 1e-6
LN_THETA = math.log(10000.0)
PI = math.pi


class Ctx:
    pass


def setup_common(P, NT):
    c = Ctx()
    c.ones = P.sb([128, 128], BF16, "ones")
    P.memset('pool', c.ones[:], 1.0, writes=['ones'])
    c.psn = 0
    c.pst = [P.ps([128, 512], F32, "psb%d" % i) for i in range(8)]
    c.NT = NT
    return c


def next_ps(c):
    i = c.psn % 8
    c.psn += 1
    return c.pst[i], "psb%d" % i


def rms_rstd(P, c, src, skey, KC, Dn, out_rstd, okey, tmpname):
    NT = c.NT
    ps, pk = next_ps(c)
    for kc in range(KC):
        sq = c.sq[kc % 2]
        sk = "sq%d" % (kc % 2)
        P.act(sq[:], src[:, kc, :], AF.Square, reads=[skey], writes=[sk])
        P.mm(ps[:, 0:NT], c.ones[:], sq[:], kc == 0, kc == KC - 1, reads=['ones', sk], writes=[pk])
    P.ts('dve', out_rstd[:], ps[:, 0:NT], 1.0 / Dn, EPS, ALU.mult, ALU.add, reads=[pk], writes=[okey])
    P.act(out_rstd[:], out_rstd[:], AF.Sqrt, reads=[okey], writes=[okey])
    P.op('dve', lambda e: e.reciprocal(out_rstd[:], out_rstd[:]), reads=[okey], writes=[okey])


def build_k1(T, NT):
    nc = bass.Bass("TRN2", target_bir_lowering=False)
    EIN = 3416
    xT = nc.dram_tensor("xT", [D, T], F32, kind="ExternalInput").ap()
    pos = nc.dram_tensor("pos", [1, T], F32, kind="ExternalInput").ap()
    ridx = nc.dram_tensor("ridx", [128, 4], F32, kind="ExternalInput").ap()
    gpre = nc.dram_tensor("gpre", [D], F32, kind="ExternalInput").ap()
    w_in = nc.dram_tensor("w_in", [D, EIN], F32, kind="ExternalInput").ap()
    qn = nc.dram_tensor("qn", [512], F32, kind="ExternalInput").ap()
    w_uq = nc.dram_tensor("w_uq", [512, 1536], F32, kind="ExternalInput").ap()
    kvn = nc.dram_tensor("kvn", [256], F32, kind="ExternalInput").ap()
    w_ukv = nc.dram_tensor("w_ukv", [256, 2048], F32, kind="ExternalInput").ap()
    o_qa = nc.dram_tensor("o_qa", [1024, T], BF16, kind="ExternalOutput").ap()
    o_kva = nc.dram_tensor("o_kva", [1536, T], BF16, kind="ExternalOutput").ap()
    o_gate = nc.dram_tensor("o_gate", [24, T], F32, kind="ExternalOutput").ap()
    o_qmn = nc.dram_tensor("o_qmn", [1024, T], BF16, kind="ExternalOutput").ap()
    o_qmp = nc.dram_tensor("o_qmp", [512, T], BF16, kind="ExternalOutput").ap()
    o_kmn = nc.dram_tensor("o_kmn", [1024, T], BF16, kind="ExternalOutput").ap()
    o_vm = nc.dram_tensor("o_vm", [1024, T], BF16, kind="ExternalOutput").ap()
    o_kpe = nc.dram_tensor("o_kpe", [64, T], BF16, kind="ExternalOutput").ap()

    with contextlib.ExitStack() as st:
        P = Prog(nc, st)
        c = setup_common(P, NT)
        c.sq = [P.sb([128, NT], BF16, "sq%d" % i) for i in range(2)]
        wsb = P.sb([128, 16, EIN], BF16, "wsb")
        wuq = P.sb([128, 4, 1536], BF16, "wuq")
        wukv = P.sb([128, 2, 2048], BF16, "wukv")
        for kc in range(16):
            P.dma('pool', wsb[:, kc, :], w_in[kc * 128:(kc + 1) * 128, :], writes=['wsb'])
        for kc in range(4):
            P.dma('pool', wuq[:, kc, :], w_uq[kc * 128:(kc + 1) * 128, :], writes=['wuq'])
        for kc in range(2):
            P.dma('pool', wukv[:, kc, :], w_ukv[kc * 128:(kc + 1) * 128, :], writes=['wukv'])
        gsb = P.sb([128, 16], F32, "gsb")
        qnsb = P.sb([128, 4], F32, "qnsb")
        kvnsb = P.sb([128, 2], F32, "kvnsb")
        rsb = P.sb([128, 4], F32, "rsb")
        P.dma('sp', gsb[:], gpre.rearrange("(kc p) -> p kc", p=128), writes=['gsb'], slow=True)
        P.dma('sp', qnsb[:], qn.rearrange("(kc p) -> p kc", p=128), writes=['qnsb'], slow=True)
        P.dma('sp', kvnsb[:], kvn.rearrange("(kc p) -> p kc", p=128), writes=['kvnsb'], slow=True)
        P.dma('sp', rsb[:], ridx[:, :], writes=['rsb'])
        rc = P.sb([128, 8], F32, "rc")
        P.act(rc[:, 0:1], rsb[:, 0:1], AF.Exp, reads=['rsb'], writes=['rc'], scale=-2.0 * LN_THETA / 128.0)
        P.act(rc[:, 1:2], rsb[:, 2:3], AF.Exp, reads=['rsb'], writes=['rc'], scale=-2.0 * LN_THETA / 64.0)
        P.ts('dve', rc[:, 2:3], rsb[:, 1:2], -PI, None, ALU.mult, reads=['rsb', 'rc'], writes=['rc'])
        P.ts('dve', rc[:, 3:4], rsb[:, 3:4], -PI, None, ALU.mult, reads=['rsb', 'rc'], writes=['rc'])
        P.memset('dve', rc[:, 4:5], -PI, writes=['rc'])

        xs = P.sb([128, 16, NT], F32, "xs")
        us = P.sb([128, 16, NT], BF16, "us")
        rstd = P.sb([128, NT], F32, "rstd")
        posb = P.sb([128, NT], F32, "posb")
        ang = P.sb([128, NT], F32, "ang")
        rr = P.sb([128, NT], F32, "rr")
        ri = P.sb([128, NT], I32, "ri")
        cos128 = P.sb([128, NT], F32, "cos128")
        sin128 = P.sb([128, NT], F32, "sin128")
        cos64 = P.sb([128, NT], F32, "cos64")
        sin64 = P.sb([128, NT], F32, "sin64")
        cq = P.sb([128, 4, NT], F32, "cq")
        ckv = P.sb([128, 2, NT], F32, "ckv")
        cqn = P.sb([128, 4, NT], BF16, "cqn")
        ckvn = P.sb([128, 2, NT], BF16, "ckvn")
        t1 = [P.sb([128, NT], F32, "t1_%d" % i) for i in range(2)]
        t2 = [P.sb([128, NT], F32, "t2_%d" % i) for i in range(2)]
        ob = [P.sb([128, NT], BF16, "ob%d" % i) for i in range(4)]
        og = P.sb([128, NT], F32, "og")
        state = {'ob': 0, 'tt': 0, 'q': 0}

        def out_tile():
            i = state['ob'] % 4
            state['ob'] += 1
            return ob[i], "ob%d" % i

        def store(dst, tile, key, rows):
            q = 'sp'
            P.dma(q, dst, tile[0:rows, :], reads=[key])

        def linear(wt, wkey, act, akey, KC, col0, ncols, prow0=0, ps=None, pk=None, first=True):
            if ps is None:
                ps, pk = next_ps(c)
            for kc in range(KC):
                P.mm(ps[prow0:prow0 + ncols, 0:NT], wt[:, kc, col0:col0 + ncols], act[:, kc, :],
                     kc == 0, kc == KC - 1, reads=[wkey, akey], writes=[pk])
            return ps, pk

        def rope_epi(psz, kz, psr, kr, rows, cosT, sinT, dst):
            i = state['tt'] % 2
            state['tt'] += 1
            a, b = t1[i], t2[i]
            P.tt('dve', a[0:rows, :], psz[0:rows, 0:NT], cosT[0:rows, :], ALU.mult,
                 reads=[kz, 'tables'], writes=["t1_%d" % i])
            P.tt('dve', b[0:rows, :], psr[0:rows, 0:NT], sinT[0:rows, :], ALU.mult,
                 reads=[kr, 'tables'], writes=["t2_%d" % i])
            o, ok = out_tile()
            P.tt('pool', o[0:rows, :], a[0:rows, :], b[0:rows, :], ALU.add,
                 reads=["t1_%d" % i, "t2_%d" % i], writes=[ok])
            store(dst, o, ok, rows)

        def plain_epi(psz, kz, rows, dst):
            o, ok = out_tile()
            P.copy('act', o[0:rows, :], psz[0:rows, 0:NT], reads=[kz], writes=[ok])
            store(dst, o, ok, rows)

        def rope_lin(wt, wkey, act, akey, KC, col0, ncols, cosT, sinT, dst):
            half = ncols // 2
            psz, kz = linear(wt, wkey, act, akey, KC, col0, ncols)
            psr, kr = next_ps(c)
            linear(wt, wkey, act, akey, KC, col0 + half, half, 0, psr, kr)
            linear(wt, wkey, act, akey, KC, col0, half, half, psr, kr)
            rope_epi(psz, kz, psr, kr, ncols, cosT, sinT, dst)

        for s0 in range(0, T, NT):
            sl = slice(s0, s0 + NT)
            for kc in range(16):
                P.dma('sp' if kc % 2 == 0 else 'act', xs[:, kc, :], xT[kc * 128:(kc + 1) * 128, sl], writes=['xs'])
            P.dma('sp', posb[:], pos[0:1, sl].to_broadcast([128, NT]), writes=['posb'])
            for (inv_c, sgn_c, cosT, sinT) in ((0, 1, cos128, sin128), (1, 3, cos64, sin64)):
                P.ts('dve', ang[:], posb[:], rc[:, inv_c:inv_c + 1], None, ALU.mult,
                     reads=['posb', 'rc'], writes=['ang'])
                for (shift, outT, scale) in ((0.0, sinT, rsb[:, sgn_c:sgn_c + 1]), (0.5 * PI, cosT, 1.0)):
                    P.ts('dve', rr[:], ang[:], 1.0 / (2 * PI), shift / (2 * PI), ALU.mult, ALU.add,
                         reads=['ang'], writes=['rr'])
                    P.copy('dve', ri[:], rr[:], reads=['rr'], writes=['ri'])
                    P.copy('dve', rr[:], ri[:], reads=['ri'], writes=['rr'])
                    P.stt('dve', rr[:], rr[:], -2 * PI, ang[:], ALU.mult, ALU.add, reads=['rr', 'ang'], writes=['rr'])
                    P.ts('dve', rr[:], rr[:], shift, PI, ALU.add, ALU.min, reads=['rr'], writes=['rr'])
                    P.ts('dve', rr[:], rr[:], -PI, None, ALU.max, reads=['rr'], writes=['rr'])
                    P.act(outT[:], rr[:], AF.Sin, reads=['rr', 'rsb'], writes=['tables'], scale=scale)
            rms_rstd(P, c, xs, 'xs', 16, D, rstd, 'rstd', 'sq')
            for kc in range(16):
                P.stt('dve', us[:, kc, :], xs[:, kc, :], gsb[:, kc:kc + 1], rstd[:],
                      ALU.mult, ALU.mult, reads=['xs', 'gsb', 'rstd'], writes=['us'])
            for h in range(8):
                rope_lin(wsb, 'wsb', us, 'us', 16, h * 128, 128, cos128, sin128, o_qa[h * 128:(h + 1) * 128, sl])
            o1 = 1024
            for ch in range(12):
                col = o1 + ch * 128
                if ch % 4 < 2:
                    rope_lin(wsb, 'wsb', us, 'us', 16, col, 128, cos128, sin128, o_kva[ch * 128:(ch + 1) * 128, sl])
                else:
                    psz, kz = linear(wsb, 'wsb', us, 'us', 16, col, 128)
                    plain_epi(psz, kz, 128, o_kva[ch * 128:(ch + 1) * 128, sl])
            o2 = o1 + 1536
            psz, kz = linear(wsb, 'wsb', us, 'us', 16, o2, 24)
            P.act(og[0:24, :], psz[0:24, 0:NT], AF.Sigmoid, reads=[kz], writes=['og'])
            P.dma('sp', o_gate[:, sl], og[0:24, :], reads=['og'])
            o3 = o2 + 24
            for i in range(4):
                psz, kz = linear(wsb, 'wsb', us, 'us', 16, o3 + i * 128, 128)
                P.copy('act', cq[:, i, :], psz[:, 0:NT], reads=[kz], writes=['cq'])
            o4 = o3 + 512
            for i in range(2):
                psz, kz = linear(wsb, 'wsb', us, 'us', 16, o4 + i * 128, 128)
                P.copy('act', ckv[:, i, :], psz[:, 0:NT], reads=[kz], writes=['ckv'])
            o5 = o4 + 256
            rope_lin(wsb, 'wsb', us, 'us', 16, o5, 64, cos64, sin64, o_kpe[:, sl])
            rms_rstd(P, c, cq, 'cq', 4, 512, rstd, 'rstd', 'sq')
            for kc in range(4):
                P.stt('dve', cqn[:, kc, :], cq[:, kc, :], qnsb[:, kc:kc + 1], rstd[:],
                      ALU.mult, ALU.mult, reads=['cq', 'qnsb', 'rstd'], writes=['cqn'])
            for h in range(8):
                psz, kz = linear(wuq, 'wuq', cqn, 'cqn', 4, h * 192, 128)
                plain_epi(psz, kz, 128, o_qmn[h * 128:(h + 1) * 128, sl])
                rope_lin(wuq, 'wuq', cqn, 'cqn', 4, h * 192 + 128, 64, cos64, sin64, o_qmp[h * 64:(h + 1) * 64, sl])
            rms_rstd(P, c, ckv, 'ckv', 2, 256, rstd, 'rstd', 'sq')
            for kc in range(2):
                P.stt('dve', ckvn[:, kc, :], ckv[:, kc, :], kvnsb[:, kc:kc + 1], rstd[:],
                      ALU.mult, ALU.mult, reads=['ckv', 'kvnsb', 'rstd'], writes=['ckvn'])
            for h in range(8):
                psz, kz = linear(wukv, 'wukv', ckvn, 'ckvn', 2, h * 256, 128)
                plain_epi(psz, kz, 128, o_kmn[h * 128:(h + 1) * 128, sl])
                psz, kz = linear(wukv, 'wukv', ckvn, 'ckvn', 2, h * 256 + 128, 128)
                plain_epi(psz, kz, 128, o_vm[h * 128:(h + 1) * 128, sl])
        P.emit()
    return nc


def ridx_np():
    p = np.arange(128)
    r = np.zeros((128, 4), np.float32)
    r[:, 0] = p % 64
    r[:, 1] = np.where(p < 64, -1.0, 1.0)
    r[:, 2] = p % 32
    r[:, 3] = np.where((p % 64) < 32, -1.0, 1.0)
    return r

import math, contextlib


def make_mask(P, tile, key, QW, base, cm, step, tmp_i, tmp_key):
    P.op('pool', lambda e: e.iota(tmp_i[:, 0:QW], [[step, QW]], base=base, channel_multiplier=cm), (), [tmp_key])
    P.op('dve', lambda e: e.tensor_single_scalar(tile[:, 0:QW], tmp_i[:, 0:QW], 0, ALU.is_ge), [tmp_key], [key])


def build_attn(n_units, Tq, Tk, dchunks, QW, sched, scale, mode, masks_spec):
    nc = bass.Bass("TRN2", target_bir_lowering=False)
    dq = sum(n for _, n in dchunks)
    qT = nc.dram_tensor("qT", [n_units, dq, Tq], BF16, kind="ExternalInput").ap()
    kT = nc.dram_tensor("kT", [n_units, dq, Tk], BF16, kind="ExternalInput").ap()
    v = nc.dram_tensor("v", [n_units, Tk, 128], BF16, kind="ExternalInput").ap()
    ow = 128 if mode == 'norm' else 129
    out = nc.dram_tensor("out", [n_units, Tq, ow], F32, kind="ExternalOutput").ap()
    NKC = Tk // 128
    NQS = QW // 128
    with contextlib.ExitStack() as st:
        P = Prog(nc, st)
        tmp_i = P.sb([128, 512], I32, "tmp_i")
        masks = {}
        for mid, (base, cm, step) in masks_spec.items():
            t = P.sb([128, QW], BF16, "mask_%s" % mid)
            make_mask(P, t, "mask_%s" % mid, QW, base, cm, step, tmp_i, "tmp_i")
            masks[mid] = t
        kts = [P.sb([n, Tk], BF16, "kt%d" % i) for i, (_, n) in enumerate(dchunks)]
        qts = [P.sb([n, Tq], BF16, "qt%d" % i) for i, (_, n) in enumerate(dchunks)]
        vaug = P.sb([128, NKC, 129], BF16, "vaug")
        ps_s = [P.ps([128, 512], F32, "ps_s%d" % i) for i in range(2)]
        ps_a = [P.ps([128, 512], F32, "ps_a%d" % i) for i in range(4)]
        pt = [P.sb([128, QW], BF16, "pt%d" % i) for i in range(3)]
        osb = [P.sb([128, 129], F32, "osb%d" % i) for i in range(2)]
        rden = [P.sb([128, 1], F32, "rden%d" % i) for i in range(2)]
        cnt = {'s': 0, 'pt': 0, 'o': 0, 'a': 0}
        for u in range(n_units):
            P.memset('pool', vaug[:], 1.0, writes=['vaug'])
            for i, (r0, n) in enumerate(dchunks):
                for t0 in range(0, Tk, 4096):
                    t1 = min(Tk, t0 + 4096)
                    P.dma('sp', kts[i][:, t0:t1], kT[u, r0:r0 + n, t0:t1], writes=['kt%d' % i])
                for t0 in range(0, Tq, 4096):
                    t1 = min(Tq, t0 + 4096)
                    P.dma('act', qts[i][:, t0:t1], qT[u, r0:r0 + n, t0:t1], writes=['qt%d' % i])
            for c0 in range(0, NKC, 32):
                c1 = min(NKC, c0 + 32)
                P.dma('sp', vaug[:, c0:c1, 0:128],
                      v[u, c0 * 128:c1 * 128, :].rearrange("(c p) d -> p c d", p=128), writes=['vaug'])
            for qt in range(Tq // QW):
                q0 = qt * QW
                chunks = sched(qt)
                started = [False] * NQS
                last = [None] * NQS
                for ci, (kj, mid, lo, hi) in enumerate(chunks):
                    for qs in range(lo, hi + 1):
                        last[qs] = ci
                accs = []
                for qs in range(NQS):
                    ai = cnt['a'] % 4
                    cnt['a'] += 1
                    accs.append((ps_a[ai], "ps_a%d" % ai))
                for ci, (kj, mid, lo, hi) in enumerate(chunks):
                    si = cnt['s'] % 2
                    cnt['s'] += 1
                    pss, psk = ps_s[si], "ps_s%d" % si
                    for i in range(len(dchunks)):
                        P.mm(pss[:, 0:QW], kts[i][:, kj * 128:(kj + 1) * 128], qts[i][:, q0:q0 + QW],
                             i == 0, i == len(dchunks) - 1, reads=['kt%d' % i, 'qt%d' % i], writes=[psk])
                    pi = cnt['pt'] % 3
                    cnt['pt'] += 1
                    ptt, ptk = pt[pi], "pt%d" % pi
                    P.act(ptt[:], pss[:, 0:QW], AF.Exp, reads=[psk], writes=[ptk], scale=scale)
                    if mid is not None:
                        P.tt('dve', ptt[:], ptt[:], masks[mid][:], ALU.mult, reads=[ptk, "mask_%s" % mid], writes=[ptk])
                    for qs in range(lo, hi + 1):
                        acc, ak = accs[qs]
                        P.mm(acc[:, 0:129], ptt[:, qs * 128:(qs + 1) * 128], vaug[:, kj, :],
                             not started[qs], last[qs] == ci, reads=[ptk, 'vaug'], writes=[ak])
                        started[qs] = True
                for qs in range(NQS):
                    acc, ak = accs[qs]
                    oi = cnt['o'] % 2
                    cnt['o'] += 1
                    o, ok = osb[oi], "osb%d" % oi
                    rows = out[u, q0 + qs * 128:q0 + (qs + 1) * 128, :]
                    if not started[qs]:
                        P.memset('dve', o[:, 0:ow], 0.0, writes=[ok])
                    elif mode == 'raw':
                        P.copy('dve', o[:, 0:129], acc[:, 0:129], reads=[ak], writes=[ok])
                    else:
                        rd, rk = rden[oi], "rden%d" % oi
                        P.ts('dve', rd[:], acc[:, 128:129], 1e-30, None, ALU.max, reads=[ak], writes=[rk])
                        P.op('dve', lambda e, rd=rd: e.reciprocal(rd[:], rd[:]), [rk], [rk])
                        P.ts('dve', o[:, 0:128], acc[:, 0:128], rd[:, 0:1], None, ALU.mult, reads=[ak, rk], writes=[ok])
                    P.dma('sp', rows, o[:, 0:ow], reads=[ok])
        P.emit()
    return nc


def causal_sched(QW):
    n = QW // 128
    def f(qt):
        res = []
        for kj in range(0, n * qt):
            res.append((kj, None, 0, n - 1))
        for dd in range(n):
            res.append((n * qt + dd, "d%d" % dd, dd, n - 1))
        return res
    masks = {"d%d" % dd: (-128 * dd, -1, 1) for dd in range(n)}
    return f, masks


def band_sched():
    def f(qt):
        res = []
        if qt > 0:
            res.append((qt - 1, "prev", 0, 0))
        res.append((qt, "diag", 0, 0))
        return res
    return f, {"diag": (0, -1, 1), "prev": (0, 1, -1)}

import math, contextlib


BIG = 1.0e4


def build_nsa(S):
    nc = bass.Bass("TRN2", target_bir_lowering=False)
    NCMP = (S - 32) // 16 + 1
    NCC = (NCMP + 127) // 128
    NSLC = S // 64
    NJC = (NSLC + 127) // 128
    NKC = S // 128
    scale = 128 ** -0.5
    qT = nc.dram_tensor("qT", [4, 128, S], BF16, kind="ExternalInput").ap()
    kcT = nc.dram_tensor("kcT", [128, S], BF16, kind="ExternalInput").ap()
    vcT = nc.dram_tensor("vcT", [128, S], BF16, kind="ExternalInput").ap()
    ksT = nc.dram_tensor("ksT", [128, S], BF16, kind="ExternalInput").ap()
    vs = nc.dram_tensor("vs", [S, 128], BF16, kind="ExternalInput").ap()
    kwT = nc.dram_tensor("kwT", [128, S], BF16, kind="ExternalInput").ap()
    vw = nc.dram_tensor("vw", [S, 128], BF16, kind="ExternalInput").ap()
    gate = nc.dram_tensor("gate", [S, 6], F32, kind="ExternalInput").ap()
    pe_k = nc.dram_tensor("pe_k", [32, 128], F32, kind="ExternalInput").ap()
    w1_k = nc.dram_tensor("w1_k", [4096, 128], F32, kind="ExternalInput").ap()
    w2_k = nc.dram_tensor("w2_k", [128, 128], F32, kind="ExternalInput").ap()
    pe_v = nc.dram_tensor("pe_v", [32, 128], F32, kind="ExternalInput").ap()
    w1_v = nc.dram_tensor("w1_v", [4096, 128], F32, kind="ExternalInput").ap()
    w2_v = nc.dram_tensor("w2_v", [128, 128], F32, kind="ExternalInput").ap()
    out = nc.dram_tensor("out", [2, S, 128], F32, kind="ExternalOutput").ap()

    with contextlib.ExitStack() as st:
        P = Prog(nc, st)
        tmp_i = P.sb([128, 512], I32, "tmp_i")
        mdiag = P.sb([128, 128], BF16, "m_diag")
        make_mask(P, mdiag, "m_diag", 128, 0, -1, 1, tmp_i, "tmp_i")
        mstrict = P.sb([128, 128], BF16, "m_strict")
        make_mask(P, mstrict, "m_strict", 128, -1, 1, -1, tmp_i, "tmp_i")
        mge = P.sb([128, 128], BF16, "m_ge")
        make_mask(P, mge, "m_ge", 128, 0, 1, -1, tmp_i, "tmp_i")
        ident = P.sb([128, 128], BF16, "ident")
        P.tt('dve', ident[:], mdiag[:], mge[:], ALU.mult, reads=['m_diag', 'm_ge'], writes=['ident'])
        cmask = []
        for m in range(17):
            t = P.sb([128, 128], BF16, "cm%d" % m)
            make_mask(P, t, "cm%d" % m, 128, 128 * m - 31, -16, 1, tmp_i, "tmp_i")
            cmask.append(t)
        Emat = P.sb([128, 64, 128], BF16, "Emat")
        e_i = P.sb([128, 128], I32, "e_i")
        e_f = P.sb([128, 128], BF16, "e_f")
        P.op('pool', lambda e: e.iota(e_i[:], [[1, 128]], base=0, channel_multiplier=-1), (), ['e_i'])
        P.op('dve', lambda e: e.tensor_single_scalar(e_f[:], e_i[:], 0, ALU.is_equal), ['e_i'], ['e_f'])
        for half in range(2):
            src = e_f[:].rearrange("p (m h) -> p m h", h=2)[:, :, half:half + 1]
            P.copy('dve', Emat[:, :, half * 64:(half + 1) * 64], src.to_broadcast([128, 64, 64]), reads=['e_f'], writes=['Emat'])
        fpB = P.sb([128, 3], F32, "fpB")
        P.memset('dve', fpB[:], 0.0, writes=['fpB'])
        P.memset('dve', fpB[0:64, 0:1], BIG, writes=['fpB'])
        P.memset('dve', fpB[:, 1:2], BIG, writes=['fpB'])
        P.memset('dve', fpB[64:128, 2:3], BIG, writes=['fpB'])

        pa = [P.ps([128, 512], F32, "pa%d" % i) for i in range(4)]
        pss = [P.ps([128, 512], F32, "pss%d" % i) for i in range(2)]
        pst = P.ps([128, 1024], BF16, "pst")
        psx = P.ps([128, 512], F32, "psx")

        kcmpT = P.sb([128, NCC * 128], BF16, "kcmpT")
        VAc = P.sb([128, NCC, 385], BF16, "VAc")
        P.memset('pool', kcmpT[:], 0.0, writes=['kcmpT'])
        P.memset('pool', VAc[:], 0.0, writes=['VAc'])
        P.memset('pool', VAc[:, :, 128:129], 1.0, writes=['VAc'])
        am = P.sb([128, 256], BF16, "am")
        for c in range(NCC):
            P.op('pool', lambda e, c=c: e.iota(tmp_i[:, 0:256], [[-4, 256]], base=128 * c + 1, channel_multiplier=1), (), ['tmp_i'])
            P.op('dve', lambda e: e.tensor_single_scalar(am[:], tmp_i[:, 0:256], 0, ALU.is_ge), ['tmp_i'], ['am'])
            P.op('pool', lambda e, c=c: e.iota(tmp_i[:, 0:256], [[4, 256]], base=3 - 128 * c, channel_multiplier=-1), (), ['tmp_i'])
            P.op('dve', lambda e, c=c: e.tensor_single_scalar(VAc[:, c, 129:385], tmp_i[:, 0:256], 0, ALU.is_ge), ['tmp_i'], ['VAc'])
            P.tt('dve', VAc[:, c, 129:385], VAc[:, c, 129:385], am[:], ALU.mult, reads=['VAc', 'am'], writes=['VAc'])
        inner = contextlib.ExitStack()
        outer_stack = P.stack
        P.stack = inner
        srcs = P.sb([128, S], BF16, "srcs")
        w1s = P.sb([128, 32, 128], BF16, "w1s")
        w2s = P.sb([128, 128], BF16, "w2s")
        peT = P.sb([128, 32], BF16, "peT")
        cb = P.sb([128, 1], F32, "cb")
        xs_ = P.sb([128, 512], F32, "cx")
        x2 = P.sb([128, 512], F32, "cx2")
        gl = P.sb([128, NCC * 128], BF16, "gl")
        for which in range(2):
            w1, w2, pe, srcd = ((w1_k, w2_k, pe_k, kcT), (w1_v, w2_v, pe_v, vcT))[which]
            src, skey = srcs, 'srcs'
            P.dma('sp', srcs[:], srcd[:, :], writes=['srcs'])
            P.dma('pool', w1s[:], w1.rearrange("(l d) h -> d l h", d=128), writes=['w1s'])
            P.dma('pool', w2s[:], w2[:, :], writes=['w2s'])
            P.dma('pool', peT[:], pe.rearrange("l d -> d l"), writes=['peT'], slow=True)
            for l in range(32):
                P.mm(psx[:, 0:1], w1s[:, l, :], peT[:, l:l + 1], l == 0, l == 31, reads=['w1s', 'peT'], writes=['psx'])
            P.copy('dve', cb[:], psx[:, 0:1], reads=['psx'], writes=['cb'])
            P.memset('pool', gl[:], 0.0, writes=['gl'])
            src3 = src[:].rearrange("p (n r) -> p n r", r=16)
            for n0 in range(0, NCMP, 512):
                ncols = min(512, NCMP - n0)
                for l in range(32):
                    P.mm(psx[:, 0:ncols], w1s[:, l, :], src3[:, n0 + l // 16:n0 + l // 16 + ncols, l % 16],
                         l == 0, l == 31, reads=['w1s', skey], writes=['psx'])
                xa = xs_[:, 0:ncols]
                xb = x2[:, 0:ncols]
                P.act(xa, psx[:, 0:ncols], AF.Identity, reads=['psx', 'cb'], writes=['cx'], bias=cb[:, 0:1], scale=1.0)
                P.tt('dve', xb, xa, xa, ALU.mult, reads=['cx'], writes=['cx2'])
                P.ts('dve', xb, xb, 0.044715, 1.0, ALU.mult, ALU.add, reads=['cx2'], writes=['cx2'])
                P.tt('dve', xb, xb, xa, ALU.mult, reads=['cx', 'cx2'], writes=['cx2'])
                P.act(xb, xb, AF.Tanh, reads=['cx2'], writes=['cx2'], scale=math.sqrt(2.0 / math.pi))
                P.ts('dve', xb, xb, 0.5, 0.5, ALU.mult, ALU.add, reads=['cx2'], writes=['cx2'])
                P.tt('dve', gl[:, n0:n0 + ncols], xb, xa, ALU.mult, reads=['cx', 'cx2'], writes=['gl'])
            if which == 0:
                for n0 in range(0, NCMP, 512):
                    ncols = min(512, NCMP - n0)
                    P.mm(psx[:, 0:ncols], w2s[:], gl[:, n0:n0 + ncols], True, True, reads=['w2s', 'gl'], writes=['psx'])
                    P.copy('dve', kcmpT[:, n0:n0 + ncols], psx[:, 0:ncols], reads=['psx'], writes=['kcmpT'])
            else:
                for c in range(NCC):
                    P.mm(psx[:, 0:128], gl[:, c * 128:(c + 1) * 128], w2s[:], True, True, reads=['w2s', 'gl'], writes=['psx'])
                    P.copy('dve', VAc[:, c, 0:128], psx[:, 0:128], reads=['psx'], writes=['VAc'])

        inner.close()
        P.stack = outer_stack
        P.fence()
        kss = P.sb([128, S], BF16, "kss")
        kws = P.sb([128, S], BF16, "kws")
        P.dma('sp', kss[:], ksT[:, :], writes=['kss'])
        P.dma('act', kws[:], kwT[:, :], writes=['kws'])
        vsa = P.sb([128, NKC, 129], BF16, "vsa")
        vwa = P.sb([128, NKC, 129], BF16, "vwa")
        P.memset('pool', vsa[:], 1.0, writes=['vsa'])
        P.memset('pool', vwa[:], 1.0, writes=['vwa'])
        for c0 in range(0, NKC, 32):
            c1 = min(NKC, c0 + 32)
            P.dma('sp', vsa[:, c0:c1, 0:128], vs[c0 * 128:c1 * 128, :].rearrange("(c p) d -> p c d", p=128), writes=['vsa'])
            P.dma('act', vwa[:, c0:c1, 0:128], vw[c0 * 128:c1 * 128, :].rearrange("(c p) d -> p c d", p=128), writes=['vwa'])
        gsb = P.sb([128, NKC, 6], F32, "gsb")
        P.dma('sp', gsb[:], gate.rearrange("(c p) g -> p c g", p=128), writes=['gsb'])
        qblk = [P.sb([128, 4, 512], BF16, "qblk%d" % i) for i in range(2)]
        pt = [P.sb([128, 128], BF16, "pt%d" % i) for i in range(4)]
        rden = P.sb([128, 8], F32, "rden")
        imp = P.sb([128, NSLC], F32, "imp")
        work = P.sb([128, NSLC], F32, "work")
        m8 = P.sb([128, 16], F32, "m8")
        thr = P.sb([128, 1], F32, "thr")
        sel = P.sb([128, NJC * 128], BF16, "sel")
        P.memset('pool', sel[:], 0.0, writes=['sel'])
        biasT = P.sb([128, NJC, 128], BF16, "biasT")
        P.memset('pool', biasT[:], 0.0, writes=['biasT'])
        oc = [P.sb([128, 128], F32, "oc%d" % h) for h in range(2)]
        ot = [P.sb([128, 128], F32, "ot%d" % h) for h in range(2)]
        rg = P.sb([128, 8], F32, "rg")
        cnt = {'s': 0, 'pt': 0}

        def score_exp(klhs, kkey, kj, qh, qkey, q0, mask, mkey, bias_args=None):
            si = cnt['s'] % 2
            cnt['s'] += 1
            ps, pk = pss[si], "pss%d" % si
            P.mm(ps[:, 0:128], klhs[:, kj * 128:(kj + 1) * 128], qh[:, q0:q0 + 128], True, bias_args is None,
                 reads=[kkey, qkey], writes=[pk])
            if bias_args is not None:
                P.mm(ps[:, 0:128], Emat[:, kj % 64, :], biasT[:, kj // 64, :], False, True,
                     reads=['Emat', 'biasT'], writes=[pk])
            pi = cnt['pt'] % 4
            cnt['pt'] += 1
            p_, ptk = pt[pi], "pt%d" % pi
            P.act(p_[:], ps[:, 0:128], AF.Exp, reads=[pk], writes=[ptk], scale=scale)
            if mask is not None:
                P.tt('dve', p_[:], p_[:], mask[:], ALU.mult, reads=[ptk, mkey], writes=[ptk])
            return p_, ptk

        class QV:
            pass
        for i in range(NKC):
            q0 = 128 * i
            if i % 4 == 0:
                qb, qbk = qblk[(i // 4) % 2], "qblk%d" % ((i // 4) % 2)
                w = min(512, S - q0)
                for h in range(4):
                    P.dma('sp' if h % 2 == 0 else 'act', qb[:, h, 0:w], qT[h, :, q0:q0 + w], writes=[qbk])
            qoff = q0 - 512 * (i // 4)
            vis = []
            for c in range(NCC):
                m = (q0 - 2048 * c) // 128
                if m < 0:
                    continue
                vis.append((c, m))
            for h in range(4):
                for idx, (c, m) in enumerate(vis):
                    mk = cmask[m] if m <= 16 else None
                    p_, ptk = score_exp(kcmpT, 'kcmpT', c, qb[:, h, :], qbk, qoff, mk, "cm%d" % m if m <= 16 else None)
                    P.mm(pa[h][:, 0:385], p_[:], VAc[:, c, :], idx == 0, idx == len(vis) - 1,
                         reads=[ptk, 'VAc'], writes=['pa%d' % h])
                P.ts('dve', rden[:, h:h + 1], pa[h][:, 128:129], 1e-30, None, ALU.max, reads=['pa%d' % h], writes=['rden'])
                P.op('dve', lambda e, h=h: e.reciprocal(rden[:, h:h + 1], rden[:, h:h + 1]), ['rden'], ['rden'])
                if h < 2:
                    P.ts('dve', oc[h][:], pa[h][:, 0:128], rden[:, h:h + 1], None, ALU.mult,
                         reads=['pa%d' % h, 'rden'], writes=['oc%d' % h])
                if h == 0:
                    P.ts('dve', imp[:], pa[h][:, 129:129 + NSLC], rden[:, h:h + 1], None, ALU.mult,
                         reads=['pa%d' % h, 'rden'], writes=['imp'])
                else:
                    P.stt('dve', imp[:], pa[h][:, 129:129 + NSLC], rden[:, h:h + 1], imp[:], ALU.mult, ALU.add,
                          reads=['pa%d' % h, 'rden', 'imp'], writes=['imp'])
            lo = max(0, 2 * i - 1)
            P.tt('dve', imp[:, lo:2 * i + 2], imp[:, lo:2 * i + 2], fpB[:, lo - (2 * i - 1):3], ALU.add,
                 reads=['imp', 'fpB'], writes=['imp'])
            P.ts('dve', imp[:, 0:1], imp[:, 0:1], BIG, None, ALU.add, reads=['imp'], writes=['imp'])
            P.op('dve', lambda e: e.max(out=m8[:, 0:8], in_=imp[:]), ['imp'], ['m8'])
            P.op('dve', lambda e: e.match_replace(out=work[:], in_to_replace=m8[:, 0:8], in_values=imp[:], imm_value=-1.0e9),
                 ['imp', 'm8'], ['work'])
            P.op('dve', lambda e: e.max(out=m8[:, 8:16], in_=work[:]), ['work'], ['m8'])
            P.op('dve', lambda e: e.tensor_reduce(out=thr[:], in_=m8[:, 8:16], axis=AX.X, op=ALU.min), ['m8'], ['thr'])
            P.ts('dve', sel[:, 0:NSLC], imp[:], thr[:, 0:1], None, ALU.is_ge, reads=['imp', 'thr'], writes=['sel'])
            for jc in range(NJC):
                rows = min(128, NSLC - jc * 128)
                P.transpose(pst[0:rows, jc * 128:(jc + 1) * 128], sel[:, jc * 128:jc * 128 + rows], ident[:],
                            reads=['sel', 'ident'], writes=['pst'])
                P.ts('dve', biasT[0:rows, jc, :], pst[0:rows, jc * 128:(jc + 1) * 128], -1.0, 30000.0, ALU.add, ALU.mult,
                     reads=['pst'], writes=['biasT'])
            for h in range(2):
                accs, acck = pa[h], 'pa%d' % h
                for kj in range(i + 1):
                    p_, ptk = score_exp(kss, 'kss', kj, qb[:, h, :], qbk, qoff,
                                        mdiag if kj == i else None, 'm_diag', bias_args=True)
                    P.mm(accs[:, 0:129], p_[:], vsa[:, kj, :], kj == 0, kj == i, reads=[ptk, 'vsa'], writes=[acck])
                accw, acwk = pa[2 + h], 'pa%d' % (2 + h)
                wl = list(range(max(0, i - 4), i + 1))
                for idx, kj in enumerate(wl):
                    mk, mkey = (mdiag, 'm_diag') if kj == i else ((mstrict, 'm_strict') if kj == i - 4 else (None, None))
                    p_, ptk = score_exp(kws, 'kws', kj, qb[:, h, :], qbk, qoff, mk, mkey)
                    P.mm(accw[:, 0:129], p_[:], vwa[:, kj, :], idx == 0, idx == len(wl) - 1, reads=[ptk, 'vwa'], writes=[acwk])
                P.ts('dve', rg[:, 0:1], accs[:, 128:129], 1e-30, None, ALU.max, reads=[acck], writes=['rg'])
                P.ts('dve', rg[:, 1:2], accw[:, 128:129], 1e-30, None, ALU.max, reads=[acwk], writes=['rg'])
                P.op('dve', lambda e: e.reciprocal(rg[:, 0:2], rg[:, 0:2]), ['rg'], ['rg'])
                P.tt('dve', rg[:, 0:2], rg[:, 0:2], gsb[:, i, 3 * h + 1:3 * h + 3], ALU.mult, reads=['rg', 'gsb'], writes=['rg'])
                o_, ok = ot[h], 'ot%d' % h
                P.ts('dve', o_[:], oc[h][:], gsb[:, i, 3 * h:3 * h + 1], None, ALU.mult, reads=['oc%d' % h, 'gsb'], writes=[ok])
                P.stt('dve', o_[:], accs[:, 0:128], rg[:, 0:1], o_[:], ALU.mult, ALU.add, reads=[acck, 'rg', ok], writes=[ok])
                P.stt('dve', o_[:], accw[:, 0:128], rg[:, 1:2], o_[:], ALU.mult, ALU.add, reads=[acwk, 'rg', ok], writes=[ok])
                P.dma('sp', out[h, q0:q0 + 128, :], o_[:], reads=[ok])
        P.emit()
    return nc

import math, contextlib


DFF = 8192


def build_post(T, NT, layer):
    nc = bass.Bass("TRN2", target_bir_lowering=False)
    KCo = 16 if layer == 0 else 8
    hT = nc.dram_tensor("hT", [D, T], F32, kind="ExternalInput").ap()
    if layer == 0:
        oT = nc.dram_tensor("oT", [2048, T], F32, kind="ExternalInput").ap()
        pos = nc.dram_tensor("pos", [1, T], F32, kind="ExternalInput").ap()
        ridx = nc.dram_tensor("ridx", [128, 4], F32, kind="ExternalInput").ap()
        g_in = nc.dram_tensor("g_in", [D], F32, kind="ExternalInput").ap()
        w_in = nc.dram_tensor("w_in", [D, 9216], F32, kind="ExternalInput").ap()
        o_z = nc.dram_tensor("o_z", [9216, T], BF16, kind="ExternalOutput").ap()
    else:
        numT = nc.dram_tensor("numT", [3, 1024, T], F32, kind="ExternalInput").ap()
        den = nc.dram_tensor("den", [3, 8, T], F32, kind="ExternalInput").ap()
    w_out = nc.dram_tensor("w_out", [KCo * 128, D], F32, kind="ExternalInput").ap()
    g_post = nc.dram_tensor("g_post", [D], F32, kind="ExternalInput").ap()
    g_fpre = nc.dram_tensor("g_fpre", [D], F32, kind="ExternalInput").ap()
    w_ff1 = nc.dram_tensor("w_ff1", [D, DFF], F32, kind="ExternalInput").ap()
    w_ff2 = nc.dram_tensor("w_ff2", [DFF, D], F32, kind="ExternalInput").ap()
    g_fpost = nc.dram_tensor("g_fpost", [D], F32, kind="ExternalInput").ap()
    o_h = nc.dram_tensor("o_h", [D, T], F32, kind="ExternalOutput").ap()

    with contextlib.ExitStack() as st:
        P = Prog(nc, st)
        c = setup_common(P, NT)
        c.sq = [P.sb([128, NT], BF16, "sq%d" % i) for i in range(2)]
        gs = P.sb([128, 4, 16], F32, "gs")
        for i, g in enumerate([g_post, g_fpre, g_fpost] + ([g_in] if layer == 0 else [])):
            P.dma('sp', gs[:, i, :], g.rearrange("(kc p) -> p kc", p=128), writes=['gs'], slow=True)
        hs = P.sb([128, 16, NT], F32, "hs")
        us = P.sb([128, 16, NT], BF16, "us")
        ms = P.sb([128, 16, NT], F32, "ms")
        f1 = P.sb([128, 64, NT], BF16, "f1")
        rstd = P.sb([128, NT], F32, "rstd")
        tmp = [P.sb([128, NT], F32, "tmp%d" % i) for i in range(2)]
        wb = [P.sb([128, 8192], BF16, "wb%d" % i) for i in range(2)]
        ob = [P.sb([128, NT], BF16, "ob%d" % i) for i in range(3)]
        cnt = {'w': 0, 'ob': 0, 't': 0}
        if layer == 0:
            rsb = P.sb([128, 4], F32, "rsb")
            P.dma('sp', rsb[:], ridx[:, :], writes=['rsb'])
            rc = P.sb([128, 2], F32, "rc")
            P.act(rc[:, 0:1], rsb[:, 0:1], AF.Exp, reads=['rsb'], writes=['rc'], scale=-2.0 * LN_THETA / 128.0)
            posb = P.sb([128, NT], F32, "posb")
            ang = P.sb([128, NT], F32, "ang")
            ri = P.sb([128, NT], I32, "ri")
            cosT = P.sb([128, NT], F32, "cosT")
            sinT = P.sb([128, NT], F32, "sinT")

        def wload(src_ap_fn, nseg, segw):
            i = cnt['w'] % 2
            cnt['w'] += 1
            t, k = wb[i], "wb%d" % i
            view = t[:, 0:nseg * segw].rearrange("p (s w) -> p s w", w=segw)
            half = (nseg + 1) // 2
            for a, b in ((0, half), (half, nseg)):
                if b > a:
                    P.dma('pool', view[:, a:b, :], src_ap_fn(a, b), writes=[k])
            return view, k

        def add_norm(src, skey, gi):
            rms_rstd(P, c, src, skey, 16, D, rstd, 'rstd', 'sq')
            for kc in range(16):
                i = cnt['t'] % 2
                cnt['t'] += 1
                P.stt('dve', tmp[i][:], src[:, kc, :], gs[:, gi, kc:kc + 1], rstd[:], ALU.mult, ALU.mult,
                      reads=[skey, 'gs', 'rstd'], writes=['tmp%d' % i])
                P.tt('pool', hs[:, kc, :], hs[:, kc, :], tmp[i][:], ALU.add, reads=['hs', 'tmp%d' % i], writes=['hs'])

        def norm_to_us(gi):
            rms_rstd(P, c, hs, 'hs', 16, D, rstd, 'rstd', 'sq')
            for kc in range(16):
                P.stt('dve', us[:, kc, :], hs[:, kc, :], gs[:, gi, kc:kc + 1], rstd[:], ALU.mult, ALU.mult,
                      reads=['hs', 'gs', 'rstd'], writes=['us'])

        for s0 in range(0, T, NT):
            sl = slice(s0, s0 + NT)
            for kc in range(16):
                P.dma('sp' if kc % 2 == 0 else 'act', hs[:, kc, :], hT[kc * 128:(kc + 1) * 128, sl], writes=['hs'])
            if layer == 0:
                for kc in range(16):
                    P.dma('pool', us[:, kc, :], oT[kc * 128:(kc + 1) * 128, sl], writes=['us'])
            else:
                for h in range(8):
                    for g in range(3):
                        P.dma('sp' if g % 2 == 0 else 'act', ms[:, g, :], numT[g, h * 128:(h + 1) * 128, sl], writes=['ms'])
                        P.dma('act' if g % 2 == 0 else 'sp', ms[:, 4 + g, :], den[g, h:h + 1, sl].to_broadcast([128, NT]), writes=['ms'])
                    P.tt('dve', ms[:, 0, :], ms[:, 0, :], ms[:, 1, :], ALU.add, reads=['ms'], writes=['ms'])
                    P.tt('dve', ms[:, 0, :], ms[:, 0, :], ms[:, 2, :], ALU.add, reads=['ms'], writes=['ms'])
                    P.tt('pool', ms[:, 4, :], ms[:, 4, :], ms[:, 5, :], ALU.add, reads=['ms'], writes=['ms'])
                    P.tt('pool', ms[:, 4, :], ms[:, 4, :], ms[:, 6, :], ALU.add, reads=['ms'], writes=['ms'])
                    P.ts('dve', ms[:, 4, :], ms[:, 4, :], 1e-30, None, ALU.max, reads=['ms'], writes=['ms'])
                    P.op('dve', lambda e: e.reciprocal(ms[:, 4, :], ms[:, 4, :]), ['ms'], ['ms'])
                    P.tt('dve', us[:, h, :], ms[:, 0, :], ms[:, 4, :], ALU.mult, reads=['ms'], writes=['us'])
            for cb in range(4):
                wv, wk = wload(lambda a, b, cb=cb: w_out[a * 128:b * 128, cb * 512:(cb + 1) * 512].rearrange("(s p) w -> p s w", p=128), KCo, 512)
                for j in range(4):
                    ps, pk = next_ps(c)
                    for kc in range(KCo):
                        P.mm(ps[:, 0:NT], wv[:, kc, j * 128:(j + 1) * 128], us[:, kc, :], kc == 0, kc == KCo - 1,
                             reads=[wk, 'us'], writes=[pk])
                    P.copy('act', ms[:, cb * 4 + j, :], ps[:, 0:NT], reads=[pk], writes=['ms'])
            add_norm(ms, 'ms', 0)
            norm_to_us(1)
            for cb in range(16):
                wv, wk = wload(lambda a, b, cb=cb: w_ff1[a * 128:b * 128, cb * 512:(cb + 1) * 512].rearrange("(s p) w -> p s w", p=128), 16, 512)
                for j in range(4):
                    ps, pk = next_ps(c)
                    for kc in range(16):
                        P.mm(ps[:, 0:NT], wv[:, kc, j * 128:(j + 1) * 128], us[:, kc, :], kc == 0, kc == 15,
                             reads=[wk, 'us'], writes=[pk])
                    i = cnt['t'] % 2
                    cnt['t'] += 1
                    P.act(tmp[i][:], ps[:, 0:NT], AF.Relu, reads=[pk], writes=['tmp%d' % i])
                    P.tt('dve', f1[:, cb * 4 + j, :], tmp[i][:], tmp[i][:], ALU.mult, reads=['tmp%d' % i], writes=['f1'])
            for cb in range(16):
                wv, wk = wload(lambda a, b, cb=cb: w_ff2[a * 128:b * 128, cb * 128:(cb + 1) * 128].rearrange("(s p) w -> p s w", p=128), 64, 128)
                ps, pk = next_ps(c)
                for kc in range(64):
                    P.mm(ps[:, 0:NT], wv[:, kc, :], f1[:, kc, :], kc == 0, kc == 63, reads=[wk, 'f1'], writes=[pk])
                P.copy('act', ms[:, cb, :], ps[:, 0:NT], reads=[pk], writes=['ms'])
            add_norm(ms, 'ms', 2)
            for kc in range(16):
                P.dma('sp' if kc % 2 == 0 else 'act', o_h[kc * 128:(kc + 1) * 128, sl], hs[:, kc, :], reads=['hs'])
            if layer == 0:
                P.dma('sp', posb[:], pos[0:1, sl].to_broadcast([128, NT]), writes=['posb'])
                P.ts('dve', ang[:], posb[:], rc[:, 0:1], None, ALU.mult, reads=['posb', 'rc'], writes=['ang'])
                for (shift, outT, scale) in ((0.0, sinT, rsb[:, 1:2]), (0.5 * PI, cosT, 1.0)):
                    rr = tmp[0]
                    P.ts('dve', rr[:], ang[:], 1.0 / (2 * PI), shift / (2 * PI), ALU.mult, ALU.add, reads=['ang'], writes=['tmp0'])
                    P.copy('dve', ri[:], rr[:], reads=['tmp0'], writes=['ri'])
                    P.copy('dve', rr[:], ri[:], reads=['ri'], writes=['tmp0'])
                    P.stt('dve', rr[:], rr[:], -2 * PI, ang[:], ALU.mult, ALU.add, reads=['tmp0', 'ang'], writes=['tmp0'])
                    P.ts('dve', rr[:], rr[:], shift, PI, ALU.add, ALU.min, reads=['tmp0'], writes=['tmp0'])
                    P.ts('dve', rr[:], rr[:], -PI, None, ALU.max, reads=['tmp0'], writes=['tmp0'])
                    P.act(outT[:], rr[:], AF.Sin, reads=['tmp0', 'rsb'], writes=['tables'], scale=scale)
                norm_to_us(3)
                for cb in range(18):
                    wv, wk = wload(lambda a, b, cb=cb: w_in[a * 128:b * 128, cb * 512:(cb + 1) * 512].rearrange("(s p) w -> p s w", p=128), 16, 512)
                    for j in range(4):
                        ch = cb * 4 + j
                        ttype = (ch // 8) % 3
                        ps, pk = next_ps(c)
                        for kc in range(16):
                            P.mm(ps[:, 0:NT], wv[:, kc, j * 128:(j + 1) * 128], us[:, kc, :], kc == 0, kc == 15,
                                 reads=[wk, 'us'], writes=[pk])
                        oi = cnt['ob'] % 3
                        cnt['ob'] += 1
                        o, ok = ob[oi], "ob%d" % oi
                        if ttype == 2:
                            P.copy('act', o[:], ps[:, 0:NT], reads=[pk], writes=[ok])
                        else:
                            pr, prk = next_ps(c)
                            for kc in range(16):
                                P.mm(pr[0:64, 0:NT], wv[:, kc, j * 128 + 64:j * 128 + 128], us[:, kc, :], kc == 0, kc == 15,
                                     reads=[wk, 'us'], writes=[prk])
                            for kc in range(16):
                                P.mm(pr[64:128, 0:NT], wv[:, kc, j * 128:j * 128 + 64], us[:, kc, :], kc == 0, kc == 15,
                                     reads=[wk, 'us'], writes=[prk])
                            P.tt('dve', tmp[0][:], ps[:, 0:NT], cosT[:], ALU.mult, reads=[pk, 'tables'], writes=['tmp0'])
                            P.tt('dve', tmp[1][:], pr[:, 0:NT], sinT[:], ALU.mult, reads=[prk, 'tables'], writes=['tmp1'])
                            P.tt('pool', o[:], tmp[0][:], tmp[1][:], ALU.add, reads=['tmp0', 'tmp1'], writes=[ok])
                        P.dma('sp', o_z[ch * 128:(ch + 1) * 128, sl], o[:], reads=[ok])
        P.emit()
    return nc


import ml_dtypes as _mld

_B, _S, _NCORE = 2, 16384, 8
_T = 4096


def _run(nc, in_maps):
    res = run_bass_kernel_spmd(nc, in_maps, core_ids=list(range(_NCORE)))
    return res.results


def kernel(**inp):
    inp = {k: np.asarray(v) for k, v in inp.items()}
    x = inp["x"]
    B, S, T = _B, _S, _T
    pos_all = np.arange(S, dtype=np.float32)
    ridx = ridx_np()

    def tok(c):
        b = c // 4
        s0 = (c % 4) * T
        return b, slice(s0, s0 + T)

    nc = build_k1(T, 256)
    in_maps = []
    xT_core = []
    for c in range(_NCORE):
        b, sl = tok(c)
        xT = np.ascontiguousarray(x[b, sl].T)
        xT_core.append(xT)
        in_maps.append(dict(xT=xT, pos=pos_all[None, sl].copy(), ridx=ridx, gpre=inp["l0_norm_mix_pre"], w_in=inp["l0_w_in"],
                            qn=inp["l0_mla_q_norm"], w_uq=inp["l0_mla_w_uq"], kvn=inp["l0_mla_kv_norm"], w_ukv=inp["l0_mla_w_ukv"]))
    r1 = _run(nc, in_maps)
    del in_maps

    def full(name, b):
        return np.concatenate([np.asarray(r1[c][name]) for c in range(_NCORE) if c // 4 == b], axis=1)
    QA = [full("o_qa", b) for b in range(B)]
    KVA = [full("o_kva", b) for b in range(B)]
    GATE = [full("o_gate", b) for b in range(B)]
    QMN = [full("o_qmn", b) for b in range(B)]
    QMP = [full("o_qmp", b) for b in range(B)]
    KMN = [full("o_kmn", b) for b in range(B)]
    VM = [full("o_vm", b) for b in range(B)]
    KPE = [full("o_kpe", b) for b in range(B)]
    del r1

    nc = build_nsa(S)
    in_maps = []
    own_heads = []
    for c in range(_NCORE):
        b, g, half = c // 4, (c // 2) % 2, c % 2
        own = [g * 4 + half * 2, g * 4 + half * 2 + 1]
        oth = [g * 4 + (1 - half) * 2, g * 4 + (1 - half) * 2 + 1]
        own_heads.append((b, own))
        ch = lambda br, kv: KVA[b][(br * 4 + kv * 2 + g) * 128:(br * 4 + kv * 2 + g + 1) * 128]
        qT = np.stack([QA[b][h * 128:(h + 1) * 128] for h in own + oth])
        gate = np.ascontiguousarray(np.concatenate([GATE[b][h * 3:(h + 1) * 3] for h in own], axis=0).T)
        in_maps.append(dict(qT=qT, kcT=np.ascontiguousarray(ch(0, 0)), vcT=np.ascontiguousarray(ch(0, 1)),
                            ksT=np.ascontiguousarray(ch(1, 0)), vs=np.ascontiguousarray(ch(1, 1).T),
                            kwT=np.ascontiguousarray(ch(2, 0)), vw=np.ascontiguousarray(ch(2, 1).T), gate=gate,
                            pe_k=inp["l0_cmp_pe_k"], w1_k=inp["l0_cmp_w1_k"], w2_k=inp["l0_cmp_w2_k"],
                            pe_v=inp["l0_cmp_pe_v"], w1_v=inp["l0_cmp_w1_v"], w2_v=inp["l0_cmp_w2_v"]))
    r2 = _run(nc, in_maps)
    del in_maps
    OT = [np.empty((2048, S), np.float32) for _ in range(B)]
    for c in range(_NCORE):
        b, own = own_heads[c]
        o = np.asarray(r2[c]["out"])
        for i, h in enumerate(own):
            OT[b][h * 128:(h + 1) * 128] = o[i].T
    del r2

    sched, masks = causal_sched(512)
    nc = build_attn(2, S, S, [(0, 128), (128, 64)], 512, sched, 192 ** -0.5, 'norm', masks)
    in_maps = []
    for c in range(_NCORE):
        b = c // 4
        hs_ = [2 * (c % 4), 2 * (c % 4) + 1]
        qT = np.stack([np.concatenate([QMN[b][h * 128:(h + 1) * 128], QMP[b][h * 64:(h + 1) * 64]], axis=0) for h in hs_])
        kT = np.stack([np.concatenate([KMN[b][h * 128:(h + 1) * 128], KPE[b]], axis=0) for h in hs_])
        v = np.stack([np.ascontiguousarray(VM[b][h * 128:(h + 1) * 128].T) for h in hs_])
        in_maps.append(dict(qT=qT, kT=kT, v=v))
    r3 = _run(nc, in_maps)
    del in_maps
    for c in range(_NCORE):
        b = c // 4
        o = np.asarray(r3[c]["out"])
        for i, h in enumerate([2 * (c % 4), 2 * (c % 4) + 1]):
            OT[b][1024 + h * 128:1024 + (h + 1) * 128] = o[i].T
    del r3, QA, KVA, GATE, QMN, QMP, KMN, VM, KPE

    nc = build_post(T, 512, 0)
    in_maps = []
    for c in range(_NCORE):
        b, sl = tok(c)
        in_maps.append(dict(hT=xT_core[c], oT=np.ascontiguousarray(OT[b][:, sl]), pos=pos_all[None, sl].copy(), ridx=ridx,
                            g_in=inp["l1_norm_mix_pre"], w_in=inp["l1_w_in"], w_out=inp["l0_w_out"],
                            g_post=inp["l0_norm_mix_post"], g_fpre=inp["l0_norm_ffn_pre"], w_ff1=inp["l0_w_ff1"],
                            w_ff2=inp["l0_w_ff2"], g_fpost=inp["l0_norm_ffn_post"]))
    r4 = _run(nc, in_maps)
    del in_maps, OT, xT_core
    h2T_core = [np.asarray(r4[c]["o_h"]) for c in range(_NCORE)]
    Z = [np.concatenate([np.asarray(r4[c]["o_z"]) for c in range(_NCORE) if c // 4 == b], axis=1) for b in range(B)]
    del r4

    NUM = np.empty((3, B, 1024, S), np.float32)
    DEN = np.empty((3, B, 8, S), np.float32)
    sched, masks = band_sched()
    for g, r in enumerate((1, 4, 16)):
        nu = 2 * r
        Tq = S // r
        nc = build_attn(nu, Tq, Tq, [(0, 128)], 128, sched, 128 ** -0.5, 'raw', masks)
        units = [(b, h, rho) for b in range(B) for h in range(8) for rho in range(r)]
        in_maps = []
        for c in range(_NCORE):
            us_ = units[c * nu:(c + 1) * nu]
            chq = lambda b, t, h: Z[b][((g * 3 + t) * 8 + h) * 128:((g * 3 + t) * 8 + h + 1) * 128]
            qT = np.stack([np.ascontiguousarray(chq(b, 0, h)[:, rho::r]) for (b, h, rho) in us_])
            kT = np.stack([np.ascontiguousarray(chq(b, 1, h)[:, rho::r]) for (b, h, rho) in us_])
            v = np.stack([np.ascontiguousarray(chq(b, 2, h)[:, rho::r].T) for (b, h, rho) in us_])
            in_maps.append(dict(qT=qT, kT=kT, v=v))
        rr = _run(nc, in_maps)
        del in_maps
        for c in range(_NCORE):
            o = np.asarray(rr[c]["out"])
            for i, (b, h, rho) in enumerate(units[c * nu:(c + 1) * nu]):
                NUM[g, b, h * 128:(h + 1) * 128, rho::r] = o[i, :, 0:128].T
                DEN[g, b, h, rho::r] = o[i, :, 128]
        del rr
    del Z

    nc = build_post(T, 512, 1)
    in_maps = []
    for c in range(_NCORE):
        b, sl = tok(c)
        in_maps.append(dict(hT=h2T_core[c], numT=np.ascontiguousarray(NUM[:, b, :, sl]), den=np.ascontiguousarray(DEN[:, b, :, sl]),
                            w_out=inp["l1_w_out"], g_post=inp["l1_norm_mix_post"], g_fpre=inp["l1_norm_ffn_pre"],
                            w_ff1=inp["l1_w_ff1"], w_ff2=inp["l1_w_ff2"], g_fpost=inp["l1_norm_ffn_post"]))
    r5 = _run(nc, in_maps)
    out = np.empty((B, S, 2048), np.float32)
    for c in range(_NCORE):
        b, sl = tok(c)
        out[b, sl] = np.asarray(r5[c]["o_h"]).T
    return out
```

```python
import contextlib
import numpy as np
import concourse.bass as bass
import concourse.mybir as mybir
from concourse.bass_utils import run_bass_kernel_spmd

F32 = mybir.dt.float32
BF16 = mybir.dt.bfloat16
I32 = mybir.dt.int32
AF = mybir.ActivationFunctionType
ALU = mybir.AluOpType
AX = mybir.AxisListType

NDMA_SEM = 6
EPOCH = 100000
NEPOCH = 4


class Prog:
    def __init__(self, nc, stack):
        self.nc = nc
        self.stack = stack
        self.names = ['pe', 'act', 'dve', 'pool', 'sp']
        self.ops = {k: [] for k in self.names}
        self.cnt = {k: 0 for k in self.names}
        self.dcnt = {k: 0 for k in self.names}
        self.csem = {(k, ep): stack.enter_context(nc.semaphore("c_%s%d" % (k, ep)))
                     for k in self.names for ep in range(NEPOCH if k == 'pe' else 1)}
        self.dsem = {k: [stack.enter_context(nc.semaphore("d_%s%d" % (k, i))) for i in range(NDMA_SEM)]
                     for k in ('sp', 'pool', 'act')}
        self.seen = {k: {} for k in self.names}
        self.lastw = {}
        self.readers = {}
        self.ntile = 0

    def sb(self, shape, dt, name=None):
        self.ntile += 1
        return self.stack.enter_context(self.nc.sbuf_tensor("%s_u%d" % (name or "t", self.ntile), list(shape), dt))

    def ps(self, shape, dt, name=None):
        self.ntile += 1
        return self.stack.enter_context(self.nc.psum_tensor("%s_u%d" % (name or "p", self.ntile), list(shape), dt))

    def _need(self, eng, ev, waits):
        semkey, val, peng, isdma = ev
        if (not isdma) and peng == eng and eng == 'pe':
            return
        if self.seen[eng].get(semkey, 0) >= val:
            return
        self.seen[eng][semkey] = val
        waits.append((semkey, val))

    def op(self, eng, fn, reads=(), writes=(), dma=False):
        waits = []
        for b in reads:
            if b in self.lastw:
                self._need(eng, self.lastw[b], waits)
        for b in writes:
            if b in self.lastw:
                self._need(eng, self.lastw[b], waits)
            for ev in self.readers.get(b, ()):
                self._need(eng, ev, waits)
        if dma:
            i = self.dcnt[eng]
            self.dcnt[eng] += 1
            j = i % NDMA_SEM
            tgt = 16 * (i // NDMA_SEM + 1)
            semkey = ('d', eng, j)
            if tgt > 16:
                self._need(eng, (semkey, tgt - 16, eng, True), waits)
            ev = (semkey, tgt, eng, True)
            inc = (semkey, 16)
        else:
            self.cnt[eng] += 1
            semkey, val = self.ckey(eng, self.cnt[eng])
            ev = (semkey, val, eng, False)
            inc = (semkey, 1)
        for b in reads:
            self.readers.setdefault(b, []).append(ev)
        for b in writes:
            self.lastw[b] = ev
            self.readers[b] = []
        self.ops[eng].append((fn, waits, inc))
        return ev

    def ckey(self, eng, n):
        if eng != 'pe':
            return ('c', eng, 0), n
        return ('c', eng, (n - 1) // EPOCH), (n - 1) % EPOCH + 1

    def fence(self):
        allev = []
        for k in self.names:
            if self.cnt[k]:
                sk, v = self.ckey(k, self.cnt[k])
                allev.append((sk, v, k, False))
        for k in ('sp', 'pool', 'act'):
            for j in range(NDMA_SEM):
                n = (self.dcnt[k] - j + NDMA_SEM - 1) // NDMA_SEM
                if n > 0:
                    allev.append((('d', k, j), 16 * n, k, True))
        for eng in self.names:
            waits = []
            for semkey, val, k, isdma in allev:
                if self.seen[eng].get(semkey, 0) >= val:
                    continue
                if (not isdma) and k == eng:
                    continue
                self.seen[eng][semkey] = val
                waits.append((semkey, val))
            self.cnt[eng] += 1
            sk, v = self.ckey(eng, self.cnt[eng])
            self.ops[eng].append((lambda e: e.nop(), waits, (sk, 1)))

    def _sem(self, semkey):
        if semkey[0] == 'c':
            return self.csem[(semkey[1], semkey[2])]
        return self.dsem[semkey[1]][semkey[2]]

    def finish(self):
        waits = []
        for k in self.names:
            if self.cnt[k]:
                waits.append(self.ckey(k, self.cnt[k]))
        for k in ('sp', 'pool', 'act'):
            for j in range(NDMA_SEM):
                n = (self.dcnt[k] - j + NDMA_SEM - 1) // NDMA_SEM
                if n > 0:
                    waits.append((('d', k, j), 16 * n))
        self.final_waits = waits

    def emit(self):
        nc = self.nc
        self.finish()
        with nc.Block() as block:
            def run(name):
                def f(e):
                    for fn, waits, inc in self.ops[name]:
                        for semkey, val in waits:
                            e.wait_ge(self._sem(semkey), val)
                        ins = fn(e)
                        ins.then_inc(self._sem(inc[0]), inc[1])
                    if name == 'sp':
                        for semkey, val in self.final_waits:
                            e.wait_ge(self._sem(semkey), val)
                return f
            block.tensor(run('pe'))
            block.scalar(run('act'))
            block.vector(run('dve'))
            block.gpsimd(run('pool'))
            block.sync(run('sp'))

    def dma(self, q, out, in_, reads=(), writes=(), slow=False):
        if slow:
            return self.op(q, lambda e: e.dma_start(out=out, in_=in_, allow_slow_non_contiguous=True), reads, writes, dma=True)
        return self.op(q, lambda e: e.dma_start(out=out, in_=in_), reads, writes, dma=True)

    def mm(self, out, lhsT, rhs, start, stop, reads=(), writes=()):
        return self.op('pe', lambda e: e.matmul(out, lhsT=lhsT, rhs=rhs, start=start, stop=stop), reads, writes)

    def transpose(self, out, in_, ident, reads=(), writes=()):
        return self.op('pe', lambda e: e.transpose(out, in_, ident), reads, writes)

    def act(self, out, in_, func, reads=(), writes=(), **kw):
        return self.op('act', lambda e: e.activation(out, in_, func, **kw), reads, writes)

    def tt(self, eng, out, in0, in1, op, reads=(), writes=()):
        return self.op(eng, lambda e: e.tensor_tensor(out, in0, in1, op), reads, writes)

    def ts(self, eng, out, in0, s1, s2, op0, op1=None, reads=(), writes=(), **kw):
        if op1 is None:
            return self.op(eng, lambda e: e.tensor_scalar(out, in0, s1, s2, op0, **kw), reads, writes)
        return self.op(eng, lambda e: e.tensor_scalar(out, in0, s1, s2, op0, op1, **kw), reads, writes)

    def stt(self, eng, out, in0, scalar, in1, op0, op1, reads=(), writes=()):
        return self.op(eng, lambda e: e.scalar_tensor_tensor(out, in0, scalar, in1, op0, op1), reads, writes)

    def copy(self, eng, out, in_, reads=(), writes=()):
        if eng == 'act':
            return self.op(eng, lambda e: e.copy(out, in_), reads, writes)
        return self.op(eng, lambda e: e.tensor_copy(out, in_), reads, writes)

    def memset(self, eng, ap, val, writes=()):
        return self.op(eng, lambda e: e.memset(ap, val), (), writes)

import math, contextlib


D = 2048
EPS = 1e-6
LN_THETA = math.log(10000.0)
PI = math.pi


class Ctx:
    pass


def setup_common(P, NT):
    c = Ctx()
    c.ones = P.sb([128, 128], BF16, "ones")
    P.memset('pool', c.ones[:], 1.0, writes=['ones'])
    c.psn = 0
    c.pst = [P.ps([128, 512], F32, "psb%d" % i) for i in range(8)]
    c.NT = NT
    return c


def next_ps(c):
    i = c.psn % 8
    c.psn += 1
    return c.pst[i], "psb%d" % i


def rms_rstd(P, c, src, skey, KC, Dn, out_rstd, okey, tmpname):
    NT = c.NT
    ps, pk = next_ps(c)
    for kc in range(KC):
        sq = c.sq[kc % 2]
        sk = "sq%d" % (kc % 2)
        P.act(sq[:], src[:, kc, :], AF.Square, reads=[skey], writes=[sk])
        P.mm(ps[:, 0:NT], c.ones[:], sq[:], kc == 0, kc == KC - 1, reads=['ones', sk], writes=[pk])
    P.ts('dve', out_rstd[:], ps[:, 0:NT], 1.0 / Dn, EPS, ALU.mult, ALU.add, reads=[pk], writes=[okey])
    P.act(out_rstd[:], out_rstd[:], AF.Sqrt, reads=[okey], writes=[okey])
    P.op('dve', lambda e: e.reciprocal(out_rstd[:], out_rstd[:]), reads=[okey], writes=[okey])


def ridx_np():
    p = np.arange(128)
    r = np.zeros((128, 4), np.float32)
    r[:, 0] = p % 64
    r[:, 1] = np.where(p < 64, -1.0, 1.0)
    r[:, 2] = p % 32
    r[:, 3] = np.where((p % 64) < 32, -1.0, 1.0)
    return r

def make_mask(P, tile, key, QW, base, cm, step, tmp_i, tmp_key):
    P.op('pool', lambda e: e.iota(tmp_i[:, 0:QW], [[step, QW]], base=base, channel_multiplier=cm), (), [tmp_key])
    P.op('dve', lambda e: e.tensor_single_scalar(tile[:, 0:QW], tmp_i[:, 0:QW], 0, ALU.is_ge), [tmp_key], [key])


import math, contextlib


class Geo:
    def __init__(self, L):
        self.L = L
        self.OWN = L // 4
        self.OWN0 = L - self.OWN
        self.HAL0 = self.OWN0 - 2048
        self.QN = L - self.HAL0
        assert self.HAL0 >= 0 and self.HAL0 % 512 == 0 and self.OWN % 2048 == 0


class Phase:
    def __init__(self, P):
        self.P = P

    def __enter__(self):
        self.inner = contextlib.ExitStack()
        self.saved = self.P.stack
        self.P.stack = self.inner
        return self

    def __exit__(self, *a):
        self.P.fence()
        self.inner.close()
        self.P.stack = self.saved
        return False


def rope_tables_emit(P, posb, rc_col, sgn_ap, ang, rr, ri, cosT, sinT, keys):
    P.ts('dve', ang[:], posb[:], rc_col, None, ALU.mult, reads=[keys or 'posb', 'rc'], writes=['ang'])
    for (shift, outT, scale) in ((0.0, sinT, sgn_ap), (0.5 * PI, cosT, 1.0)):
        P.ts('dve', rr[:], ang[:], 1.0 / (2 * PI), shift / (2 * PI), ALU.mult, ALU.add, reads=['ang'], writes=['rr'])
        P.copy('dve', ri[:], rr[:], reads=['rr'], writes=['ri'])
        P.copy('dve', rr[:], ri[:], reads=['ri'], writes=['rr'])
        P.stt('dve', rr[:], rr[:], -2 * PI, ang[:], ALU.mult, ALU.add, reads=['rr', 'ang'], writes=['rr'])
        P.ts('dve', rr[:], rr[:], shift, PI, ALU.add, ALU.min, reads=['rr'], writes=['rr'])
        P.ts('dve', rr[:], rr[:], -PI, None, ALU.max, reads=['rr'], writes=['rr'])
        P.act(outT[:], rr[:], AF.Sin, reads=['rr', 'rsb'], writes=['tables'], scale=scale)


def make_rperm(P, tmp_i, tmp_key):
    r128 = P.sb([128, 128], BF16, "rperm128")
    r64 = P.sb([128, 128], BF16, "rperm64")
    ta = P.sb([128, 128], BF16, "rperm_t")
    P.op('pool', lambda e: e.iota(tmp_i[:, 0:128], [[-1, 128]], base=0, channel_multiplier=1), (), [tmp_key])
    for r, off in ((r128, 64), (r64, 32)):
        P.op('dve', lambda e, r=r, off=off: e.tensor_single_scalar(r[:], tmp_i[:, 0:128], off, ALU.is_equal), [tmp_key], ['rperm'])
        P.op('dve', lambda e, off=off: e.tensor_single_scalar(ta[:], tmp_i[:, 0:128], -off, ALU.is_equal), [tmp_key], ['rperm_t'])
        P.tt('dve', r[:], r[:], ta[:], ALU.add, reads=['rperm', 'rperm_t'], writes=['rperm'])
    return r128, r64

def phase1(P, nc, G, io):
    NT = 256
    EIN = 3416
    L, HAL0 = G.L, G.HAL0
    xT, pos = io['xT'], io['pos']
    with Phase(P):
        c = setup_common(P, NT)
        c.sq = [P.sb([128, NT], BF16, "sq%d" % i) for i in range(2)]
        wsb = P.sb([128, 16, EIN], BF16, "wsb")
        wuq = P.sb([128, 4, 1536], BF16, "wuq")
        wukv = P.sb([128, 2, 2048], BF16, "wukv")
        for kc in range(16):
            P.dma('pool', wsb[:, kc, :], io['l0_w_in'][kc * 128:(kc + 1) * 128, :], writes=['wsb'])
        for kc in range(4):
            P.dma('pool', wuq[:, kc, :], io['l0_mla_w_uq'][kc * 128:(kc + 1) * 128, :], writes=['wuq'])
        for kc in range(2):
            P.dma('pool', wukv[:, kc, :], io['l0_mla_w_ukv'][kc * 128:(kc + 1) * 128, :], writes=['wukv'])
        gsb = P.sb([128, 16], F32, "gsb")
        qnsb = P.sb([128, 4], F32, "qnsb")
        kvnsb = P.sb([128, 2], F32, "kvnsb")
        rsb = P.sb([128, 4], F32, "rsb")
        P.dma('sp', gsb[:], io['l0_norm_mix_pre'].rearrange("(kc p) -> p kc", p=128), writes=['gsb'], slow=True)
        P.dma('sp', qnsb[:], io['l0_mla_q_norm'].rearrange("(kc p) -> p kc", p=128), writes=['qnsb'], slow=True)
        P.dma('sp', kvnsb[:], io['l0_mla_kv_norm'].rearrange("(kc p) -> p kc", p=128), writes=['kvnsb'], slow=True)
        P.dma('sp', rsb[:], io['ridx'][:, :], writes=['rsb'])
        rc = P.sb([128, 2], F32, "rc")
        P.act(rc[:, 0:1], rsb[:, 0:1], AF.Exp, reads=['rsb'], writes=['rc'], scale=-2.0 * LN_THETA / 128.0)
        P.act(rc[:, 1:2], rsb[:, 2:3], AF.Exp, reads=['rsb'], writes=['rc'], scale=-2.0 * LN_THETA / 64.0)
        xsb = [P.sb([128, 16, NT], F32, "xs%d" % i) for i in range(2)]
        us = P.sb([128, 16, NT], BF16, "us")
        rstd = P.sb([128, NT], F32, "rstd")
        posbb = [P.sb([128, NT], F32, "posb%d" % i) for i in range(2)]
        ang = P.sb([128, NT], F32, "ang")
        rr = P.sb([128, NT], F32, "rr")
        ri = P.sb([128, NT], I32, "ri")
        cos128 = P.sb([128, NT], F32, "cos128")
        sin128 = P.sb([128, NT], F32, "sin128")
        cos64 = P.sb([128, NT], F32, "cos64")
        sin64 = P.sb([128, NT], F32, "sin64")
        cq = P.sb([128, 4, NT], F32, "cq")
        ckv = P.sb([128, 2, NT], F32, "ckv")
        cqn = P.sb([128, 4, NT], BF16, "cqn")
        ckvn = P.sb([128, 2, NT], BF16, "ckvn")
        t1 = [P.sb([128, NT], F32, "t1_%d" % i) for i in range(2)]
        t2 = [P.sb([128, NT], F32, "t2_%d" % i) for i in range(2)]
        ob = [P.sb([128, 512], BF16, "ob%d" % i) for i in range(4)]
        og = P.sb([128, 24], F32, "og")
        zb = [P.sb([128, NT], BF16, "zb%d" % i) for i in range(2)]
        tmp_i = P.sb([128, 128], I32, "tmp_i")
        r128, r64 = make_rperm(P, tmp_i, "tmp_i")
        state = {'ob': 0, 'tt': 0}

        def out_tile():
            i = state['ob'] % 4
            state['ob'] += 1
            return ob[i], "ob%d" % i

        def linear(wt, wkey, act, akey, KC, col0, ncols, prow0=0, ps=None, pk=None):
            if ps is None:
                ps, pk = next_ps(c)
            for kc in range(KC):
                P.mm(ps[prow0:prow0 + ncols, 0:NT], wt[:, kc, col0:col0 + ncols], act[:, kc, :],
                     kc == 0, kc == KC - 1, reads=[wkey, akey], writes=[pk])
            return ps, pk

        def tm_linear(wt, wkey, act, akey, KC, tb, col0, ncols):
            ps, pk = next_ps(c)
            for kc in range(KC):
                P.mm(ps[:, 0:ncols], act[:, kc, tb * 128:(tb + 1) * 128], wt[:, kc, col0:col0 + ncols],
                     kc == 0, kc == KC - 1, reads=[wkey, akey], writes=[pk])
            return ps, pk

        def rope_lin(wt, wkey, act, akey, KC, col0, ncols, cosT, sinT, dst):
            psz, kz = linear(wt, wkey, act, akey, KC, col0, ncols)
            i = state['tt'] % 2
            state['tt'] += 1
            zb_, zk = zb[i], "zb%d" % i
            P.copy('act', zb_[0:ncols, :], psz[0:ncols, 0:NT], reads=[kz], writes=[zk])
            psr, kr = next_ps(c)
            R = r128 if ncols == 128 else r64
            P.mm(psr[0:ncols, 0:NT], R[0:ncols, 0:ncols], zb_[0:ncols, :], True, True, reads=['rperm', zk], writes=[kr])
            a, b = t1[i], t2[i]
            P.tt('dve', a[0:ncols, :], zb_[0:ncols, :], cosT[0:ncols, :], ALU.mult, reads=[zk, 'tables'], writes=["t1_%d" % i])
            P.tt('dve', b[0:ncols, :], psr[0:ncols, 0:NT], sinT[0:ncols, :], ALU.mult, reads=[kr, 'tables'], writes=["t2_%d" % i])
            o, ok = out_tile()
            P.tt('pool', o[0:ncols, 0:NT], a[0:ncols, :], b[0:ncols, :], ALU.add, reads=["t1_%d" % i, "t2_%d" % i], writes=[ok])
            P.dma('sp', dst, o[0:ncols, 0:NT], reads=[ok])

        def plain_lin(wt, wkey, act, akey, KC, col0, ncols, dst):
            psz, kz = linear(wt, wkey, act, akey, KC, col0, ncols)
            o, ok = out_tile()
            P.copy('act', o[0:ncols, 0:NT], psz[0:ncols, 0:NT], reads=[kz], writes=[ok])
            P.dma('sp', dst, o[0:ncols, 0:NT], reads=[ok])

        o1, o2, o3, o4, o5 = 1024, 2560, 2584, 3096, 3352

        def prefetch(sbi):
            if sbi * NT >= L:
                return
            b = sbi % 2
            sl_ = slice(sbi * NT, (sbi + 1) * NT)
            for kc in range(16):
                P.dma('pool' if kc % 2 == 0 else 'act', xsb[b][:, kc, :], xT[kc * 128:(kc + 1) * 128, sl_], writes=['xs%d' % b])
            P.dma('pool', posbb[b][:], pos[0:1, sl_].to_broadcast([128, NT]), writes=['posb%d' % b])

        prefetch(0)
        for s0 in range(0, L, NT):
            full = s0 >= HAL0
            sl = slice(s0, s0 + NT)
            ql = slice(s0 - HAL0, s0 - HAL0 + NT)
            xs, xk = xsb[(s0 // NT) % 2], 'xs%d' % ((s0 // NT) % 2)
            posb, pbk = posbb[(s0 // NT) % 2], 'posb%d' % ((s0 // NT) % 2)
            prefetch(s0 // NT + 1)
            rms_rstd(P, c, xs, xk, 16, D, rstd, 'rstd', 'sq')
            for kc in range(16):
                P.stt('dve', us[:, kc, :], xs[:, kc, :], gsb[:, kc:kc + 1], rstd[:], ALU.mult, ALU.mult,
                      reads=[xk, 'gsb', 'rstd'], writes=['us'])
            for i in range(2):
                psz, kz = linear(wsb, 'wsb', us, 'us', 16, o4 + i * 128, 128)
                P.copy('act', ckv[:, i, :], psz[:, 0:NT], reads=[kz], writes=['ckv'])
            if full:
                for i in range(4):
                    psz, kz = linear(wsb, 'wsb', us, 'us', 16, o3 + i * 128, 128)
                    P.copy('act', cq[:, i, :], psz[:, 0:NT], reads=[kz], writes=['cq'])
            for g in range(2):
                plain_lin(wsb, 'wsb', us, 'us', 16, o1 + (2 + g) * 128, 128, io['KFM'][(6 + g) * 128:(7 + g) * 128, sl])
            for br, dst in ((1, io['VS']), (2, io['VW'])):
                for tb in range(NT // 128):
                    ps, pk = tm_linear(wsb, 'wsb', us, 'us', 16, tb, o1 + (br * 4 + 2) * 128, 256)
                    o, ok = out_tile()
                    P.copy('act', o[:, 0:256], ps[:, 0:256], reads=[pk], writes=[ok])
                    P.dma('sp', dst[s0 + tb * 128:s0 + (tb + 1) * 128, :], o[:, 0:256], reads=[ok])
            if full:
                for tb in range(NT // 128):
                    ps, pk = tm_linear(wsb, 'wsb', us, 'us', 16, tb, o2, 24)
                    P.act(og[:, 0:24], ps[:, 0:24], AF.Sigmoid, reads=[pk], writes=['og'])
                    P.dma('sp', io['GATE'][s0 - HAL0 + tb * 128:s0 - HAL0 + (tb + 1) * 128, :], og[:, 0:24], reads=['og'])
            rms_rstd(P, c, ckv, 'ckv', 2, 256, rstd, 'rstd', 'sq')
            for kc in range(2):
                P.stt('dve', ckvn[:, kc, :], ckv[:, kc, :], kvnsb[:, kc:kc + 1], rstd[:], ALU.mult, ALU.mult,
                      reads=['ckv', 'kvnsb', 'rstd'], writes=['ckvn'])
            if full:
                rms_rstd(P, c, cq, 'cq', 4, 512, rstd, 'rstd', 'sq')
                for kc in range(4):
                    P.stt('dve', cqn[:, kc, :], cq[:, kc, :], qnsb[:, kc:kc + 1], rstd[:], ALU.mult, ALU.mult,
                          reads=['cq', 'qnsb', 'rstd'], writes=['cqn'])
            rope_tables_emit(P, posb, rc[:, 0:1], rsb[:, 1:2], ang, rr, ri, cos128, sin128, pbk)
            rope_tables_emit(P, posb, rc[:, 1:2], rsb[:, 3:4], ang, rr, ri, cos64, sin64, pbk)
            for h in range(8):
                plain_lin(wukv, 'wukv', ckvn, 'ckvn', 2, h * 256, 128, io['KMN'][h * 128:(h + 1) * 128, sl])
            for tb in range(NT // 128):
                for h0 in (0, 4):
                    ps, pk = next_ps(c)
                    for hh in range(4):
                        for kc in range(2):
                            P.mm(ps[:, hh * 128:(hh + 1) * 128], ckvn[:, kc, tb * 128:(tb + 1) * 128],
                                 wukv[:, kc, (h0 + hh) * 256 + 128:(h0 + hh) * 256 + 256], kc == 0, kc == 1,
                                 reads=['wukv', 'ckvn'], writes=[pk])
                    o, ok = out_tile()
                    P.copy('act', o[:, 0:512], ps[:, 0:512], reads=[pk], writes=[ok])
                    P.dma('sp', io['VM'][s0 + tb * 128:s0 + (tb + 1) * 128, h0 * 128:(h0 + 4) * 128], o[:, 0:512], reads=[ok])
            if full:
                for h in range(8):
                    plain_lin(wuq, 'wuq', cqn, 'cqn', 4, h * 192, 128, io['QMN'][h * 128:(h + 1) * 128, ql])
            for br in range(3):
                for g in range(2):
                    ch = br * 4 + g
                    rope_lin(wsb, 'wsb', us, 'us', 16, o1 + ch * 128, 128, cos128, sin128,
                             io['KFM'][(br * 2 + g) * 128:(br * 2 + g + 1) * 128, sl])
            rope_lin(wsb, 'wsb', us, 'us', 16, o5, 64, cos64, sin64, io['KPE'][:, sl])
            if full:
                for h in range(8):
                    rope_lin(wsb, 'wsb', us, 'us', 16, h * 128, 128, cos128, sin128, io['QA'][h * 128:(h + 1) * 128, ql])
                for h in range(8):
                    rope_lin(wuq, 'wuq', cqn, 'cqn', 4, h * 192 + 128, 64, cos64, sin64, io['QMP'][h * 64:(h + 1) * 64, ql])


def load_vaug(P, vaug, vkey, src_rows_fn, NKC, posv, q='sp'):
    for c0 in range(0, NKC, 32):
        c1 = min(NKC, c0 + 32)
        P.dma(q, vaug[:, c0:c1, 0:128], src_rows_fn(c0, c1), writes=[vkey])
    P.op('dve', lambda e: e.tensor_single_scalar(vaug[:, :, 128:129], posv, 0.0, ALU.is_ge), ['postm'], [vkey])


BIG = 1.0e4


def attn_consts(P, tmp_i):
    cst = {}
    for name, (base, cm, step) in (("m_diag", (0, -1, 1)), ("m_strict", (-1, 1, -1)), ("m_ge", (0, 1, -1))):
        t = P.sb([128, 128], BF16, name)
        make_mask(P, t, name, 128, base, cm, step, tmp_i, "tmp_i")
        cst[name] = t
    ident = P.sb([128, 128], BF16, "ident")
    P.tt('dve', ident[:], cst["m_diag"][:], cst["m_ge"][:], ALU.mult, reads=['m_diag', 'm_ge'], writes=['ident'])
    cst["ident"] = ident
    for dd in range(4):
        t = P.sb([128, 512], BF16, "dm%d" % dd)
        make_mask(P, t, "dm%d" % dd, 512, -128 * dd, -1, 1, tmp_i, "tmp_i")
        cst["dm%d" % dd] = t
    return cst


def emit_oT(P, cst, o_f32, okey, obf, obk, pst, osb, osk, dst):
    P.copy('act', obf[:], o_f32, reads=[okey], writes=[obk])
    P.transpose(pst[:, 512:640], obf[:], cst["ident"][:], reads=[obk, 'ident'], writes=['pst'])
    P.copy('dve', osb[:], pst[:, 512:640], reads=['pst'], writes=[osk])
    P.dma('sp', dst, osb[:], reads=[osk])


def phase2(P, nc, G, io):
    L, HAL0, QN = G.L, G.HAL0, G.QN
    NCMP = (L - 32) // 16 + 1
    NCC = (NCMP + 127) // 128
    NSLC = L // 64
    NJC = (NSLC + 127) // 128
    NKC = L // 128
    scale = 128 ** -0.5
    with Phase(P):
        tmp_i = P.sb([128, 512], I32, "tmp_i")
        cst = attn_consts(P, tmp_i)
        mdiag, mstrict, ident = cst["m_diag"], cst["m_strict"], cst["ident"]
        cmask = []
        for m in range(17):
            t = P.sb([128, 128], BF16, "cm%d" % m)
            make_mask(P, t, "cm%d" % m, 128, 128 * m - 31, -16, 1, tmp_i, "tmp_i")
            cmask.append(t)
        Emat = P.sb([128, 64, 128], BF16, "Emat")
        for half in range(2):
            src = ident[:].rearrange("p (m h) -> p m h", h=2)[:, :, half:half + 1]
            P.copy('dve', Emat[:, :, half * 64:(half + 1) * 64], src.to_broadcast([128, 64, 64]), reads=['ident'], writes=['Emat'])
        fpB = P.sb([128, 3], F32, "fpB")
        P.memset('dve', fpB[:], 0.0, writes=['fpB'])
        P.memset('dve', fpB[0:64, 0:1], BIG, writes=['fpB'])
        P.memset('dve', fpB[:, 1:2], BIG, writes=['fpB'])
        P.memset('dve', fpB[64:128, 2:3], BIG, writes=['fpB'])
        f0B = P.sb([128, NSLC], F32, "f0B")
        P.dma('sp', f0B[:], io['f0'][0:1, :].to_broadcast([128, NSLC]), writes=['f0B'])
        P.ts('dve', f0B[:], f0B[:], BIG, None, ALU.mult, reads=['f0B'], writes=['f0B'])
        cval = P.sb([128, NCC], F32, "cval")
        P.dma('sp', cval[:], io['cpos'][:, :], writes=['cval'])
        P.ts('dve', cval[:], cval[:], 0.0, None, ALU.is_ge, reads=['cval'], writes=['cval'])
        postm = P.sb([128, NKC], F32, "postm")
        P.dma('sp', postm[:], io['postm'][:, :], writes=['postm'])
        pa = [P.ps([128, 512], F32, "pa%d" % i) for i in range(4)]
        pss = [P.ps([128, 512], F32, "pss%d" % i) for i in range(2)]
        pst = P.ps([128, 1024], BF16, "pst")
        kcmpT = P.sb([128, NCC * 128], BF16, "kcmpT")
        VAc = P.sb([128, NCC, 385], BF16, "VAc")
        am = P.sb([128, 256], BF16, "am")
        for g in range(2):
            P.memset('pool', kcmpT[:], 0.0, writes=['kcmpT'])
            P.memset('pool', VAc[:], 0.0, writes=['VAc'])
            P.memset('pool', VAc[:, :, 128:129], 1.0, writes=['VAc'])
            for c in range(NCC):
                P.op('pool', lambda e, c=c: e.iota(tmp_i[:, 0:256], [[-4, 256]], base=128 * c + 1, channel_multiplier=1), (), ['tmp_i'])
                P.op('dve', lambda e: e.tensor_single_scalar(am[:], tmp_i[:, 0:256], 0, ALU.is_ge), ['tmp_i'], ['am'])
                P.op('pool', lambda e, c=c: e.iota(tmp_i[:, 0:256], [[4, 256]], base=3 - 128 * c, channel_multiplier=-1), (), ['tmp_i'])
                P.op('dve', lambda e, c=c: e.tensor_single_scalar(VAc[:, c, 129:385], tmp_i[:, 0:256], 0, ALU.is_ge), ['tmp_i'], ['VAc'])
                P.tt('dve', VAc[:, c, 129:385], VAc[:, c, 129:385], am[:], ALU.mult, reads=['VAc', 'am'], writes=['VAc'])
            with Phase(P):
                psx = P.ps([128, 512], F32, "psx")
                srcs = P.sb([128, L], BF16, "srcs")
                w1s = P.sb([128, 32, 128], BF16, "w1s")
                w2s = P.sb([128, 128], BF16, "w2s")
                peT = P.sb([128, 32], BF16, "peT")
                cb = P.sb([128, 1], F32, "cb")
                xs_ = P.sb([128, 512], F32, "cx")
                x2 = P.sb([128, 512], F32, "cx2")
                gl = P.sb([128, NCC * 128], BF16, "gl")
                for which in range(2):
                    w1, w2, pe, srow = ((io['l0_cmp_w1_k'], io['l0_cmp_w2_k'], io['l0_cmp_pe_k'], g),
                                        (io['l0_cmp_w1_v'], io['l0_cmp_w2_v'], io['l0_cmp_pe_v'], 6 + g))[which]
                    P.dma('sp', srcs[:], io['KFM'][srow * 128:(srow + 1) * 128, :], writes=['srcs'])
                    P.dma('pool', w1s[:], w1.rearrange("(l d) h -> d l h", d=128), writes=['w1s'])
                    P.dma('pool', w2s[:], w2[:, :], writes=['w2s'])
                    P.dma('pool', peT[:], pe.rearrange("l d -> d l"), writes=['peT'], slow=True)
                    for l in range(32):
                        P.mm(psx[:, 0:1], w1s[:, l, :], peT[:, l:l + 1], l == 0, l == 31, reads=['w1s', 'peT'], writes=['psx'])
                    P.copy('dve', cb[:], psx[:, 0:1], reads=['psx'], writes=['cb'])
                    P.memset('pool', gl[:], 0.0, writes=['gl'])
                    src3 = srcs[:].rearrange("p (n r) -> p n r", r=16)
                    for n0 in range(0, NCMP, 512):
                        ncols = min(512, NCMP - n0)
                        for l in range(32):
                            P.mm(psx[:, 0:ncols], w1s[:, l, :], src3[:, n0 + l // 16:n0 + l // 16 + ncols, l % 16],
                                 l == 0, l == 31, reads=['w1s', 'srcs'], writes=['psx'])
                        xa = xs_[:, 0:ncols]
                        xb = x2[:, 0:ncols]
                        P.act(xa, psx[:, 0:ncols], AF.Identity, reads=['psx', 'cb'], writes=['cx'], bias=cb[:, 0:1], scale=1.0)
                        P.tt('dve', xb, xa, xa, ALU.mult, reads=['cx'], writes=['cx2'])
                        P.ts('dve', xb, xb, 0.044715, 1.0, ALU.mult, ALU.add, reads=['cx2'], writes=['cx2'])
                        P.tt('dve', xb, xb, xa, ALU.mult, reads=['cx', 'cx2'], writes=['cx2'])
                        P.act(xb, xb, AF.Tanh, reads=['cx2'], writes=['cx2'], scale=math.sqrt(2.0 / math.pi))
                        P.ts('dve', xb, xb, 0.5, 0.5, ALU.mult, ALU.add, reads=['cx2'], writes=['cx2'])
                        P.tt('dve', gl[:, n0:n0 + ncols], xb, xa, ALU.mult, reads=['cx', 'cx2'], writes=['gl'])
                    if which == 0:
                        for n0 in range(0, NCMP, 512):
                            ncols = min(512, NCMP - n0)
                            P.mm(psx[:, 0:ncols], w2s[:], gl[:, n0:n0 + ncols], True, True, reads=['w2s', 'gl'], writes=['psx'])
                            P.copy('dve', kcmpT[:, n0:n0 + ncols], psx[:, 0:ncols], reads=['psx'], writes=['kcmpT'])
                    else:
                        for c in range(NCC):
                            P.mm(psx[:, 0:128], gl[:, c * 128:(c + 1) * 128], w2s[:], True, True, reads=['w2s', 'gl'], writes=['psx'])
                            P.copy('dve', VAc[:, c, 0:128], psx[:, 0:128], reads=['psx'], writes=['VAc'])
            for c in range(NCC):
                P.ts('dve', VAc[:, c, :], VAc[:, c, :], cval[:, c:c + 1], None, ALU.mult, reads=['VAc', 'cval'], writes=['VAc'])
            with Phase(P):
                pss3 = pss + [P.ps([128, 512], F32, "pss2")]
                kss = P.sb([128, L], BF16, "kss")
                kws = P.sb([128, L], BF16, "kws")
                P.dma('sp', kss[:], io['KFM'][(2 + g) * 128:(3 + g) * 128, :], writes=['kss'])
                P.dma('act', kws[:], io['KFM'][(4 + g) * 128:(5 + g) * 128, :], writes=['kws'])
                vsa = P.sb([128, NKC, 129], BF16, "vsa")
                vwa = P.sb([128, NKC, 129], BF16, "vwa")
                load_vaug(P, vsa, 'vsa', lambda c0, c1: io['VS'][c0 * 128:c1 * 128, g * 128:(g + 1) * 128].rearrange("(c p) d -> p c d", p=128), NKC, postm[:, :].rearrange("p (c o) -> p c o", o=1), 'sp')
                load_vaug(P, vwa, 'vwa', lambda c0, c1: io['VW'][c0 * 128:c1 * 128, g * 128:(g + 1) * 128].rearrange("(c p) d -> p c d", p=128), NKC, postm[:, :].rearrange("p (c o) -> p c o", o=1), 'act')
                gsb = P.sb([128, QN // 128, 12], F32, "gsb")
                P.dma('sp', gsb[:], io['GATE'][:, 12 * g:12 * g + 12].rearrange("(c p) g -> p c g", p=128), writes=['gsb'])
                qblk = [P.sb([128, 4, 512], BF16, "qblk%d" % i) for i in range(2)]
                pt = [P.sb([128, 512], BF16, "pt%d" % i) for i in range(5)]
                rden = P.sb([128, 8], F32, "rden")
                imp = P.sb([128, NSLC], F32, "imp")
                work = P.sb([128, NSLC], F32, "work")
                m8 = P.sb([128, 16], F32, "m8")
                thr = P.sb([128, 1], F32, "thr")
                sel = P.sb([128, NJC * 128], BF16, "sel")
                P.memset('pool', sel[:], 0.0, writes=['sel'])
                biasT = P.sb([128, NJC, 512], BF16, "biasT")
                P.memset('pool', biasT[:], 0.0, writes=['biasT'])
                oc = [P.sb([128, 128], F32, "oc%d" % h) for h in range(4)]
                opart = [[P.sb([128, 128], F32, "op%d_%d" % (h, qs)) for qs in range(4)] for h in range(4)]
                ofin = [P.sb([128, 128], F32, "ofin%d" % i) for i in range(2)]
                obf = [P.sb([128, 128], BF16, "obf%d" % i) for i in range(2)]
                osb = [P.sb([128, 128], BF16, "osb%d" % i) for i in range(2)]
                rg = P.sb([128, 8], F32, "rg")
                cnt = {'s': 0, 'pt': 0, 'o': 0}

                def score_exp(klhs, kkey, kj, qh, qkey, qoff, W, mask, mkey, bias=False):
                    si = cnt['s'] % 3
                    cnt['s'] += 1
                    ps, pk = pss3[si], "pss%d" % si
                    P.mm(ps[:, 0:W], klhs[:, kj * 128:(kj + 1) * 128], qh[:, qoff:qoff + W], True, not bias,
                         reads=[kkey, qkey], writes=[pk])
                    if bias:
                        P.mm(ps[:, 0:W], Emat[:, kj % 64, :], biasT[:, kj // 64, 0:W], False, True,
                             reads=['Emat', 'biasT'], writes=[pk])
                    pi = cnt['pt'] % 5
                    cnt['pt'] += 1
                    p_, ptk = pt[pi], "pt%d" % pi
                    P.act(p_[:, 0:W], ps[:, 0:W], AF.Exp, reads=[pk], writes=[ptk], scale=scale)
                    if mask is not None:
                        P.tt('dve', p_[:, 0:W], p_[:, 0:W], mask[:, 0:W], ALU.mult, reads=[ptk, mkey], writes=[ptk])
                    return p_, ptk

                for stile in range(HAL0 // 512, L // 512):
                    qb, qbk = qblk[stile % 2], "qblk%d" % (stile % 2)
                    for h in range(4):
                        P.dma('sp' if h % 2 == 0 else 'act', qb[:, h, :],
                              io['QA'][(4 * g + h) * 128:(4 * g + h + 1) * 128, stile * 512 - HAL0:stile * 512 - HAL0 + 512], writes=[qbk])
                    def emit_pvw(p_, ptk, h, kj, first, last):
                        P.mm(pa[h][:, 0:129], p_[:, 0:128], vwa[:, kj, :], first, last, reads=[ptk, 'vwa'], writes=['pa%d' % h])
                    def emit_pva(p_, ptk, h, c, first, last):
                        P.mm(pa[h][:, 0:385], p_[:, 0:128], VAc[:, c, :], first, last, reads=[ptk, 'VAc'], writes=['pa%d' % h])
                    for qs in range(4):
                        i = 4 * stile + qs
                        q0 = 128 * i
                        gi = (q0 - HAL0) // 128
                        qoff = qs * 128
                        vis = [(c, (q0 - 2048 * c) // 128) for c in range(NCC) if (q0 - 2048 * c) >= 0]
                        penda = None
                        for h in range(4):
                            for idx, (c, m) in enumerate(vis):
                                mk = cmask[m] if m <= 16 else None
                                p_, ptk = score_exp(kcmpT, 'kcmpT', c, qb[:, h, :], qbk, qoff, 128, mk, "cm%d" % m if m <= 16 else None)
                                if penda is not None:
                                    emit_pva(*penda)
                                penda = (p_, ptk, h, c, idx == 0, idx == len(vis) - 1)
                        emit_pva(*penda)
                        for h in range(4):
                            P.ts('dve', rden[:, h:h + 1], pa[h][:, 128:129], 1e-30, None, ALU.max, reads=['pa%d' % h], writes=['rden'])
                            P.op('dve', lambda e, h=h: e.reciprocal(rden[:, h:h + 1], rden[:, h:h + 1]), ['rden'], ['rden'])
                            P.ts('dve', oc[h][:], pa[h][:, 0:128], rden[:, h:h + 1], None, ALU.mult,
                                 reads=['pa%d' % h, 'rden'], writes=['oc%d' % h])
                            if h == 0:
                                P.ts('dve', imp[:], pa[h][:, 129:129 + NSLC], rden[:, h:h + 1], None, ALU.mult,
                                     reads=['pa%d' % h, 'rden'], writes=['imp'])
                            else:
                                P.stt('dve', imp[:], pa[h][:, 129:129 + NSLC], rden[:, h:h + 1], imp[:], ALU.mult, ALU.add,
                                      reads=['pa%d' % h, 'rden', 'imp'], writes=['imp'])
                        lo = max(0, 2 * i - 1)
                        P.tt('dve', imp[:, lo:2 * i + 2], imp[:, lo:2 * i + 2], fpB[:, lo - (2 * i - 1):3], ALU.add,
                             reads=['imp', 'fpB'], writes=['imp'])
                        P.tt('dve', imp[:], imp[:], f0B[:], ALU.add, reads=['imp', 'f0B'], writes=['imp'])
                        P.op('dve', lambda e: e.max(out=m8[:, 0:8], in_=imp[:]), ['imp'], ['m8'])
                        P.op('dve', lambda e: e.match_replace(out=work[:], in_to_replace=m8[:, 0:8], in_values=imp[:], imm_value=-1.0e9),
                             ['imp', 'm8'], ['work'])
                        P.op('dve', lambda e: e.max(out=m8[:, 8:16], in_=work[:]), ['work'], ['m8'])
                        P.op('dve', lambda e: e.tensor_reduce(out=thr[:], in_=m8[:, 8:16], axis=AX.X, op=ALU.min), ['m8'], ['thr'])
                        P.ts('dve', sel[:, 0:NSLC], imp[:], thr[:, 0:1], None, ALU.is_ge, reads=['imp', 'thr'], writes=['sel'])
                        pendw = None
                        wl = list(range(max(0, i - 4), i + 1))
                        for h in range(4):
                            for idx, kj in enumerate(wl):
                                mk, mkey = (mdiag, 'm_diag') if kj == i else ((mstrict, 'm_strict') if kj == i - 4 else (None, None))
                                p_, ptk = score_exp(kws, 'kws', kj, qb[:, h, :], qbk, qoff, 128, mk, mkey)
                                if pendw is not None:
                                    emit_pvw(*pendw)
                                pendw = (p_, ptk, h, kj, idx == 0, idx == len(wl) - 1)
                        emit_pvw(*pendw)
                        for h in range(4):
                            accw, acwk = pa[h], 'pa%d' % h
                            P.ts('dve', rg[:, h:h + 1], accw[:, 128:129], 1e-30, None, ALU.max, reads=[acwk], writes=['rg'])
                            P.op('dve', lambda e, h=h: e.reciprocal(rg[:, h:h + 1], rg[:, h:h + 1]), ['rg'], ['rg'])
                            P.tt('dve', rg[:, h:h + 1], rg[:, h:h + 1], gsb[:, gi, 3 * h + 2:3 * h + 3], ALU.mult, reads=['rg', 'gsb'], writes=['rg'])
                            op_, opk = opart[h][qs], "op%d_%d" % (h, qs)
                            P.ts('dve', op_[:], oc[h][:], gsb[:, gi, 3 * h:3 * h + 1], None, ALU.mult, reads=['oc%d' % h, 'gsb'], writes=[opk])
                            P.stt('dve', op_[:], accw[:, 0:128], rg[:, h:h + 1], op_[:], ALU.mult, ALU.add, reads=[acwk, 'rg', opk], writes=[opk])
                        for jc in range(NJC):
                            rows = min(128, NSLC - jc * 128)
                            P.transpose(pst[0:rows, jc * 128:(jc + 1) * 128], sel[:, jc * 128:jc * 128 + rows], ident[:],
                                        reads=['sel', 'ident'], writes=['pst'])
                            P.ts('dve', biasT[0:rows, jc, qoff:qoff + 128], pst[0:rows, jc * 128:(jc + 1) * 128], -1.0, 30000.0,
                                 ALU.add, ALU.mult, reads=['pst'], writes=['biasT'])
                    def emit_pvb(p_, ptk, kj, dd, stile):
                        for qs in range(max(dd, 0), 4):
                            P.mm(pa[qs][:, 0:129], p_[:, qs * 128:(qs + 1) * 128], vsa[:, kj, :], kj == 0, kj == 4 * stile + qs,
                                 reads=[ptk, 'vsa'], writes=['pa%d' % qs])
                    pendb = []
                    for h in range(4):
                        nk = 4 * stile + 4
                        for kj in range(nk):
                            dd = kj - 4 * stile
                            mk, mkey = (cst["dm%d" % dd], "dm%d" % dd) if dd >= 0 else (None, None)
                            p_, ptk = score_exp(kss, 'kss', kj, qb[:, h, :], qbk, 0, 512, mk, mkey, bias=True)
                            if len(pendb) == 2:
                                emit_pvb(*pendb.pop(0))
                            pendb.append((p_, ptk, kj, dd, stile))
                        while pendb:
                            emit_pvb(*pendb.pop(0))
                        for qs in range(4):
                            gi = (128 * (4 * stile + qs) - HAL0) // 128
                            P.ts('dve', rg[:, 4:5], pa[qs][:, 128:129], 1e-30, None, ALU.max, reads=['pa%d' % qs], writes=['rg'])
                            P.op('dve', lambda e: e.reciprocal(rg[:, 4:5], rg[:, 4:5]), ['rg'], ['rg'])
                            P.tt('dve', rg[:, 4:5], rg[:, 4:5], gsb[:, gi, 3 * h + 1:3 * h + 2], ALU.mult, reads=['rg', 'gsb'], writes=['rg'])
                            oi = cnt['o'] % 2
                            cnt['o'] += 1
                            P.stt('dve', ofin[oi][:], pa[qs][:, 0:128], rg[:, 4:5], opart[h][qs][:], ALU.mult, ALU.add,
                                  reads=['pa%d' % qs, 'rg', "op%d_%d" % (h, qs)], writes=['ofin%d' % oi])
                            tcol = 128 * (4 * stile + qs) - HAL0
                            emit_oT(P, cst, ofin[oi][:], 'ofin%d' % oi, obf[oi], 'obf%d' % oi, pst, osb[oi], 'osb%d' % oi,
                                    io['OT'][(4 * g + h) * 128:(4 * g + h + 1) * 128, tcol:tcol + 128])


def phase3(P, nc, G, io):
    L, HAL0, QN = G.L, G.HAL0, G.QN
    NKC = L // 128
    scale = 192 ** -0.5
    with Phase(P):
        tmp_i = P.sb([128, 512], I32, "tmp_i")
        cst = attn_consts(P, tmp_i)
        postm = P.sb([128, NKC], F32, "postm")
        P.dma('sp', postm[:], io['postm'][:, :], writes=['postm'])
        kta = P.sb([128, L], BF16, "kta")
        ktb = P.sb([128, L], BF16, "ktb")
        qta = P.sb([128, QN], BF16, "qta")
        qtb = P.sb([128, QN], BF16, "qtb")
        vaug = P.sb([128, NKC, 129], BF16, "vaug")
        ps_s = [P.ps([128, 512], F32, "ps_s%d" % i) for i in range(3)]
        ps_a = [P.ps([128, 512], F32, "ps_a%d" % i) for i in range(4)]
        pst = P.ps([128, 1024], BF16, "pst")
        pt = [P.sb([128, 512], BF16, "pt%d" % i) for i in range(4)]
        P.memset('pool', ktb[64:128, :], 0.0, writes=['ktb'])
        P.memset('pool', qtb[64:128, :], 0.0, writes=['qtb'])
        ofin = [P.sb([128, 128], F32, "ofin%d" % i) for i in range(2)]
        obf = [P.sb([128, 128], BF16, "obf%d" % i) for i in range(2)]
        osb = [P.sb([128, 128], BF16, "osb%d" % i) for i in range(2)]
        rden = [P.sb([128, 1], F32, "rden%d" % i) for i in range(2)]
        cnt = {'s': 0, 'pt': 0, 'o': 0}
        P.dma('sp', ktb[0:64, :], io['KPE'][:, :], writes=['ktb'])
        for h in range(8):
            P.dma('sp', kta[:], io['KMN'][h * 128:(h + 1) * 128, :], writes=['kta'])
            P.dma('act', qta[:], io['QMN'][h * 128:(h + 1) * 128, :], writes=['qta'])
            P.dma('act', qtb[0:64, :], io['QMP'][h * 64:(h + 1) * 64, :], writes=['qtb'])
            load_vaug(P, vaug, 'vaug', lambda c0, c1: io['VM'][c0 * 128:c1 * 128, h * 128:(h + 1) * 128].rearrange("(c p) d -> p c d", p=128), NKC, postm[:, :].rearrange("p (c o) -> p c o", o=1), 'sp')
            def emit_pv(ptt, ptk, kj, dd, stile):
                for qs in range(max(dd, 0), 4):
                    P.mm(ps_a[qs][:, 0:129], ptt[:, qs * 128:(qs + 1) * 128], vaug[:, kj, :], kj == 0, kj == 4 * stile + qs,
                         reads=[ptk, 'vaug'], writes=['ps_a%d' % qs])
            pend = []
            for stile in range(HAL0 // 512, L // 512):
                qoff = stile * 512 - HAL0
                nk = 4 * stile + 4
                for kj in range(nk):
                    dd = kj - 4 * stile
                    si = cnt['s'] % 3
                    cnt['s'] += 1
                    pss, psk = ps_s[si], "ps_s%d" % si
                    P.mm(pss[:, 0:512], kta[:, kj * 128:(kj + 1) * 128], qta[:, qoff:qoff + 512], True, False, reads=['kta', 'qta'], writes=[psk])
                    P.mm(pss[:, 0:512], ktb[:, kj * 128:(kj + 1) * 128], qtb[:, qoff:qoff + 512], False, True, reads=['ktb', 'qtb'], writes=[psk])
                    pi = cnt['pt'] % 4
                    cnt['pt'] += 1
                    ptt, ptk = pt[pi], "pt%d" % pi
                    P.act(ptt[:], pss[:, 0:512], AF.Exp, reads=[psk], writes=[ptk], scale=scale)
                    if dd >= 0:
                        P.tt('dve', ptt[:], ptt[:], cst["dm%d" % dd][:], ALU.mult, reads=[ptk, "dm%d" % dd], writes=[ptk])
                    if len(pend) == 2:
                        emit_pv(*pend.pop(0))
                    pend.append((ptt, ptk, kj, dd, stile))
                while pend:
                    emit_pv(*pend.pop(0))
                for qs in range(4):
                    oi = cnt['o'] % 2
                    cnt['o'] += 1
                    rd, rk = rden[oi], "rden%d" % oi
                    P.ts('dve', rd[:], ps_a[qs][:, 128:129], 1e-30, None, ALU.max, reads=['ps_a%d' % qs], writes=[rk])
                    P.op('dve', lambda e, rd=rd: e.reciprocal(rd[:], rd[:]), [rk], [rk])
                    P.ts('dve', ofin[oi][:], ps_a[qs][:, 0:128], rd[:, 0:1], None, ALU.mult, reads=['ps_a%d' % qs, rk], writes=['ofin%d' % oi])
                    tcol = qoff + qs * 128
                    emit_oT(P, cst, ofin[oi][:], 'ofin%d' % oi, obf[oi], 'obf%d' % oi, pst, osb[oi], 'osb%d' % oi,
                            io['OT'][1024 + h * 128:1024 + (h + 1) * 128, tcol:tcol + 128])


DFF = 8192


def phase_post(P, nc, G, io, layer):
    NT = 512
    if layer == 0:
        KCo, NTOK, h_src, h_off, o_src, o_dst = 16, G.QN, io['xT'], G.HAL0, io['OT'], io['H2T']
    else:
        KCo, NTOK, h_src, h_off, o_src, o_dst = 8, G.OWN, io['H2T'], 2048, io['OT1'], io['out']
    Lp = "l%d_" % layer
    w_out, w_ff1, w_ff2 = io[Lp + 'w_out'], io[Lp + 'w_ff1'], io[Lp + 'w_ff2']
    with Phase(P):
        c = setup_common(P, NT)
        c.sq = [P.sb([128, NT], BF16, "sq%d" % i) for i in range(2)]
        gs = P.sb([128, 4, 16], F32, "gs")
        gl_ = [io[Lp + 'norm_mix_post'], io[Lp + 'norm_ffn_pre'], io[Lp + 'norm_ffn_post']] + ([io['l1_norm_mix_pre']] if layer == 0 else [])
        for i, g in enumerate(gl_):
            P.dma('sp', gs[:, i, :], g.rearrange("(kc p) -> p kc", p=128), writes=['gs'], slow=True)
        hs = P.sb([128, 16, NT], F32, "hs")
        us = P.sb([128, 16, NT], BF16, "us")
        ms = P.sb([128, 16, NT], F32, "ms")
        f1 = P.sb([128, 64, NT], BF16, "f1")
        rstd = P.sb([128, NT], F32, "rstd")
        tmp = [P.sb([128, NT], F32, "tmp%d" % i) for i in range(2)]
        wb = [P.sb([128, 4096], BF16, "wb%d" % i) for i in range(4)]
        ob = [P.sb([128, NT], BF16, "ob%d" % i) for i in range(3)]
        cnt = {'w': 0, 'ob': 0, 't': 0}
        if layer == 0:
            rsb = P.sb([128, 4], F32, "rsb")
            P.dma('sp', rsb[:], io['ridx'][:, :], writes=['rsb'])
            rc = P.sb([128, 2], F32, "rc")
            P.act(rc[:, 0:1], rsb[:, 0:1], AF.Exp, reads=['rsb'], writes=['rc'], scale=-2.0 * LN_THETA / 128.0)
            posb = P.sb([128, NT], F32, "posb")
            ang = P.sb([128, NT], F32, "ang")
            ri = P.sb([128, NT], I32, "ri")
            rr_ = P.sb([128, NT], F32, "rr")
            zb = [P.sb([128, NT], BF16, "zb%d" % i) for i in range(2)]
            tmp_i = P.sb([128, 128], I32, "tmp_i")
            r128, r64 = make_rperm(P, tmp_i, "tmp_i")
            cosT = P.sb([128, NT], F32, "cosT")
            sinT = P.sb([128, NT], F32, "sinT")

        WB = io['WB%d' % layer]

        def wload(src_ap_fn, nseg, segw, bid, first):
            i = cnt['w'] % 4
            cnt['w'] += 1
            t, k = wb[i], "wb%d" % i
            n = nseg * segw
            assert n <= 4096
            view = t[:, 0:n].rearrange("p (s w) -> p s w", w=segw)
            if first:
                P.dma('pool', view[:, :, :], src_ap_fn(0, nseg), writes=[k])
                P.dma('sp', WB[bid, :, 0:n], t[:, 0:n], reads=[k], writes=['WB_%d' % bid])
            else:
                P.dma('sp' if cnt['w'] % 2 == 0 else 'pool', t[:, 0:n], WB[bid, :, 0:n], reads=['WB_%d' % bid], writes=[k])
            return view, k

        def add_norm(src, skey, gi):
            rms_rstd(P, c, src, skey, 16, D, rstd, 'rstd', 'sq')
            for kc in range(16):
                i = cnt['t'] % 2
                cnt['t'] += 1
                P.stt('dve', tmp[i][:], src[:, kc, :], gs[:, gi, kc:kc + 1], rstd[:], ALU.mult, ALU.mult,
                      reads=[skey, 'gs', 'rstd'], writes=['tmp%d' % i])
                P.tt('pool', hs[:, kc, :], hs[:, kc, :], tmp[i][:], ALU.add, reads=['hs', 'tmp%d' % i], writes=['hs'])

        def norm_to_us(gi):
            rms_rstd(P, c, hs, 'hs', 16, D, rstd, 'rstd', 'sq')
            for kc in range(16):
                P.stt('dve', us[:, kc, :], hs[:, kc, :], gs[:, gi, kc:kc + 1], rstd[:], ALU.mult, ALU.mult,
                      reads=['hs', 'gs', 'rstd'], writes=['us'])

        for s0 in range(0, NTOK, NT):
            sl = slice(s0, s0 + NT)
            hl = slice(h_off + s0, h_off + s0 + NT)
            for kc in range(16):
                P.dma('sp' if kc % 2 == 0 else 'act', hs[:, kc, :], h_src[kc * 128:(kc + 1) * 128, hl], writes=['hs'])
            for kc in range(KCo):
                P.dma('act' if kc % 2 == 0 else 'sp', us[:, kc, :], o_src[kc * 128:(kc + 1) * 128, sl], writes=['us'])
            for cb in range(8):
                wv, wk = wload(lambda a, b, cb=cb: w_out[a * 128:b * 128, cb * 256:(cb + 1) * 256].rearrange("(s p) w -> p s w", p=128), KCo, 256, cb, s0 == 0)
                for j in range(2):
                    ps, pk = next_ps(c)
                    for kc in range(KCo):
                        P.mm(ps[:, 0:NT], wv[:, kc, j * 128:(j + 1) * 128], us[:, kc, :], kc == 0, kc == KCo - 1,
                             reads=[wk, 'us'], writes=[pk])
                    P.copy('act', ms[:, cb * 2 + j, :], ps[:, 0:NT], reads=[pk], writes=['ms'])
            add_norm(ms, 'ms', 0)
            norm_to_us(1)
            for cb in range(32):
                wv, wk = wload(lambda a, b, cb=cb: w_ff1[a * 128:b * 128, cb * 256:(cb + 1) * 256].rearrange("(s p) w -> p s w", p=128), 16, 256, 8 + cb, s0 == 0)
                for j in range(2):
                    ps, pk = next_ps(c)
                    for kc in range(16):
                        P.mm(ps[:, 0:NT], wv[:, kc, j * 128:(j + 1) * 128], us[:, kc, :], kc == 0, kc == 15,
                             reads=[wk, 'us'], writes=[pk])
                    i = cnt['t'] % 2
                    cnt['t'] += 1
                    P.act(tmp[i][:], ps[:, 0:NT], AF.Relu, reads=[pk], writes=['tmp%d' % i])
                    P.tt('dve', f1[:, cb * 2 + j, :], tmp[i][:], tmp[i][:], ALU.mult, reads=['tmp%d' % i], writes=['f1'])
            for cb in range(16):
                ps, pk = next_ps(c)
                for hf in range(2):
                    wv, wk = wload(lambda a, b, cb=cb, hf=hf: w_ff2[(hf * 32 + a) * 128:(hf * 32 + b) * 128, cb * 128:(cb + 1) * 128].rearrange("(s p) w -> p s w", p=128),
                                   32, 128, 40 + cb * 2 + hf, s0 == 0)
                    for kc in range(32):
                        P.mm(ps[:, 0:NT], wv[:, kc, :], f1[:, hf * 32 + kc, :], hf == 0 and kc == 0, hf == 1 and kc == 31,
                             reads=[wk, 'f1'], writes=[pk])
                P.copy('act', ms[:, cb, :], ps[:, 0:NT], reads=[pk], writes=['ms'])
            add_norm(ms, 'ms', 2)
            for kc in range(16):
                P.dma('sp' if kc % 2 == 0 else 'act', o_dst[kc * 128:(kc + 1) * 128, sl], hs[:, kc, :], reads=['hs'])
            if layer == 0:
                w_in = io['l1_w_in']
                P.dma('sp', posb[:], io['pos'][0:1, hl].to_broadcast([128, NT]), writes=['posb'])
                rope_tables_emit(P, posb, rc[:, 0:1], rsb[:, 1:2], ang, rr_, ri, cosT, sinT, None)
                norm_to_us(3)
                for cb in range(36):
                    wv, wk = wload(lambda a, b, cb=cb: w_in[a * 128:b * 128, cb * 256:(cb + 1) * 256].rearrange("(s p) w -> p s w", p=128), 16, 256, 72 + cb, s0 == 0)
                    ch0 = cb * 2
                    ttype, g = (ch0 // 8) % 3, ch0 // 24
                    if ttype == 2:
                        for tb in range(NT // 128):
                            ps, pk = next_ps(c)
                            for kc in range(16):
                                P.mm(ps[:, 0:256], us[:, kc, tb * 128:(tb + 1) * 128], wv[:, kc, :], kc == 0, kc == 15,
                                     reads=[wk, 'us'], writes=[pk])
                            oi = cnt['ob'] % 3
                            cnt['ob'] += 1
                            o, ok = ob[oi], "ob%d" % oi
                            P.copy('act', o[:, 0:256], ps[:, 0:256], reads=[pk], writes=[ok])
                            hc0 = (ch0 % 8) * 128
                            P.dma('sp', io['V1'][s0 + tb * 128:s0 + (tb + 1) * 128, g * 1024 + hc0:g * 1024 + hc0 + 256], o[:, 0:256], reads=[ok])
                        continue
                    for j in range(2):
                        ch = ch0 + j
                        h = ch % 8
                        ps, pk = next_ps(c)
                        for kc in range(16):
                            P.mm(ps[:, 0:NT], wv[:, kc, j * 128:(j + 1) * 128], us[:, kc, :], kc == 0, kc == 15,
                                 reads=[wk, 'us'], writes=[pk])
                        zi = cnt['ob'] % 2
                        zb_, zk = zb[zi], "zb%d" % zi
                        P.copy('act', zb_[:], ps[:, 0:NT], reads=[pk], writes=[zk])
                        pr, prk = next_ps(c)
                        P.mm(pr[:, 0:NT], r128[:], zb_[:], True, True, reads=['rperm', zk], writes=[prk])
                        oi = cnt['ob'] % 3
                        cnt['ob'] += 1
                        o, ok = ob[oi], "ob%d" % oi
                        P.tt('dve', tmp[0][:], zb_[:], cosT[:], ALU.mult, reads=[zk, 'tables'], writes=['tmp0'])
                        P.tt('dve', tmp[1][:], pr[:, 0:NT], sinT[:], ALU.mult, reads=[prk, 'tables'], writes=['tmp1'])
                        P.tt('pool', o[:], tmp[0][:], tmp[1][:], ALU.add, reads=['tmp0', 'tmp1'], writes=[ok])
                        zr = ((g * 2 + ttype) * 8 + h) * 128
                        P.dma('sp', io['Z1'][zr:zr + 128, sl], o[:], reads=[ok])


def phase5(P, nc, G, io):
    QN, OWN = G.QN, G.OWN
    NC1 = QN // 128
    M4 = QN // 512
    M16 = QN // 2048
    scale = 128 ** -0.5
    with Phase(P):
        tmp_i = P.sb([128, 512], I32, "tmp_i")
        cst = attn_consts(P, tmp_i)
        mdiag, mge = cst["m_diag"], cst["m_ge"]
        m4 = {}
        am = P.sb([128, 128], BF16, "am4")
        for c4 in range(4):
            for mm in range(-1, 4):
                t = P.sb([128, 128], BF16, "m4_%d_%d" % (c4, mm + 1))
                k = "m4_%d_%d" % (c4, mm + 1)
                make_mask(P, t, k, 128, c4 - 128 * mm, -1, 4, tmp_i, "tmp_i")
                make_mask(P, am, "am4", 128, 128 - c4 + 128 * mm, 1, -4, tmp_i, "tmp_i")
                P.tt('dve', t[:], t[:], am[:], ALU.mult, reads=[k, 'am4'], writes=[k])
                m4[(c4, mm)] = (t, k)
        posq = P.sb([128, NC1], F32, "posq")
        posd4 = P.sb([128, 4 * M4], F32, "posd4")
        posd16 = P.sb([128, 16 * M16], F32, "posd16")
        P.dma('sp', posq[:], io['posq'][:, :], writes=['posq'])
        P.dma('sp', posd4[:], io['posd4'][:, :], writes=['posd4'])
        P.dma('sp', posd16[:], io['posd16'][:, :], writes=['posd16'])
        Ks = [P.sb([128, QN], BF16, "dk%d" % g) for g in range(3)]
        Qs = [P.sb([128, OWN], BF16, "dq%d" % g) for g in range(3)]
        v1 = P.sb([128, NC1, 129], BF16, "dv1")
        v4 = P.sb([128, 4 * M4, 129], BF16, "dv4")
        v16 = P.sb([128, 16 * M16, 129], BF16, "dv16")
        ps_s = [P.ps([128, 512], F32, "ps_s%d" % i) for i in range(2)]
        ps_a = [P.ps([128, 512], F32, "ps_a%d" % i) for i in range(2)]
        pst = P.ps([128, 1024], BF16, "pst")
        pt = [P.sb([128, 128], BF16, "pt%d" % i) for i in range(3)]
        raw = [P.sb([128, 129], F32, "raw%d" % i) for i in range(2)]
        ofin = [P.sb([128, 128], F32, "ofin%d" % i) for i in range(2)]
        obf = [P.sb([128, 128], BF16, "obf%d" % i) for i in range(2)]
        osb = [P.sb([128, 128], BF16, "osb%d" % i) for i in range(2)]
        rden = [P.sb([128, 1], F32, "rden%d" % i) for i in range(2)]
        cnt = {'s': 0, 'pt': 0, 'o': 0, 'a': 0}

        def visit(kap, kkey, qap, qkey, mask, mkey, vap, vkey, acc, ak, first, last):
            si = cnt['s'] % 2
            cnt['s'] += 1
            ps, pk = ps_s[si], "ps_s%d" % si
            P.mm(ps[:, 0:128], kap, qap, True, True, reads=[kkey, qkey], writes=[pk])
            pi = cnt['pt'] % 3
            cnt['pt'] += 1
            p_, ptk = pt[pi], "pt%d" % pi
            P.act(p_[:], ps[:, 0:128], AF.Exp, reads=[pk], writes=[ptk], scale=scale)
            P.tt('dve', p_[:], p_[:], mask[:], ALU.mult, reads=[ptk, mkey], writes=[ptk])
            P.mm(acc[:, 0:129], p_[:], vap, first, last, reads=[ptk, vkey], writes=[ak])

        for h in range(8):
            for g in range(3):
                P.dma('sp', Ks[g][:], io['Z1'][((g * 2 + 1) * 8 + h) * 128:((g * 2 + 1) * 8 + h + 1) * 128, :], writes=['dk%d' % g])
                P.dma('act', Qs[g][:], io['Z1'][((g * 2) * 8 + h) * 128:((g * 2) * 8 + h + 1) * 128, 2048:], writes=['dq%d' % g])
            vcol = lambda g: slice(g * 1024 + h * 128, g * 1024 + (h + 1) * 128)
            P.dma('sp', v1[:, :, 0:128], io['V1'][:, vcol(0)].rearrange("(c p) d -> p c d", p=128), writes=['dv1'])
            P.op('dve', lambda e: e.tensor_single_scalar(v1[:, :, 128:129], posq[:, :].rearrange("p (c o) -> p c o", o=1), 0.0, ALU.is_ge), ['posq'], ['dv1'])
            for rho in range(4):
                P.dma('act', v4[:, rho * M4:(rho + 1) * M4, 0:128],
                      io['V1'][:, vcol(1)].rearrange("(m p r) d -> r p m d", p=128, r=4)[rho], writes=['dv4'])
            P.op('dve', lambda e: e.tensor_single_scalar(v4[:, :, 128:129], posd4[:, :].rearrange("p (c o) -> p c o", o=1), 0.0, ALU.is_ge), ['posd4'], ['dv4'])
            for rho in range(16):
                P.dma('sp' if rho % 2 == 0 else 'act', v16[:, rho * M16:(rho + 1) * M16, 0:128],
                      io['V1'][:, vcol(2)].rearrange("(m p r) d -> r p m d", p=128, r=16)[rho], writes=['dv16'])
            P.op('dve', lambda e: e.tensor_single_scalar(v16[:, :, 128:129], posd16[:, :].rearrange("p (c o) -> p c o", o=1), 0.0, ALU.is_ge), ['posd16'], ['dv16'])
            K4v = Ks[1][:].rearrange("p (m k r) -> p r m k", k=128, r=4)
            K16v = Ks[2][:].rearrange("p (m k r) -> p r m k", k=128, r=16)
            Q4v = Qs[1][:].rearrange("p (u j r) -> p u r j", j=128, r=16)
            Q16v = Qs[2][:].rearrange("p (u j r) -> p u r j", j=128, r=16)
            rawd = io['RAW'][:, h, :].rearrange("(u j r) d -> u r j d", j=128, r=16)
            for u in range(OWN // 2048):
                for rho in range(16):
                    ai = cnt['a'] % 2
                    cnt['a'] += 1
                    acc, ak = ps_a[ai], "ps_a%d" % ai
                    rho4, c4 = rho % 4, rho // 4
                    visits = []
                    visits.append((K16v[:, rho, u, :], 'dk2', Q16v[:, u, rho, :], 'dq2', mge, 'm_ge', v16[:, rho * M16 + u, :], 'dv16'))
                    visits.append((K16v[:, rho, u + 1, :], 'dk2', Q16v[:, u, rho, :], 'dq2', mdiag, 'm_diag', v16[:, rho * M16 + u + 1, :], 'dv16'))
                    for mm in range(-1, 4):
                        m = 4 * (1 + u) + mm
                        mk, mkk = m4[(c4, mm)]
                        visits.append((K4v[:, rho4, m, :], 'dk1', Q4v[:, u, rho, :], 'dq1', mk, mkk, v4[:, rho4 * M4 + m, :], 'dv4'))
                    for vi, (kap, kk, qap, qk, mk, mkk, vap, vk) in enumerate(visits):
                        visit(kap, kk, qap, qk, mk, mkk, vap, vk, acc, ak, vi == 0, vi == len(visits) - 1)
                    ri_ = cnt['o'] % 2
                    cnt['o'] += 1
                    P.copy('dve', raw[ri_][:], acc[:, 0:129], reads=[ak], writes=['raw%d' % ri_])
                    P.dma('sp', rawd[u, rho], raw[ri_][:], reads=['raw%d' % ri_])
            P.fence()
            for i in range(OWN // 128):
                ai = cnt['a'] % 2
                cnt['a'] += 1
                acc, ak = ps_a[ai], "ps_a%d" % ai
                qap = Qs[0][:, i * 128:(i + 1) * 128]
                visit(Ks[0][:, (15 + i) * 128:(16 + i) * 128], 'dk0', qap, 'dq0', mge, 'm_ge', v1[:, 15 + i, :], 'dv1', acc, ak, True, False)
                visit(Ks[0][:, (16 + i) * 128:(17 + i) * 128], 'dk0', qap, 'dq0', mdiag, 'm_diag', v1[:, 16 + i, :], 'dv1', acc, ak, False, True)
                ri_ = cnt['o'] % 2
                cnt['o'] += 1
                r_, rk_ = raw[ri_], 'raw%d' % ri_
                P.dma('act', r_[:], io['RAW'][i * 128:(i + 1) * 128, h, :], writes=[rk_])
                P.tt('dve', r_[:], r_[:], acc[:, 0:129], ALU.add, reads=[rk_, ak], writes=[rk_])
                rd, rdk = rden[ri_], "rden%d" % ri_
                P.ts('dve', rd[:], r_[:, 128:129], 1e-30, None, ALU.max, reads=[rk_], writes=[rdk])
                P.op('dve', lambda e, rd=rd: e.reciprocal(rd[:], rd[:]), [rdk], [rdk])
                P.ts('dve', ofin[ri_][:], r_[:, 0:128], rd[:, 0:1], None, ALU.mult, reads=[rk_, rdk], writes=['ofin%d' % ri_])
                emit_oT(P, cst, ofin[ri_][:], 'ofin%d' % ri_, obf[ri_], 'obf%d' % ri_, pst, osb[ri_], 'osb%d' % ri_,
                        io['OT1'][h * 128:(h + 1) * 128, i * 128:(i + 1) * 128])
            P.fence()


WNAMES = ["l0_norm_mix_pre", "l0_w_in", "l0_cmp_pe_k", "l0_cmp_w1_k", "l0_cmp_w2_k", "l0_cmp_pe_v", "l0_cmp_w1_v", "l0_cmp_w2_v",
          "l0_mla_q_norm", "l0_mla_w_uq", "l0_mla_kv_norm", "l0_mla_w_ukv", "l0_w_out", "l0_norm_mix_post", "l0_norm_ffn_pre",
          "l0_w_ff1", "l0_w_ff2", "l0_norm_ffn_post", "l1_norm_mix_pre", "l1_w_in", "l1_w_out", "l1_norm_mix_post",
          "l1_norm_ffn_pre", "l1_w_ff1", "l1_w_ff2", "l1_norm_ffn_post"]


def build_fused(L, wshapes, phases=(1, 2, 3, 4, 5, 6)):
    G = Geo(L)
    nc = bass.Bass("TRN2", target_bir_lowering=False)
    io = {}
    def ein(name, shape, dt=F32):
        io[name] = nc.dram_tensor(name, list(shape), dt, kind="ExternalInput").ap()
    def scr(name, shape, dt):
        io[name] = nc.dram_tensor(name, list(shape), dt).ap()
    ein('xT', [D, L]); ein('pos', [1, L]); ein('ridx', [128, 4]); ein('postm', [128, L // 128])
    ein('cpos', [128, ((L - 32) // 16 + 1 + 127) // 128]); ein('f0', [1, L // 64])
    ein('posq', [128, G.QN // 128]); ein('posd4', [128, 4 * (G.QN // 512)]); ein('posd16', [128, 16 * (G.QN // 2048)])
    for n in WNAMES:
        ein(n, wshapes[n])
    io['out'] = nc.dram_tensor("out", [D, G.OWN], F32, kind="ExternalOutput").ap()
    scr('QA', [1024, G.QN], BF16); scr('GATE', [G.QN, 24], F32); scr('KFM', [1024, L], BF16)
    scr('VS', [L, 256], BF16); scr('VW', [L, 256], BF16); scr('KPE', [64, L], BF16)
    scr('QMN', [1024, G.QN], BF16); scr('QMP', [512, G.QN], BF16); scr('KMN', [1024, L], BF16); scr('VM', [L, 1024], BF16)
    scr('OT', [2048, G.QN], BF16); scr('H2T', [D, G.QN], F32); scr('Z1', [6144, G.QN], BF16); scr('V1', [G.QN, 3072], BF16)
    scr('RAW', [G.OWN, 8, 129], F32); scr('OT1', [1024, G.OWN], BF16)
    scr('WB0', [108, 128, 4096], BF16); scr('WB1', [72, 128, 4096], BF16)
    with contextlib.ExitStack() as st:
        P = Prog(nc, st)
        if 1 in phases: phase1(P, nc, G, io)
        if 2 in phases: phase2(P, nc, G, io)
        if 3 in phases: phase3(P, nc, G, io)
        if 4 in phases: phase_post(P, nc, G, io, 0)
        if 5 in phases: phase5(P, nc, G, io)
        if 6 in phases: phase_post(P, nc, G, io, 1)
        P.emit()
    return nc, G


def fused_inputs(x, weights, L_total_S):
    B, S, _ = x.shape
    L = S
    G = Geo(L)
    maps = []
    for c in range(8):
        b, k = c // 4, c % 4
        end = (k + 1) * G.OWN
        shift = L - end
        xT = np.zeros((D, L), np.float32)
        xT[:, shift:] = x[b, :end].T
        pos = (np.arange(L) - shift).astype(np.float32)
        d = dict(xT=xT, pos=pos[None, :].copy(), ridx=ridx_np(),
                 postm=np.ascontiguousarray(pos.reshape(L // 128, 128).T))
        ncc = ((L - 32) // 16 + 1 + 127) // 128
        cp = np.full(ncc * 128, -1.0, np.float32)
        nvalid = min(ncc * 128, L // 16)
        cp[:nvalid] = pos[0:16 * nvalid:16]
        ncmp = (L - 32) // 16 + 1
        cp[ncmp:] = -1.0
        d['cpos'] = np.ascontiguousarray(cp.reshape(ncc, 128).T)
        f0 = np.zeros((1, L // 64), np.float32)
        f0[0, shift // 64] = 1.0
        d['f0'] = f0
        pq = pos[G.HAL0:]
        d['posq'] = np.ascontiguousarray(pq.reshape(G.QN // 128, 128).T)
        d['posd4'] = np.ascontiguousarray(pq.reshape(G.QN // 512, 128, 4).transpose(1, 2, 0).reshape(128, -1))
        d['posd16'] = np.ascontiguousarray(pq.reshape(G.QN // 2048, 128, 16).transpose(1, 2, 0).reshape(128, -1))
        for n in WNAMES:
            d[n] = weights[n]
        maps.append(d)
    return maps, G


def kernel(**inp):
    inp = {k: np.asarray(v) for k, v in inp.items()}
    x = inp["x"]
    B, S, _ = x.shape
    weights = {n: inp[n] for n in WNAMES}
    nc, G = build_fused(S, {n: weights[n].shape for n in WNAMES})
    maps, G = fused_inputs(x, weights, S)
    res = run_bass_kernel_spmd(nc, maps, core_ids=list(range(8)))
    del maps
    out = np.empty((B, S, D), np.float32)
    for c in range(8):
        b, k = c // 4, c % 4
        out[b, k * G.OWN:(k + 1) * G.OWN] = np.asarray(res.results[c]["out"]).T
    return out
```

```python
import contextlib
import numpy as np
import concourse.bass as bass
import concourse.mybir as mybir
from concourse.bass_utils import run_bass_kernel_spmd

F32 = mybir.dt.float32
BF16 = mybir.dt.bfloat16
I32 = mybir.dt.int32
AF = mybir.ActivationFunctionType
ALU = mybir.AluOpType
AX = mybir.AxisListType

NDMA_SEM = 6
EPOCH = 100000
NEPOCH = 4


class Prog:
    def __init__(self, nc, stack):
        self.nc = nc
        self.stack = stack
        self.names = ['pe', 'act', 'dve', 'pool', 'sp']
        self.ops = {k: [] for k in self.names}
        self.cnt = {k: 0 for k in self.names}
        self.dcnt = {k: 0 for k in self.names}
        self.csem = {(k, ep): stack.enter_context(nc.semaphore("c_%s%d" % (k, ep)))
                     for k in self.names for ep in range(NEPOCH if k == 'pe' else 1)}
        self.dsem = {k: [stack.enter_context(nc.semaphore("d_%s%d" % (k, i))) for i in range(NDMA_SEM)]
                     for k in ('sp', 'pool', 'act')}
        self.seen = {k: {} for k in self.names}
        self.lastw = {}
        self.readers = {}
        self.ntile = 0

    def sb(self, shape, dt, name=None):
        self.ntile += 1
        return self.stack.enter_context(self.nc.sbuf_tensor("%s_u%d" % (name or "t", self.ntile), list(shape), dt))

    def ps(self, shape, dt, name=None):
        self.ntile += 1
        return self.stack.enter_context(self.nc.psum_tensor("%s_u%d" % (name or "p", self.ntile), list(shape), dt))

    def _need(self, eng, ev, waits):
        semkey, val, peng, isdma = ev
        if (not isdma) and peng == eng and eng == 'pe':
            return
        if self.seen[eng].get(semkey, 0) >= val:
            return
        self.seen[eng][semkey] = val
        waits.append((semkey, val))

    def op(self, eng, fn, reads=(), writes=(), dma=False):
        waits = []
        for b in reads:
            if b in self.lastw:
                self._need(eng, self.lastw[b], waits)
        for b in writes:
            if b in self.lastw:
                self._need(eng, self.lastw[b], waits)
            for ev in self.readers.get(b, ()):
                self._need(eng, ev, waits)
        if dma:
            i = self.dcnt[eng]
            self.dcnt[eng] += 1
            j = i % NDMA_SEM
            tgt = 16 * (i // NDMA_SEM + 1)
            semkey = ('d', eng, j)
            if tgt > 16:
                self._need(eng, (semkey, tgt - 16, eng, True), waits)
            ev = (semkey, tgt, eng, True)
            inc = (semkey, 16)
        else:
            self.cnt[eng] += 1
            semkey, val = self.ckey(eng, self.cnt[eng])
            ev = (semkey, val, eng, False)
            inc = (semkey, 1)
        for b in reads:
            self.readers.setdefault(b, []).append(ev)
        for b in writes:
            self.lastw[b] = ev
            self.readers[b] = []
        self.ops[eng].append((fn, waits, inc))
        return ev

    def ckey(self, eng, n):
        if eng != 'pe':
            return ('c', eng, 0), n
        return ('c', eng, (n - 1) // EPOCH), (n - 1) % EPOCH + 1

    def fence(self):
        allev = []
        for k in self.names:
            if self.cnt[k]:
                sk, v = self.ckey(k, self.cnt[k])
                allev.append((sk, v, k, False))
        for k in ('sp', 'pool', 'act'):
            for j in range(NDMA_SEM):
                n = (self.dcnt[k] - j + NDMA_SEM - 1) // NDMA_SEM
                if n > 0:
                    allev.append((('d', k, j), 16 * n, k, True))
        for eng in self.names:
            waits = []
            for semkey, val, k, isdma in allev:
                if self.seen[eng].get(semkey, 0) >= val:
                    continue
                if (not isdma) and k == eng:
                    continue
                self.seen[eng][semkey] = val
                waits.append((semkey, val))
            self.cnt[eng] += 1
            sk, v = self.ckey(eng, self.cnt[eng])
            self.ops[eng].append((lambda e: e.nop(), waits, (sk, 1)))

    def _sem(self, semkey):
        if semkey[0] == 'c':
            return self.csem[(semkey[1], semkey[2])]
        return self.dsem[semkey[1]][semkey[2]]

    def finish(self):
        waits = []
        for k in self.names:
            if self.cnt[k]:
                waits.append(self.ckey(k, self.cnt[k]))
        for k in ('sp', 'pool', 'act'):
            for j in range(NDMA_SEM):
                n = (self.dcnt[k] - j + NDMA_SEM - 1) // NDMA_SEM
                if n > 0:
                    waits.append((('d', k, j), 16 * n))
        self.final_waits = waits

    def emit(self):
        nc = self.nc
        self.finish()
        with nc.Block() as block:
            def run(name):
                def f(e):
                    for fn, waits, inc in self.ops[name]:
                        for semkey, val in waits:
                            e.wait_ge(self._sem(semkey), val)
                        ins = fn(e)
                        ins.then_inc(self._sem(inc[0]), inc[1])
                    if name == 'sp':
                        for semkey, val in self.final_waits:
                            e.wait_ge(self._sem(semkey), val)
                return f
            block.tensor(run('pe'))
            block.scalar(run('act'))
            block.vector(run('dve'))
            block.gpsimd(run('pool'))
            block.sync(run('sp'))

    def dma(self, q, out, in_, reads=(), writes=(), slow=False):
        if slow:
            return self.op(q, lambda e: e.dma_start(out=out, in_=in_, allow_slow_non_contiguous=True), reads, writes, dma=True)
        return self.op(q, lambda e: e.dma_start(out=out, in_=in_), reads, writes, dma=True)

    def mm(self, out, lhsT, rhs, start, stop, reads=(), writes=()):
        return self.op('pe', lambda e: e.matmul(out, lhsT=lhsT, rhs=rhs, start=start, stop=stop), reads, writes)

    def transpose(self, out, in_, ident, reads=(), writes=()):
        return self.op('pe', lambda e: e.transpose(out, in_, ident), reads, writes)

    def act(self, out, in_, func, reads=(), writes=(), **kw):
        return self.op('act', lambda e: e.activation(out, in_, func, **kw), reads, writes)

    def tt(self, eng, out, in0, in1, op, reads=(), writes=()):
        return self.op(eng, lambda e: e.tensor_tensor(out, in0, in1, op), reads, writes)

    def ts(self, eng, out, in0, s1, s2, op0, op1=None, reads=(), writes=(), **kw):
        if op1 is None:
            return self.op(eng, lambda e: e.tensor_scalar(out, in0, s1, s2, op0, **kw), reads, writes)
        return self.op(eng, lambda e: e.tensor_scalar(out, in0, s1, s2, op0, op1, **kw), reads, writes)

    def stt(self, eng, out, in0, scalar, in1, op0, op1, reads=(), writes=()):
        return self.op(eng, lambda e: e.scalar_tensor_tensor(out, in0, scalar, in1, op0, op1), reads, writes)

    def copy(self, eng, out, in_, reads=(), writes=()):
        if eng == 'act':
            return self.op(eng, lambda e: e.copy(out, in_), reads, writes)
        return self.op(eng, lambda e: e.tensor_copy(out, in_), reads, writes)

    def memset(self, eng, ap, val, writes=()):
        return self.op(eng, lambda e: e.memset(ap, val), (), writes)

import math, contextlib


D = 2048
EPS = 1e-6
LN_THETA = math.log(10000.0)
PI = math.pi


class Ctx:
    pass


def setup_common(P, NT):
    c = Ctx()
    c.ones = P.sb([128, 128], BF16, "ones")
    P.memset('pool', c.ones[:], 1.0, writes=['ones'])
    c.psn = 0
    c.pst = [P.ps([128, 512], F32, "psb%d" % i) for i in range(8)]
    c.NT = NT
    return c


def next_ps(c):
    i = c.psn % 8
    c.psn += 1
    return c.pst[i], "psb%d" % i


def rms_rstd(P, c, src, skey, KC, Dn, out_rstd, okey, tmpname):
    NT = c.NT
    ps, pk = next_ps(c)
    for kc in range(KC):
        sq = c.sq[kc % 2]
        sk = "sq%d" % (kc % 2)
        P.act(sq[:], src[:, kc, :], AF.Square, reads=[skey], writes=[sk])
        P.mm(ps[:, 0:NT], c.ones[:], sq[:], kc == 0, kc == KC - 1, reads=['ones', sk], writes=[pk])
    P.ts('dve', out_rstd[:], ps[:, 0:NT], 1.0 / Dn, EPS, ALU.mult, ALU.add, reads=[pk], writes=[okey])
    P.act(out_rstd[:], out_rstd[:], AF.Sqrt, reads=[okey], writes=[okey])
    P.op('dve', lambda e: e.reciprocal(out_rstd[:], out_rstd[:]), reads=[okey], writes=[okey])


def ridx_np():
    p = np.arange(128)
    r = np.zeros((128, 4), np.float32)
    r[:, 0] = p % 64
    r[:, 1] = np.where(p < 64, -1.0, 1.0)
    r[:, 2] = p % 32
    r[:, 3] = np.where((p % 64) < 32, -1.0, 1.0)
    return r

def make_mask(P, tile, key, QW, base, cm, step, tmp_i, tmp_key):
    P.op('pool', lambda e: e.iota(tmp_i[:, 0:QW], [[step, QW]], base=base, channel_multiplier=cm), (), [tmp_key])
    P.op('dve', lambda e: e.tensor_single_scalar(tile[:, 0:QW], tmp_i[:, 0:QW], 0, ALU.is_ge), [tmp_key], [key])


import math, contextlib


class Geo:
    def __init__(self, L):
        self.L = L
        self.OWN = L // 4
        self.OWN0 = L - self.OWN
        self.HAL0 = self.OWN0 - 2048
        self.QN = L - self.HAL0
        assert self.HAL0 >= 0 and self.HAL0 % 512 == 0 and self.OWN % 2048 == 0


class Phase:
    def __init__(self, P):
        self.P = P

    def __enter__(self):
        self.inner = contextlib.ExitStack()
        self.saved = self.P.stack
        self.P.stack = self.inner
        return self

    def __exit__(self, *a):
        self.P.fence()
        self.inner.close()
        self.P.stack = self.saved
        return False


def rope_tables_emit(P, posb, rc_col, sgn_ap, ang, rr, ri, cosT, sinT, keys):
    P.ts('dve', ang[:], posb[:], rc_col, None, ALU.mult, reads=[keys or 'posb', 'rc'], writes=['ang'])
    for (shift, outT, scale) in ((0.0, sinT, sgn_ap), (0.5 * PI, cosT, 1.0)):
        P.ts('dve', rr[:], ang[:], 1.0 / (2 * PI), shift / (2 * PI), ALU.mult, ALU.add, reads=['ang'], writes=['rr'])
        P.copy('dve', ri[:], rr[:], reads=['rr'], writes=['ri'])
        P.copy('dve', rr[:], ri[:], reads=['ri'], writes=['rr'])
        P.stt('dve', rr[:], rr[:], -2 * PI, ang[:], ALU.mult, ALU.add, reads=['rr', 'ang'], writes=['rr'])
        P.ts('dve', rr[:], rr[:], shift, PI, ALU.add, ALU.min, reads=['rr'], writes=['rr'])
        P.ts('dve', rr[:], rr[:], -PI, None, ALU.max, reads=['rr'], writes=['rr'])
        P.act(outT[:], rr[:], AF.Sin, reads=['rr', 'rsb'], writes=['tables'], scale=scale)


def make_rperm(P, tmp_i, tmp_key):
    r128 = P.sb([128, 128], BF16, "rperm128")
    r64 = P.sb([128, 128], BF16, "rperm64")
    ta = P.sb([128, 128], BF16, "rperm_t")
    P.op('pool', lambda e: e.iota(tmp_i[:, 0:128], [[-1, 128]], base=0, channel_multiplier=1), (), [tmp_key])
    for r, off in ((r128, 64), (r64, 32)):
        P.op('dve', lambda e, r=r, off=off: e.tensor_single_scalar(r[:], tmp_i[:, 0:128], off, ALU.is_equal), [tmp_key], ['rperm'])
        P.op('dve', lambda e, off=off: e.tensor_single_scalar(ta[:], tmp_i[:, 0:128], -off, ALU.is_equal), [tmp_key], ['rperm_t'])
        P.tt('dve', r[:], r[:], ta[:], ALU.add, reads=['rperm', 'rperm_t'], writes=['rperm'])
    return r128, r64

def phase1(P, nc, G, io):
    NT = 256
    EIN = 3416
    L, HAL0 = G.L, G.HAL0
    xT, pos = io['xT'], io['pos']
    with Phase(P):
        c = setup_common(P, NT)
        c.sq = [P.sb([128, NT], BF16, "sq%d" % i) for i in range(2)]
        wsb = P.sb([128, 16, EIN], BF16, "wsb")
        wuq = P.sb([128, 4, 1536], BF16, "wuq")
        wukv = P.sb([128, 2, 2048], BF16, "wukv")
        for kc in range(16):
            P.dma('pool', wsb[:, kc, :], io['l0_w_in'][kc * 128:(kc + 1) * 128, :], writes=['wsb'])
        for kc in range(4):
            P.dma('pool', wuq[:, kc, :], io['l0_mla_w_uq'][kc * 128:(kc + 1) * 128, :], writes=['wuq'])
        for kc in range(2):
            P.dma('pool', wukv[:, kc, :], io['l0_mla_w_ukv'][kc * 128:(kc + 1) * 128, :], writes=['wukv'])
        gsb = P.sb([128, 16], F32, "gsb")
        qnsb = P.sb([128, 4], F32, "qnsb")
        kvnsb = P.sb([128, 2], F32, "kvnsb")
        rsb = P.sb([128, 4], F32, "rsb")
        P.dma('sp', gsb[:], io['l0_norm_mix_pre'].rearrange("(kc p) -> p kc", p=128), writes=['gsb'], slow=True)
        P.dma('sp', qnsb[:], io['l0_mla_q_norm'].rearrange("(kc p) -> p kc", p=128), writes=['qnsb'], slow=True)
        P.dma('sp', kvnsb[:], io['l0_mla_kv_norm'].rearrange("(kc p) -> p kc", p=128), writes=['kvnsb'], slow=True)
        P.dma('sp', rsb[:], io['ridx'][:, :], writes=['rsb'])
        rc = P.sb([128, 2], F32, "rc")
        P.act(rc[:, 0:1], rsb[:, 0:1], AF.Exp, reads=['rsb'], writes=['rc'], scale=-2.0 * LN_THETA / 128.0)
        P.act(rc[:, 1:2], rsb[:, 2:3], AF.Exp, reads=['rsb'], writes=['rc'], scale=-2.0 * LN_THETA / 64.0)
        xsb = [P.sb([128, 16, NT], F32, "xs%d" % i) for i in range(2)]
        us = P.sb([128, 16, NT], BF16, "us")
        rstd = P.sb([128, NT], F32, "rstd")
        posbb = [P.sb([128, NT], F32, "posb%d" % i) for i in range(2)]
        ang = P.sb([128, NT], F32, "ang")
        rr = P.sb([128, NT], F32, "rr")
        ri = P.sb([128, NT], I32, "ri")
        cos128 = P.sb([128, NT], F32, "cos128")
        sin128 = P.sb([128, NT], F32, "sin128")
        cos64 = P.sb([128, NT], F32, "cos64")
        sin64 = P.sb([128, NT], F32, "sin64")
        cq = P.sb([128, 4, NT], F32, "cq")
        ckv = P.sb([128, 2, NT], F32, "ckv")
        cqn = P.sb([128, 4, NT], BF16, "cqn")
        ckvn = P.sb([128, 2, NT], BF16, "ckvn")
        t1 = [P.sb([128, NT], F32, "t1_%d" % i) for i in range(2)]
        t2 = [P.sb([128, NT], F32, "t2_%d" % i) for i in range(2)]
        ob = [P.sb([128, 512], BF16, "ob%d" % i) for i in range(4)]
        og = P.sb([128, 24], F32, "og")
        zb = [P.sb([128, NT], BF16, "zb%d" % i) for i in range(2)]
        tmp_i = P.sb([128, 128], I32, "tmp_i")
        r128, r64 = make_rperm(P, tmp_i, "tmp_i")
        state = {'ob': 0, 'tt': 0}

        def out_tile():
            i = state['ob'] % 4
            state['ob'] += 1
            return ob[i], "ob%d" % i

        def linear(wt, wkey, act, akey, KC, col0, ncols, prow0=0, ps=None, pk=None):
            if ps is None:
                ps, pk = next_ps(c)
            for kc in range(KC):
                P.mm(ps[prow0:prow0 + ncols, 0:NT], wt[:, kc, col0:col0 + ncols], act[:, kc, :],
                     kc == 0, kc == KC - 1, reads=[wkey, akey], writes=[pk])
            return ps, pk

        def tm_linear(wt, wkey, act, akey, KC, tb, col0, ncols):
            ps, pk = next_ps(c)
            for kc in range(KC):
                P.mm(ps[:, 0:ncols], act[:, kc, tb * 128:(tb + 1) * 128], wt[:, kc, col0:col0 + ncols],
                     kc == 0, kc == KC - 1, reads=[wkey, akey], writes=[pk])
            return ps, pk

        def rope_lin(wt, wkey, act, akey, KC, col0, ncols, cosT, sinT, dst):
            psz, kz = linear(wt, wkey, act, akey, KC, col0, ncols)
            i = state['tt'] % 2
            state['tt'] += 1
            zb_, zk = zb[i], "zb%d" % i
            P.copy('act', zb_[0:ncols, :], psz[0:ncols, 0:NT], reads=[kz], writes=[zk])
            psr, kr = next_ps(c)
            R = r128 if ncols == 128 else r64
            P.mm(psr[0:ncols, 0:NT], R[0:ncols, 0:ncols], zb_[0:ncols, :], True, True, reads=['rperm', zk], writes=[kr])
            a, b = t1[i], t2[i]
            P.tt('dve', a[0:ncols, :], zb_[0:ncols, :], cosT[0:ncols, :], ALU.mult, reads=[zk, 'tables'], writes=["t1_%d" % i])
            P.tt('dve', b[0:ncols, :], psr[0:ncols, 0:NT], sinT[0:ncols, :], ALU.mult, reads=[kr, 'tables'], writes=["t2_%d" % i])
            o, ok = out_tile()
            P.tt('pool', o[0:ncols, 0:NT], a[0:ncols, :], b[0:ncols, :], ALU.add, reads=["t1_%d" % i, "t2_%d" % i], writes=[ok])
            P.dma('sp', dst, o[0:ncols, 0:NT], reads=[ok])

        def plain_lin(wt, wkey, act, akey, KC, col0, ncols, dst):
            psz, kz = linear(wt, wkey, act, akey, KC, col0, ncols)
            o, ok = out_tile()
            P.copy('act', o[0:ncols, 0:NT], psz[0:ncols, 0:NT], reads=[kz], writes=[ok])
            P.dma('sp', dst, o[0:ncols, 0:NT], reads=[ok])

        o1, o2, o3, o4, o5 = 1024, 2560, 2584, 3096, 3352

        def prefetch(sbi):
            if sbi * NT >= L:
                return
            b = sbi % 2
            sl_ = slice(sbi * NT, (sbi + 1) * NT)
            for kc in range(16):
                P.dma('pool' if kc % 2 == 0 else 'act', xsb[b][:, kc, :], xT[kc * 128:(kc + 1) * 128, sl_], writes=['xs%d' % b])
            P.dma('pool', posbb[b][:], pos[0:1, sl_].to_broadcast([128, NT]), writes=['posb%d' % b])

        prefetch(0)
        for s0 in range(0, L, NT):
            full = s0 >= HAL0
            sl = slice(s0, s0 + NT)
            ql = slice(s0 - HAL0, s0 - HAL0 + NT)
            xs, xk = xsb[(s0 // NT) % 2], 'xs%d' % ((s0 // NT) % 2)
            posb, pbk = posbb[(s0 // NT) % 2], 'posb%d' % ((s0 // NT) % 2)
            prefetch(s0 // NT + 1)
            rope_tables_emit(P, posb, rc[:, 0:1], rsb[:, 1:2], ang, rr, ri, cos128, sin128, pbk)
            rope_tables_emit(P, posb, rc[:, 1:2], rsb[:, 3:4], ang, rr, ri, cos64, sin64, pbk)
            rms_rstd(P, c, xs, xk, 16, D, rstd, 'rstd', 'sq')
            for kc in range(16):
                P.stt('dve', us[:, kc, :], xs[:, kc, :], gsb[:, kc:kc + 1], rstd[:], ALU.mult, ALU.mult,
                      reads=[xk, 'gsb', 'rstd'], writes=['us'])
            if full:
                for h in range(8):
                    rope_lin(wsb, 'wsb', us, 'us', 16, h * 128, 128, cos128, sin128, io['QA'][h * 128:(h + 1) * 128, ql])
                for tb in range(NT // 128):
                    ps, pk = tm_linear(wsb, 'wsb', us, 'us', 16, tb, o2, 24)
                    P.act(og[:, 0:24], ps[:, 0:24], AF.Sigmoid, reads=[pk], writes=['og'])
                    P.dma('sp', io['GATE'][s0 - HAL0 + tb * 128:s0 - HAL0 + (tb + 1) * 128, :], og[:, 0:24], reads=['og'])
            for br in range(3):
                for g in range(2):
                    ch = br * 4 + g
                    rope_lin(wsb, 'wsb', us, 'us', 16, o1 + ch * 128, 128, cos128, sin128,
                             io['KFM'][(br * 2 + g) * 128:(br * 2 + g + 1) * 128, sl])
            for g in range(2):
                plain_lin(wsb, 'wsb', us, 'us', 16, o1 + (2 + g) * 128, 128, io['KFM'][(6 + g) * 128:(7 + g) * 128, sl])
            for br, dst in ((1, io['VS']), (2, io['VW'])):
                for tb in range(NT // 128):
                    ps, pk = tm_linear(wsb, 'wsb', us, 'us', 16, tb, o1 + (br * 4 + 2) * 128, 256)
                    o, ok = out_tile()
                    P.copy('act', o[:, 0:256], ps[:, 0:256], reads=[pk], writes=[ok])
                    P.dma('sp', dst[s0 + tb * 128:s0 + (tb + 1) * 128, :], o[:, 0:256], reads=[ok])
            if full:
                for i in range(4):
                    psz, kz = linear(wsb, 'wsb', us, 'us', 16, o3 + i * 128, 128)
                    P.copy('act', cq[:, i, :], psz[:, 0:NT], reads=[kz], writes=['cq'])
            for i in range(2):
                psz, kz = linear(wsb, 'wsb', us, 'us', 16, o4 + i * 128, 128)
                P.copy('act', ckv[:, i, :], psz[:, 0:NT], reads=[kz], writes=['ckv'])
            rope_lin(wsb, 'wsb', us, 'us', 16, o5, 64, cos64, sin64, io['KPE'][:, sl])
            if full:
                rms_rstd(P, c, cq, 'cq', 4, 512, rstd, 'rstd', 'sq')
                for kc in range(4):
                    P.stt('dve', cqn[:, kc, :], cq[:, kc, :], qnsb[:, kc:kc + 1], rstd[:], ALU.mult, ALU.mult,
                          reads=['cq', 'qnsb', 'rstd'], writes=['cqn'])
                for h in range(8):
                    plain_lin(wuq, 'wuq', cqn, 'cqn', 4, h * 192, 128, io['QMN'][h * 128:(h + 1) * 128, ql])
                    rope_lin(wuq, 'wuq', cqn, 'cqn', 4, h * 192 + 128, 64, cos64, sin64, io['QMP'][h * 64:(h + 1) * 64, ql])
            rms_rstd(P, c, ckv, 'ckv', 2, 256, rstd, 'rstd', 'sq')
            for kc in range(2):
                P.stt('dve', ckvn[:, kc, :], ckv[:, kc, :], kvnsb[:, kc:kc + 1], rstd[:], ALU.mult, ALU.mult,
                      reads=['ckv', 'kvnsb', 'rstd'], writes=['ckvn'])
            for h in range(8):
                plain_lin(wukv, 'wukv', ckvn, 'ckvn', 2, h * 256, 128, io['KMN'][h * 128:(h + 1) * 128, sl])
            for tb in range(NT // 128):
                for h0 in (0, 4):
                    ps, pk = next_ps(c)
                    for hh in range(4):
                        for kc in range(2):
                            P.mm(ps[:, hh * 128:(hh + 1) * 128], ckvn[:, kc, tb * 128:(tb + 1) * 128],
                                 wukv[:, kc, (h0 + hh) * 256 + 128:(h0 + hh) * 256 + 256], kc == 0, kc == 1,
                                 reads=['wukv', 'ckvn'], writes=[pk])
                    o, ok = out_tile()
                    P.copy('act', o[:, 0:512], ps[:, 0:512], reads=[pk], writes=[ok])
                    P.dma('sp', io['VM'][s0 + tb * 128:s0 + (tb + 1) * 128, h0 * 128:(h0 + 4) * 128], o[:, 0:512], reads=[ok])


def load_vaug(P, vaug, vkey, src_rows_fn, NKC, posv, q='sp'):
    for c0 in range(0, NKC, 32):
        c1 = min(NKC, c0 + 32)
        P.dma(q, vaug[:, c0:c1, 0:128], src_rows_fn(c0, c1), writes=[vkey])
    P.op('dve', lambda e: e.tensor_single_scalar(vaug[:, :, 128:129], posv, 0.0, ALU.is_ge), ['postm'], [vkey])


BIG = 1.0e4


def attn_consts(P, tmp_i):
    cst = {}
    for name, (base, cm, step) in (("m_diag", (0, -1, 1)), ("m_strict", (-1, 1, -1)), ("m_ge", (0, 1, -1))):
        t = P.sb([128, 128], BF16, name)
        make_mask(P, t, name, 128, base, cm, step, tmp_i, "tmp_i")
        cst[name] = t
    ident = P.sb([128, 128], BF16, "ident")
    P.tt('dve', ident[:], cst["m_diag"][:], cst["m_ge"][:], ALU.mult, reads=['m_diag', 'm_ge'], writes=['ident'])
    cst["ident"] = ident
    for dd in range(4):
        t = P.sb([128, 512], BF16, "dm%d" % dd)
        make_mask(P, t, "dm%d" % dd, 512, -128 * dd, -1, 1, tmp_i, "tmp_i")
        cst["dm%d" % dd] = t
    return cst


def emit_oT(P, cst, o_f32, okey, obf, obk, pst, osb, osk, dst):
    P.copy('act', obf[:], o_f32, reads=[okey], writes=[obk])
    P.transpose(pst[:, 512:640], obf[:], cst["ident"][:], reads=[obk, 'ident'], writes=['pst'])
    P.copy('dve', osb[:], pst[:, 512:640], reads=['pst'], writes=[osk])
    P.dma('sp', dst, osb[:], reads=[osk])


def phase2(P, nc, G, io):
    L, HAL0, QN = G.L, G.HAL0, G.QN
    NCMP = (L - 32) // 16 + 1
    NCC = (NCMP + 127) // 128
    NSLC = L // 64
    NJC = (NSLC + 127) // 128
    NKC = L // 128
    scale = 128 ** -0.5
    with Phase(P):
        tmp_i = P.sb([128, 512], I32, "tmp_i")
        cst = attn_consts(P, tmp_i)
        mdiag, mstrict, ident = cst["m_diag"], cst["m_strict"], cst["ident"]
        cmask = []
        for m in range(17):
            t = P.sb([128, 128], BF16, "cm%d" % m)
            make_mask(P, t, "cm%d" % m, 128, 128 * m - 31, -16, 1, tmp_i, "tmp_i")
            cmask.append(t)
        Emat = P.sb([128, 64, 128], BF16, "Emat")
        for half in range(2):
            src = ident[:].rearrange("p (m h) -> p m h", h=2)[:, :, half:half + 1]
            P.copy('dve', Emat[:, :, half * 64:(half + 1) * 64], src.to_broadcast([128, 64, 64]), reads=['ident'], writes=['Emat'])
        fpB = P.sb([128, 3], F32, "fpB")
        P.memset('dve', fpB[:], 0.0, writes=['fpB'])
        P.memset('dve', fpB[0:64, 0:1], BIG, writes=['fpB'])
        P.memset('dve', fpB[:, 1:2], BIG, writes=['fpB'])
        P.memset('dve', fpB[64:128, 2:3], BIG, writes=['fpB'])
        f0B = P.sb([128, NSLC], F32, "f0B")
        P.dma('sp', f0B[:], io['f0'][0:1, :].to_broadcast([128, NSLC]), writes=['f0B'])
        P.ts('dve', f0B[:], f0B[:], BIG, None, ALU.mult, reads=['f0B'], writes=['f0B'])
        cval = P.sb([128, NCC], F32, "cval")
        P.dma('sp', cval[:], io['cpos'][:, :], writes=['cval'])
        P.ts('dve', cval[:], cval[:], 0.0, None, ALU.is_ge, reads=['cval'], writes=['cval'])
        postm = P.sb([128, NKC], F32, "postm")
        P.dma('sp', postm[:], io['postm'][:, :], writes=['postm'])
        pa = [P.ps([128, 512], F32, "pa%d" % i) for i in range(4)]
        pss = [P.ps([128, 512], F32, "pss%d" % i) for i in range(2)]
        pst = P.ps([128, 1024], BF16, "pst")
        kcmpT = P.sb([128, NCC * 128], BF16, "kcmpT")
        VAc = P.sb([128, NCC, 385], BF16, "VAc")
        am = P.sb([128, 256], BF16, "am")
        for g in range(2):
            P.memset('pool', kcmpT[:], 0.0, writes=['kcmpT'])
            P.memset('pool', VAc[:], 0.0, writes=['VAc'])
            P.memset('pool', VAc[:, :, 128:129], 1.0, writes=['VAc'])
            for c in range(NCC):
                P.op('pool', lambda e, c=c: e.iota(tmp_i[:, 0:256], [[-4, 256]], base=128 * c + 1, channel_multiplier=1), (), ['tmp_i'])
                P.op('dve', lambda e: e.tensor_single_scalar(am[:], tmp_i[:, 0:256], 0, ALU.is_ge), ['tmp_i'], ['am'])
                P.op('pool', lambda e, c=c: e.iota(tmp_i[:, 0:256], [[4, 256]], base=3 - 128 * c, channel_multiplier=-1), (), ['tmp_i'])
                P.op('dve', lambda e, c=c: e.tensor_single_scalar(VAc[:, c, 129:385], tmp_i[:, 0:256], 0, ALU.is_ge), ['tmp_i'], ['VAc'])
                P.tt('dve', VAc[:, c, 129:385], VAc[:, c, 129:385], am[:], ALU.mult, reads=['VAc', 'am'], writes=['VAc'])
            with Phase(P):
                psx = P.ps([128, 512], F32, "psx")
                srcs = P.sb([128, L], BF16, "srcs")
                w1s = P.sb([128, 32, 128], BF16, "w1s")
                w2s = P.sb([128, 128], BF16, "w2s")
                peT = P.sb([128, 32], BF16, "peT")
                cb = P.sb([128, 1], F32, "cb")
                xs_ = P.sb([128, 512], F32, "cx")
                x2 = P.sb([128, 512], F32, "cx2")
                gl = P.sb([128, NCC * 128], BF16, "gl")
                for which in range(2):
                    w1, w2, pe, srow = ((io['l0_cmp_w1_k'], io['l0_cmp_w2_k'], io['l0_cmp_pe_k'], g),
                                        (io['l0_cmp_w1_v'], io['l0_cmp_w2_v'], io['l0_cmp_pe_v'], 6 + g))[which]
                    P.dma('sp', srcs[:], io['KFM'][srow * 128:(srow + 1) * 128, :], writes=['srcs'])
                    P.dma('pool', w1s[:], w1.rearrange("(l d) h -> d l h", d=128), writes=['w1s'])
                    P.dma('pool', w2s[:], w2[:, :], writes=['w2s'])
                    P.dma('pool', peT[:], pe.rearrange("l d -> d l"), writes=['peT'], slow=True)
                    for l in range(32):
                        P.mm(psx[:, 0:1], w1s[:, l, :], peT[:, l:l + 1], l == 0, l == 31, reads=['w1s', 'peT'], writes=['psx'])
                    P.copy('dve', cb[:], psx[:, 0:1], reads=['psx'], writes=['cb'])
                    P.memset('pool', gl[:], 0.0, writes=['gl'])
                    src3 = srcs[:].rearrange("p (n r) -> p n r", r=16)
                    for n0 in range(0, NCMP, 512):
                        ncols = min(512, NCMP - n0)
                        for l in range(32):
                            P.mm(psx[:, 0:ncols], w1s[:, l, :], src3[:, n0 + l // 16:n0 + l // 16 + ncols, l % 16],
                                 l == 0, l == 31, reads=['w1s', 'srcs'], writes=['psx'])
                        xa = xs_[:, 0:ncols]
                        xb = x2[:, 0:ncols]
                        P.act(xa, psx[:, 0:ncols], AF.Identity, reads=['psx', 'cb'], writes=['cx'], bias=cb[:, 0:1], scale=1.0)
                        P.tt('dve', xb, xa, xa, ALU.mult, reads=['cx'], writes=['cx2'])
                        P.ts('dve', xb, xb, 0.044715, 1.0, ALU.mult, ALU.add, reads=['cx2'], writes=['cx2'])
                        P.tt('dve', xb, xb, xa, ALU.mult, reads=['cx', 'cx2'], writes=['cx2'])
                        P.act(xb, xb, AF.Tanh, reads=['cx2'], writes=['cx2'], scale=math.sqrt(2.0 / math.pi))
                        P.ts('dve', xb, xb, 0.5, 0.5, ALU.mult, ALU.add, reads=['cx2'], writes=['cx2'])
                        P.tt('dve', gl[:, n0:n0 + ncols], xb, xa, ALU.mult, reads=['cx', 'cx2'], writes=['gl'])
                    if which == 0:
                        for n0 in range(0, NCMP, 512):
                            ncols = min(512, NCMP - n0)
                            P.mm(psx[:, 0:ncols], w2s[:], gl[:, n0:n0 + ncols], True, True, reads=['w2s', 'gl'], writes=['psx'])
                            P.copy('dve', kcmpT[:, n0:n0 + ncols], psx[:, 0:ncols], reads=['psx'], writes=['kcmpT'])
                    else:
                        for c in range(NCC):
                            P.mm(psx[:, 0:128], gl[:, c * 128:(c + 1) * 128], w2s[:], True, True, reads=['w2s', 'gl'], writes=['psx'])
                            P.copy('dve', VAc[:, c, 0:128], psx[:, 0:128], reads=['psx'], writes=['VAc'])
            for c in range(NCC):
                P.ts('dve', VAc[:, c, :], VAc[:, c, :], cval[:, c:c + 1], None, ALU.mult, reads=['VAc', 'cval'], writes=['VAc'])
            with Phase(P):
                pss3 = pss + [P.ps([128, 512], F32, "pss2")]
                kss = P.sb([128, L], BF16, "kss")
                kws = P.sb([128, L], BF16, "kws")
                P.dma('sp', kss[:], io['KFM'][(2 + g) * 128:(3 + g) * 128, :], writes=['kss'])
                P.dma('act', kws[:], io['KFM'][(4 + g) * 128:(5 + g) * 128, :], writes=['kws'])
                vsa = P.sb([128, NKC, 129], BF16, "vsa")
                vwa = P.sb([128, NKC, 129], BF16, "vwa")
                load_vaug(P, vsa, 'vsa', lambda c0, c1: io['VS'][c0 * 128:c1 * 128, g * 128:(g + 1) * 128].rearrange("(c p) d -> p c d", p=128), NKC, postm[:, :].rearrange("p (c o) -> p c o", o=1), 'sp')
                load_vaug(P, vwa, 'vwa', lambda c0, c1: io['VW'][c0 * 128:c1 * 128, g * 128:(g + 1) * 128].rearrange("(c p) d -> p c d", p=128), NKC, postm[:, :].rearrange("p (c o) -> p c o", o=1), 'act')
                gsb = P.sb([128, QN // 128, 12], F32, "gsb")
                P.dma('sp', gsb[:], io['GATE'][:, 12 * g:12 * g + 12].rearrange("(c p) g -> p c g", p=128), writes=['gsb'])
                qblk = [P.sb([128, 4, 512], BF16, "qblk%d" % i) for i in range(2)]
                pt = [P.sb([128, 512], BF16, "pt%d" % i) for i in range(5)]
                rden = P.sb([128, 8], F32, "rden")
                imp = P.sb([128, NSLC], F32, "imp")
                work = P.sb([128, NSLC], F32, "work")
                m8 = P.sb([128, 16], F32, "m8")
                thr = P.sb([128, 1], F32, "thr")
                sel = P.sb([128, NJC * 128], BF16, "sel")
                P.memset('pool', sel[:], 0.0, writes=['sel'])
                biasT = P.sb([128, NJC, 512], BF16, "biasT")
                P.memset('pool', biasT[:], 0.0, writes=['biasT'])
                oc = [P.sb([128, 128], F32, "oc%d" % h) for h in range(4)]
                opart = [[P.sb([128, 128], F32, "op%d_%d" % (h, qs)) for qs in range(4)] for h in range(4)]
                ofin = [P.sb([128, 128], F32, "ofin%d" % i) for i in range(2)]
                obf = [P.sb([128, 128], BF16, "obf%d" % i) for i in range(2)]
                osb = [P.sb([128, 128], BF16, "osb%d" % i) for i in range(2)]
                rg = P.sb([128, 8], F32, "rg")
                cnt = {'s': 0, 'pt': 0, 'o': 0}

                def score_exp(klhs, kkey, kj, qh, qkey, qoff, W, mask, mkey, bias=False):
                    si = cnt['s'] % 3
                    cnt['s'] += 1
                    ps, pk = pss3[si], "pss%d" % si
                    P.mm(ps[:, 0:W], klhs[:, kj * 128:(kj + 1) * 128], qh[:, qoff:qoff + W], True, not bias,
                         reads=[kkey, qkey], writes=[pk])
                    if bias:
                        P.mm(ps[:, 0:W], Emat[:, kj % 64, :], biasT[:, kj // 64, 0:W], False, True,
                             reads=['Emat', 'biasT'], writes=[pk])
                    pi = cnt['pt'] % 5
                    cnt['pt'] += 1
                    p_, ptk = pt[pi], "pt%d" % pi
                    P.act(p_[:, 0:W], ps[:, 0:W], AF.Exp, reads=[pk], writes=[ptk], scale=scale)
                    if mask is not None:
                        P.tt('dve', p_[:, 0:W], p_[:, 0:W], mask[:, 0:W], ALU.mult, reads=[ptk, mkey], writes=[ptk])
                    return p_, ptk

                for stile in range(HAL0 // 512, L // 512):
                    qb, qbk = qblk[stile % 2], "qblk%d" % (stile % 2)
                    for h in range(4):
                        P.dma('sp' if h % 2 == 0 else 'act', qb[:, h, :],
                              io['QA'][(4 * g + h) * 128:(4 * g + h + 1) * 128, stile * 512 - HAL0:stile * 512 - HAL0 + 512], writes=[qbk])
                    def emit_pvw(p_, ptk, h, kj, first, last):
                        P.mm(pa[h][:, 0:129], p_[:, 0:128], vwa[:, kj, :], first, last, reads=[ptk, 'vwa'], writes=['pa%d' % h])
                    def emit_pva(p_, ptk, h, c, first, last):
                        P.mm(pa[h][:, 0:385], p_[:, 0:128], VAc[:, c, :], first, last, reads=[ptk, 'VAc'], writes=['pa%d' % h])
                    for qs in range(4):
                        i = 4 * stile + qs
                        q0 = 128 * i
                        gi = (q0 - HAL0) // 128
                        qoff = qs * 128
                        vis = [(c, (q0 - 2048 * c) // 128) for c in range(NCC) if (q0 - 2048 * c) >= 0]
                        penda = None
                        for h in range(4):
                            for idx, (c, m) in enumerate(vis):
                                mk = cmask[m] if m <= 16 else None
                                p_, ptk = score_exp(kcmpT, 'kcmpT', c, qb[:, h, :], qbk, qoff, 128, mk, "cm%d" % m if m <= 16 else None)
                                if penda is not None:
                                    emit_pva(*penda)
                                penda = (p_, ptk, h, c, idx == 0, idx == len(vis) - 1)
                        emit_pva(*penda)
                        for h in range(4):
                            P.ts('dve', rden[:, h:h + 1], pa[h][:, 128:129], 1e-30, None, ALU.max, reads=['pa%d' % h], writes=['rden'])
                            P.op('dve', lambda e, h=h: e.reciprocal(rden[:, h:h + 1], rden[:, h:h + 1]), ['rden'], ['rden'])
                            P.ts('dve', oc[h][:], pa[h][:, 0:128], rden[:, h:h + 1], None, ALU.mult,
                                 reads=['pa%d' % h, 'rden'], writes=['oc%d' % h])
                            if h == 0:
                                P.ts('dve', imp[:], pa[h][:, 129:129 + NSLC], rden[:, h:h + 1], None, ALU.mult,
                                     reads=['pa%d' % h, 'rden'], writes=['imp'])
                            else:
                                P.stt('dve', imp[:], pa[h][:, 129:129 + NSLC], rden[:, h:h + 1], imp[:], ALU.mult, ALU.add,
                                      reads=['pa%d' % h, 'rden', 'imp'], writes=['imp'])
                        lo = max(0, 2 * i - 1)
                        P.tt('dve', imp[:, lo:2 * i + 2], imp[:, lo:2 * i + 2], fpB[:, lo - (2 * i - 1):3], ALU.add,
                             reads=['imp', 'fpB'], writes=['imp'])
                        P.tt('dve', imp[:], imp[:], f0B[:], ALU.add, reads=['imp', 'f0B'], writes=['imp'])
                        P.op('dve', lambda e: e.max(out=m8[:, 0:8], in_=imp[:]), ['imp'], ['m8'])
                        P.op('dve', lambda e: e.match_replace(out=work[:], in_to_replace=m8[:, 0:8], in_values=imp[:], imm_value=-1.0e9),
                             ['imp', 'm8'], ['work'])
                        P.op('dve', lambda e: e.max(out=m8[:, 8:16], in_=work[:]), ['work'], ['m8'])
                        P.op('dve', lambda e: e.tensor_reduce(out=thr[:], in_=m8[:, 8:16], axis=AX.X, op=ALU.min), ['m8'], ['thr'])
                        P.ts('dve', sel[:, 0:NSLC], imp[:], thr[:, 0:1], None, ALU.is_ge, reads=['imp', 'thr'], writes=['sel'])
                        pendw = None
                        wl = list(range(max(0, i - 4), i + 1))
                        for h in range(4):
                            for idx, kj in enumerate(wl):
                                mk, mkey = (mdiag, 'm_diag') if kj == i else ((mstrict, 'm_strict') if kj == i - 4 else (None, None))
                                p_, ptk = score_exp(kws, 'kws', kj, qb[:, h, :], qbk, qoff, 128, mk, mkey)
                                if pendw is not None:
                                    emit_pvw(*pendw)
                                pendw = (p_, ptk, h, kj, idx == 0, idx == len(wl) - 1)
                        emit_pvw(*pendw)
                        for h in range(4):
                            accw, acwk = pa[h], 'pa%d' % h
                            P.ts('dve', rg[:, h:h + 1], accw[:, 128:129], 1e-30, None, ALU.max, reads=[acwk], writes=['rg'])
                            P.op('dve', lambda e, h=h: e.reciprocal(rg[:, h:h + 1], rg[:, h:h + 1]), ['rg'], ['rg'])
                            P.tt('dve', rg[:, h:h + 1], rg[:, h:h + 1], gsb[:, gi, 3 * h + 2:3 * h + 3], ALU.mult, reads=['rg', 'gsb'], writes=['rg'])
                            op_, opk = opart[h][qs], "op%d_%d" % (h, qs)
                            P.ts('dve', op_[:], oc[h][:], gsb[:, gi, 3 * h:3 * h + 1], None, ALU.mult, reads=['oc%d' % h, 'gsb'], writes=[opk])
                            P.stt('dve', op_[:], accw[:, 0:128], rg[:, h:h + 1], op_[:], ALU.mult, ALU.add, reads=[acwk, 'rg', opk], writes=[opk])
                        for jc in range(NJC):
                            rows = min(128, NSLC - jc * 128)
                            P.transpose(pst[0:rows, jc * 128:(jc + 1) * 128], sel[:, jc * 128:jc * 128 + rows], ident[:],
                                        reads=['sel', 'ident'], writes=['pst'])
                            P.ts('dve', biasT[0:rows, jc, qoff:qoff + 128], pst[0:rows, jc * 128:(jc + 1) * 128], -1.0, 30000.0,
                                 ALU.add, ALU.mult, reads=['pst'], writes=['biasT'])
                    def emit_pvb(p_, ptk, kj, dd, stile):
                        for qs in range(max(dd, 0), 4):
                            P.mm(pa[qs][:, 0:129], p_[:, qs * 128:(qs + 1) * 128], vsa[:, kj, :], kj == 0, kj == 4 * stile + qs,
                                 reads=[ptk, 'vsa'], writes=['pa%d' % qs])
                    pendb = []
                    for h in range(4):
                        nk = 4 * stile + 4
                        for kj in range(nk):
                            dd = kj - 4 * stile
                            mk, mkey = (cst["dm%d" % dd], "dm%d" % dd) if dd >= 0 else (None, None)
                            p_, ptk = score_exp(kss, 'kss', kj, qb[:, h, :], qbk, 0, 512, mk, mkey, bias=True)
                            if len(pendb) == 2:
                                emit_pvb(*pendb.pop(0))
                            pendb.append((p_, ptk, kj, dd, stile))
                        while pendb:
                            emit_pvb(*pendb.pop(0))
                        for qs in range(4):
                            gi = (128 * (4 * stile + qs) - HAL0) // 128
                            P.ts('dve', rg[:, 4:5], pa[qs][:, 128:129], 1e-30, None, ALU.max, reads=['pa%d' % qs], writes=['rg'])
                            P.op('dve', lambda e: e.reciprocal(rg[:, 4:5], rg[:, 4:5]), ['rg'], ['rg'])
                            P.tt('dve', rg[:, 4:5], rg[:, 4:5], gsb[:, gi, 3 * h + 1:3 * h + 2], ALU.mult, reads=['rg', 'gsb'], writes=['rg'])
                            oi = cnt['o'] % 2
                            cnt['o'] += 1
                            P.stt('dve', ofin[oi][:], pa[qs][:, 0:128], rg[:, 4:5], opart[h][qs][:], ALU.mult, ALU.add,
                                  reads=['pa%d' % qs, 'rg', "op%d_%d" % (h, qs)], writes=['ofin%d' % oi])
                            tcol = 128 * (4 * stile + qs) - HAL0
                            emit_oT(P, cst, ofin[oi][:], 'ofin%d' % oi, obf[oi], 'obf%d' % oi, pst, osb[oi], 'osb%d' % oi,
                                    io['OT'][(4 * g + h) * 128:(4 * g + h + 1) * 128, tcol:tcol + 128])


def phase3(P, nc, G, io):
    L, HAL0, QN = G.L, G.HAL0, G.QN
    NKC = L // 128
    scale = 192 ** -0.5
    with Phase(P):
        tmp_i = P.sb([128, 512], I32, "tmp_i")
        cst = attn_consts(P, tmp_i)
        postm = P.sb([128, NKC], F32, "postm")
        P.dma('sp', postm[:], io['postm'][:, :], writes=['postm'])
        kta = P.sb([128, L], BF16, "kta")
        ktb = P.sb([128, L], BF16, "ktb")
        qta = P.sb([128, QN], BF16, "qta")
        qtb = P.sb([128, QN], BF16, "qtb")
        vaug = P.sb([128, NKC, 129], BF16, "vaug")
        ps_s = [P.ps([128, 512], F32, "ps_s%d" % i) for i in range(3)]
        ps_a = [P.ps([128, 512], F32, "ps_a%d" % i) for i in range(4)]
        pst = P.ps([128, 1024], BF16, "pst")
        pt = [P.sb([128, 512], BF16, "pt%d" % i) for i in range(4)]
        P.memset('pool', ktb[64:128, :], 0.0, writes=['ktb'])
        P.memset('pool', qtb[64:128, :], 0.0, writes=['qtb'])
        ofin = [P.sb([128, 128], F32, "ofin%d" % i) for i in range(2)]
        obf = [P.sb([128, 128], BF16, "obf%d" % i) for i in range(2)]
        osb = [P.sb([128, 128], BF16, "osb%d" % i) for i in range(2)]
        rden = [P.sb([128, 1], F32, "rden%d" % i) for i in range(2)]
        cnt = {'s': 0, 'pt': 0, 'o': 0}
        P.dma('sp', ktb[0:64, :], io['KPE'][:, :], writes=['ktb'])
        for h in range(8):
            P.dma('sp', kta[:], io['KMN'][h * 128:(h + 1) * 128, :], writes=['kta'])
            P.dma('act', qta[:], io['QMN'][h * 128:(h + 1) * 128, :], writes=['qta'])
            P.dma('act', qtb[0:64, :], io['QMP'][h * 64:(h + 1) * 64, :], writes=['qtb'])
            load_vaug(P, vaug, 'vaug', lambda c0, c1: io['VM'][c0 * 128:c1 * 128, h * 128:(h + 1) * 128].rearrange("(c p) d -> p c d", p=128), NKC, postm[:, :].rearrange("p (c o) -> p c o", o=1), 'sp')
            def emit_pv(ptt, ptk, kj, dd, stile):
                for qs in range(max(dd, 0), 4):
                    P.mm(ps_a[qs][:, 0:129], ptt[:, qs * 128:(qs + 1) * 128], vaug[:, kj, :], kj == 0, kj == 4 * stile + qs,
                         reads=[ptk, 'vaug'], writes=['ps_a%d' % qs])
            pend = []
            for stile in range(HAL0 // 512, L // 512):
                qoff = stile * 512 - HAL0
                nk = 4 * stile + 4
                for kj in range(nk):
                    dd = kj - 4 * stile
                    si = cnt['s'] % 3
                    cnt['s'] += 1
                    pss, psk = ps_s[si], "ps_s%d" % si
                    P.mm(pss[:, 0:512], kta[:, kj * 128:(kj + 1) * 128], qta[:, qoff:qoff + 512], True, False, reads=['kta', 'qta'], writes=[psk])
                    P.mm(pss[:, 0:512], ktb[:, kj * 128:(kj + 1) * 128], qtb[:, qoff:qoff + 512], False, True, reads=['ktb', 'qtb'], writes=[psk])
                    pi = cnt['pt'] % 4
                    cnt['pt'] += 1
                    ptt, ptk = pt[pi], "pt%d" % pi
                    P.act(ptt[:], pss[:, 0:512], AF.Exp, reads=[psk], writes=[ptk], scale=scale)
                    if dd >= 0:
                        P.tt('dve', ptt[:], ptt[:], cst["dm%d" % dd][:], ALU.mult, reads=[ptk, "dm%d" % dd], writes=[ptk])
                    if len(pend) == 2:
                        emit_pv(*pend.pop(0))
                    pend.append((ptt, ptk, kj, dd, stile))
                while pend:
                    emit_pv(*pend.pop(0))
                for qs in range(4):
                    oi = cnt['o'] % 2
                    cnt['o'] += 1
                    rd, rk = rden[oi], "rden%d" % oi
                    P.ts('dve', rd[:], ps_a[qs][:, 128:129], 1e-30, None, ALU.max, reads=['ps_a%d' % qs], writes=[rk])
                    P.op('dve', lambda e, rd=rd: e.reciprocal(rd[:], rd[:]), [rk], [rk])
                    P.ts('dve', ofin[oi][:], ps_a[qs][:, 0:128], rd[:, 0:1], None, ALU.mult, reads=['ps_a%d' % qs, rk], writes=['ofin%d' % oi])
                    tcol = qoff + qs * 128
                    emit_oT(P, cst, ofin[oi][:], 'ofin%d' % oi, obf[oi], 'obf%d' % oi, pst, osb[oi], 'osb%d' % oi,
                            io['OT'][1024 + h * 128:1024 + (h + 1) * 128, tcol:tcol + 128])


DFF = 8192


def phase_post(P, nc, G, io, layer):
    NT = 512
    if layer == 0:
        KCo, NTOK, h_src, h_off, o_src, o_dst = 16, G.QN, io['xT'], G.HAL0, io['OT'], io['H2T']
    else:
        KCo, NTOK, h_src, h_off, o_src, o_dst = 8, G.OWN, io['H2T'], 2048, io['OT1'], io['out']
    Lp = "l%d_" % layer
    w_out, w_ff1, w_ff2 = io[Lp + 'w_out'], io[Lp + 'w_ff1'], io[Lp + 'w_ff2']
    with Phase(P):
        c = setup_common(P, NT)
        c.sq = [P.sb([128, NT], BF16, "sq%d" % i) for i in range(2)]
        gs = P.sb([128, 4, 16], F32, "gs")
        gl_ = [io[Lp + 'norm_mix_post'], io[Lp + 'norm_ffn_pre'], io[Lp + 'norm_ffn_post']] + ([io['l1_norm_mix_pre']] if layer == 0 else [])
        for i, g in enumerate(gl_):
            P.dma('sp', gs[:, i, :], g.rearrange("(kc p) -> p kc", p=128), writes=['gs'], slow=True)
        hs = P.sb([128, 16, NT], F32, "hs")
        us = P.sb([128, 16, NT], BF16, "us")
        ms = P.sb([128, 16, NT], F32, "ms")
        f1 = P.sb([128, 64, NT], BF16, "f1")
        rstd = P.sb([128, NT], F32, "rstd")
        tmp = [P.sb([128, NT], F32, "tmp%d" % i) for i in range(2)]
        wb = [P.sb([128, 4096], BF16, "wb%d" % i) for i in range(4)]
        ob = [P.sb([128, NT], BF16, "ob%d" % i) for i in range(3)]
        cnt = {'w': 0, 'ob': 0, 't': 0}
        if layer == 0:
            rsb = P.sb([128, 4], F32, "rsb")
            P.dma('sp', rsb[:], io['ridx'][:, :], writes=['rsb'])
            rc = P.sb([128, 2], F32, "rc")
            P.act(rc[:, 0:1], rsb[:, 0:1], AF.Exp, reads=['rsb'], writes=['rc'], scale=-2.0 * LN_THETA / 128.0)
            posb = P.sb([128, NT], F32, "posb")
            ang = P.sb([128, NT], F32, "ang")
            ri = P.sb([128, NT], I32, "ri")
            rr_ = P.sb([128, NT], F32, "rr")
            zb = [P.sb([128, NT], BF16, "zb%d" % i) for i in range(2)]
            tmp_i = P.sb([128, 128], I32, "tmp_i")
            r128, r64 = make_rperm(P, tmp_i, "tmp_i")
            cosT = P.sb([128, NT], F32, "cosT")
            sinT = P.sb([128, NT], F32, "sinT")

        WB = io['WB%d' % layer]

        def wload(src_ap_fn, nseg, segw, bid, first):
            i = cnt['w'] % 4
            cnt['w'] += 1
            t, k = wb[i], "wb%d" % i
            n = nseg * segw
            assert n <= 4096
            view = t[:, 0:n].rearrange("p (s w) -> p s w", w=segw)
            if first:
                P.dma('pool', view[:, :, :], src_ap_fn(0, nseg), writes=[k])
                P.dma('sp', WB[bid, :, 0:n], t[:, 0:n], reads=[k], writes=['WB_%d' % bid])
            else:
                P.dma('sp' if cnt['w'] % 2 == 0 else 'pool', t[:, 0:n], WB[bid, :, 0:n], reads=['WB_%d' % bid], writes=[k])
            return view, k

        def add_norm(src, skey, gi):
            rms_rstd(P, c, src, skey, 16, D, rstd, 'rstd', 'sq')
            for kc in range(16):
                i = cnt['t'] % 2
                cnt['t'] += 1
                P.stt('dve', tmp[i][:], src[:, kc, :], gs[:, gi, kc:kc + 1], rstd[:], ALU.mult, ALU.mult,
                      reads=[skey, 'gs', 'rstd'], writes=['tmp%d' % i])
                P.tt('pool', hs[:, kc, :], hs[:, kc, :], tmp[i][:], ALU.add, reads=['hs', 'tmp%d' % i], writes=['hs'])

        def norm_to_us(gi):
            rms_rstd(P, c, hs, 'hs', 16, D, rstd, 'rstd', 'sq')
            for kc in range(16):
                P.stt('dve', us[:, kc, :], hs[:, kc, :], gs[:, gi, kc:kc + 1], rstd[:], ALU.mult, ALU.mult,
                      reads=['hs', 'gs', 'rstd'], writes=['us'])

        for s0 in range(0, NTOK, NT):
            sl = slice(s0, s0 + NT)
            hl = slice(h_off + s0, h_off + s0 + NT)
            for kc in range(16):
                P.dma('sp' if kc % 2 == 0 else 'act', hs[:, kc, :], h_src[kc * 128:(kc + 1) * 128, hl], writes=['hs'])
            for kc in range(KCo):
                P.dma('act' if kc % 2 == 0 else 'sp', us[:, kc, :], o_src[kc * 128:(kc + 1) * 128, sl], writes=['us'])
            for cb in range(8):
                wv, wk = wload(lambda a, b, cb=cb: w_out[a * 128:b * 128, cb * 256:(cb + 1) * 256].rearrange("(s p) w -> p s w", p=128), KCo, 256, cb, s0 == 0)
                for j in range(2):
                    ps, pk = next_ps(c)
                    for kc in range(KCo):
                        P.mm(ps[:, 0:NT], wv[:, kc, j * 128:(j + 1) * 128], us[:, kc, :], kc == 0, kc == KCo - 1,
                             reads=[wk, 'us'], writes=[pk])
                    P.copy('act', ms[:, cb * 2 + j, :], ps[:, 0:NT], reads=[pk], writes=['ms'])
            add_norm(ms, 'ms', 0)
            norm_to_us(1)
            for cb in range(32):
                wv, wk = wload(lambda a, b, cb=cb: w_ff1[a * 128:b * 128, cb * 256:(cb + 1) * 256].rearrange("(s p) w -> p s w", p=128), 16, 256, 8 + cb, s0 == 0)
                for j in range(2):
                    ps, pk = next_ps(c)
                    for kc in range(16):
                        P.mm(ps[:, 0:NT], wv[:, kc, j * 128:(j + 1) * 128], us[:, kc, :], kc == 0, kc == 15,
                             reads=[wk, 'us'], writes=[pk])
                    i = cnt['t'] % 2
                    cnt['t'] += 1
                    P.act(tmp[i][:], ps[:, 0:NT], AF.Relu, reads=[pk], writes=['tmp%d' % i])
                    P.tt('dve', f1[:, cb * 2 + j, :], tmp[i][:], tmp[i][:], ALU.mult, reads=['tmp%d' % i], writes=['f1'])
            for cb in range(16):
                ps, pk = next_ps(c)
                for hf in range(2):
                    wv, wk = wload(lambda a, b, cb=cb, hf=hf: w_ff2[(hf * 32 + a) * 128:(hf * 32 + b) * 128, cb * 128:(cb + 1) * 128].rearrange("(s p) w -> p s w", p=128),
                                   32, 128, 40 + cb * 2 + hf, s0 == 0)
                    for kc in range(32):
                        P.mm(ps[:, 0:NT], wv[:, kc, :], f1[:, hf * 32 + kc, :], hf == 0 and kc == 0, hf == 1 and kc == 31,
                             reads=[wk, 'f1'], writes=[pk])
                P.copy('act', ms[:, cb, :], ps[:, 0:NT], reads=[pk], writes=['ms'])
            add_norm(ms, 'ms', 2)
            for kc in range(16):
                P.dma('sp' if kc % 2 == 0 else 'act', o_dst[kc * 128:(kc + 1) * 128, sl], hs[:, kc, :], reads=['hs'])
            if layer == 0:
                w_in = io['l1_w_in']
                P.dma('sp', posb[:], io['pos'][0:1, hl].to_broadcast([128, NT]), writes=['posb'])
                rope_tables_emit(P, posb, rc[:, 0:1], rsb[:, 1:2], ang, rr_, ri, cosT, sinT, None)
                norm_to_us(3)
                for cb in range(36):
                    wv, wk = wload(lambda a, b, cb=cb: w_in[a * 128:b * 128, cb * 256:(cb + 1) * 256].rearrange("(s p) w -> p s w", p=128), 16, 256, 72 + cb, s0 == 0)
                    ch0 = cb * 2
                    ttype, g = (ch0 // 8) % 3, ch0 // 24
                    if ttype == 2:
                        for tb in range(NT // 128):
                            ps, pk = next_ps(c)
                            for kc in range(16):
                                P.mm(ps[:, 0:256], us[:, kc, tb * 128:(tb + 1) * 128], wv[:, kc, :], kc == 0, kc == 15,
                                     reads=[wk, 'us'], writes=[pk])
                            oi = cnt['ob'] % 3
                            cnt['ob'] += 1
                            o, ok = ob[oi], "ob%d" % oi
                            P.copy('act', o[:, 0:256], ps[:, 0:256], reads=[pk], writes=[ok])
                            hc0 = (ch0 % 8) * 128
                            P.dma('sp', io['V1'][s0 + tb * 128:s0 + (tb + 1) * 128, g * 1024 + hc0:g * 1024 + hc0 + 256], o[:, 0:256], reads=[ok])
                        continue
                    for j in range(2):
                        ch = ch0 + j
                        h = ch % 8
                        ps, pk = next_ps(c)
                        for kc in range(16):
                            P.mm(ps[:, 0:NT], wv[:, kc, j * 128:(j + 1) * 128], us[:, kc, :], kc == 0, kc == 15,
                                 reads=[wk, 'us'], writes=[pk])
                        zi = cnt['ob'] % 2
                        zb_, zk = zb[zi], "zb%d" % zi
                        P.copy('act', zb_[:], ps[:, 0:NT], reads=[pk], writes=[zk])
                        pr, prk = next_ps(c)
                        P.mm(pr[:, 0:NT], r128[:], zb_[:], True, True, reads=['rperm', zk], writes=[prk])
                        oi = cnt['ob'] % 3
                        cnt['ob'] += 1
                        o, ok = ob[oi], "ob%d" % oi
                        P.tt('dve', tmp[0][:], zb_[:], cosT[:], ALU.mult, reads=[zk, 'tables'], writes=['tmp0'])
                        P.tt('dve', tmp[1][:], pr[:, 0:NT], sinT[:], ALU.mult, reads=[prk, 'tables'], writes=['tmp1'])
                        P.tt('pool', o[:], tmp[0][:], tmp[1][:], ALU.add, reads=['tmp0', 'tmp1'], writes=[ok])
                        zr = ((g * 2 + ttype) * 8 + h) * 128
                        P.dma('sp', io['Z1'][zr:zr + 128, sl], o[:], reads=[ok])


def phase5(P, nc, G, io):
    QN, OWN = G.QN, G.OWN
    NC1 = QN // 128
    M4 = QN // 512
    M16 = QN // 2048
    scale = 128 ** -0.5
    with Phase(P):
        tmp_i = P.sb([128, 512], I32, "tmp_i")
        cst = attn_consts(P, tmp_i)
        mdiag, mge = cst["m_diag"], cst["m_ge"]
        m4 = {}
        am = P.sb([128, 128], BF16, "am4")
        for c4 in range(4):
            for mm in range(-1, 4):
                t = P.sb([128, 128], BF16, "m4_%d_%d" % (c4, mm + 1))
                k = "m4_%d_%d" % (c4, mm + 1)
                make_mask(P, t, k, 128, c4 - 128 * mm, -1, 4, tmp_i, "tmp_i")
                make_mask(P, am, "am4", 128, 128 - c4 + 128 * mm, 1, -4, tmp_i, "tmp_i")
                P.tt('dve', t[:], t[:], am[:], ALU.mult, reads=[k, 'am4'], writes=[k])
                m4[(c4, mm)] = (t, k)
        posq = P.sb([128, NC1], F32, "posq")
        posd4 = P.sb([128, 4 * M4], F32, "posd4")
        posd16 = P.sb([128, 16 * M16], F32, "posd16")
        P.dma('sp', posq[:], io['posq'][:, :], writes=['posq'])
        P.dma('sp', posd4[:], io['posd4'][:, :], writes=['posd4'])
        P.dma('sp', posd16[:], io['posd16'][:, :], writes=['posd16'])
        Ks = [P.sb([128, QN], BF16, "dk%d" % g) for g in range(3)]
        Qs = [P.sb([128, OWN], BF16, "dq%d" % g) for g in range(3)]
        v1 = P.sb([128, NC1, 129], BF16, "dv1")
        v4 = P.sb([128, 4 * M4, 129], BF16, "dv4")
        v16 = P.sb([128, 16 * M16, 129], BF16, "dv16")
        ps_s = [P.ps([128, 512], F32, "ps_s%d" % i) for i in range(3)]
        ps_a = [P.ps([128, 512], F32, "ps_a%d" % i) for i in range(2)]
        pst = P.ps([128, 1024], BF16, "pst")
        pt = [P.sb([128, 128], BF16, "pt%d" % i) for i in range(5)]
        raw = [P.sb([128, 129], F32, "raw%d" % i) for i in range(2)]
        ofin = [P.sb([128, 128], F32, "ofin%d" % i) for i in range(2)]
        obf = [P.sb([128, 128], BF16, "obf%d" % i) for i in range(2)]
        osb = [P.sb([128, 128], BF16, "osb%d" % i) for i in range(2)]
        rden = [P.sb([128, 1], F32, "rden%d" % i) for i in range(2)]
        cnt = {'s': 0, 'pt': 0, 'o': 0, 'a': 0}

        pend = []

        def emit_pv(p_, ptk, vap, vkey, acc, ak, first, last):
            P.mm(acc[:, 0:129], p_[:], vap, first, last, reads=[ptk, vkey], writes=[ak])

        def flush():
            while pend:
                emit_pv(*pend.pop(0))

        def visit(kap, kkey, qap, qkey, mask, mkey, vap, vkey, acc, ak, first, last):
            si = cnt['s'] % 3
            cnt['s'] += 1
            ps, pk = ps_s[si], "ps_s%d" % si
            P.mm(ps[:, 0:128], kap, qap, True, True, reads=[kkey, qkey], writes=[pk])
            pi = cnt['pt'] % 5
            cnt['pt'] += 1
            p_, ptk = pt[pi], "pt%d" % pi
            P.act(p_[:], ps[:, 0:128], AF.Exp, reads=[pk], writes=[ptk], scale=scale)
            P.tt('dve', p_[:], p_[:], mask[:], ALU.mult, reads=[ptk, mkey], writes=[ptk])
            if len(pend) == 2:
                emit_pv(*pend.pop(0))
            pend.append((p_, ptk, vap, vkey, acc, ak, first, last))

        for h in range(8):
            for g in range(3):
                P.dma('sp', Ks[g][:], io['Z1'][((g * 2 + 1) * 8 + h) * 128:((g * 2 + 1) * 8 + h + 1) * 128, :], writes=['dk%d' % g])
                P.dma('act', Qs[g][:], io['Z1'][((g * 2) * 8 + h) * 128:((g * 2) * 8 + h + 1) * 128, 2048:], writes=['dq%d' % g])
            vcol = lambda g: slice(g * 1024 + h * 128, g * 1024 + (h + 1) * 128)
            P.dma('sp', v1[:, :, 0:128], io['V1'][:, vcol(0)].rearrange("(c p) d -> p c d", p=128), writes=['dv1'])
            P.op('dve', lambda e: e.tensor_single_scalar(v1[:, :, 128:129], posq[:, :].rearrange("p (c o) -> p c o", o=1), 0.0, ALU.is_ge), ['posq'], ['dv1'])
            for rho in range(4):
                P.dma('act', v4[:, rho * M4:(rho + 1) * M4, 0:128],
                      io['V1'][:, vcol(1)].rearrange("(m p r) d -> r p m d", p=128, r=4)[rho], writes=['dv4'])
            P.op('dve', lambda e: e.tensor_single_scalar(v4[:, :, 128:129], posd4[:, :].rearrange("p (c o) -> p c o", o=1), 0.0, ALU.is_ge), ['posd4'], ['dv4'])
            for rho in range(16):
                P.dma('sp' if rho % 2 == 0 else 'act', v16[:, rho * M16:(rho + 1) * M16, 0:128],
                      io['V1'][:, vcol(2)].rearrange("(m p r) d -> r p m d", p=128, r=16)[rho], writes=['dv16'])
            P.op('dve', lambda e: e.tensor_single_scalar(v16[:, :, 128:129], posd16[:, :].rearrange("p (c o) -> p c o", o=1), 0.0, ALU.is_ge), ['posd16'], ['dv16'])
            K4v = Ks[1][:].rearrange("p (m k r) -> p r m k", k=128, r=4)
            K16v = Ks[2][:].rearrange("p (m k r) -> p r m k", k=128, r=16)
            Q4v = Qs[1][:].rearrange("p (u j r) -> p u r j", j=128, r=16)
            Q16v = Qs[2][:].rearrange("p (u j r) -> p u r j", j=128, r=16)
            rawd = io['RAW'][:, h, :].rearrange("(u j r) d -> u r j d", j=128, r=16)
            for u in range(OWN // 2048):
                for rho in range(16):
                    ai = cnt['a'] % 2
                    cnt['a'] += 1
                    acc, ak = ps_a[ai], "ps_a%d" % ai
                    rho4, c4 = rho % 4, rho // 4
                    visits = []
                    visits.append((K16v[:, rho, u, :], 'dk2', Q16v[:, u, rho, :], 'dq2', mge, 'm_ge', v16[:, rho * M16 + u, :], 'dv16'))
                    visits.append((K16v[:, rho, u + 1, :], 'dk2', Q16v[:, u, rho, :], 'dq2', mdiag, 'm_diag', v16[:, rho * M16 + u + 1, :], 'dv16'))
                    for mm in range(-1, 4):
                        m = 4 * (1 + u) + mm
                        mk, mkk = m4[(c4, mm)]
                        visits.append((K4v[:, rho4, m, :], 'dk1', Q4v[:, u, rho, :], 'dq1', mk, mkk, v4[:, rho4 * M4 + m, :], 'dv4'))
                    for vi, (kap, kk, qap, qk, mk, mkk, vap, vk) in enumerate(visits):
                        visit(kap, kk, qap, qk, mk, mkk, vap, vk, acc, ak, vi == 0, vi == len(visits) - 1)
                    flush()
                    ri_ = cnt['o'] % 2
                    cnt['o'] += 1
                    P.copy('dve', raw[ri_][:], acc[:, 0:129], reads=[ak], writes=['raw%d' % ri_])
                    P.dma('sp', rawd[u, rho], raw[ri_][:], reads=['raw%d' % ri_])
            P.fence()
            for i in range(OWN // 128):
                ai = cnt['a'] % 2
                cnt['a'] += 1
                acc, ak = ps_a[ai], "ps_a%d" % ai
                qap = Qs[0][:, i * 128:(i + 1) * 128]
                visit(Ks[0][:, (15 + i) * 128:(16 + i) * 128], 'dk0', qap, 'dq0', mge, 'm_ge', v1[:, 15 + i, :], 'dv1', acc, ak, True, False)
                visit(Ks[0][:, (16 + i) * 128:(17 + i) * 128], 'dk0', qap, 'dq0', mdiag, 'm_diag', v1[:, 16 + i, :], 'dv1', acc, ak, False, True)
                flush()
                ri_ = cnt['o'] % 2
                cnt['o'] += 1
                r_, rk_ = raw[ri_], 'raw%d' % ri_
                P.dma('act', r_[:], io['RAW'][i * 128:(i + 1) * 128, h, :], writes=[rk_])
                P.tt('dve', r_[:], r_[:], acc[:, 0:129], ALU.add, reads=[rk_, ak], writes=[rk_])
                rd, rdk = rden[ri_], "rden%d" % ri_
                P.ts('dve', rd[:], r_[:, 128:129], 1e-30, None, ALU.max, reads=[rk_], writes=[rdk])
                P.op('dve', lambda e, rd=rd: e.reciprocal(rd[:], rd[:]), [rdk], [rdk])
                P.ts('dve', ofin[ri_][:], r_[:, 0:128], rd[:, 0:1], None, ALU.mult, reads=[rk_, rdk], writes=['ofin%d' % ri_])
                emit_oT(P, cst, ofin[ri_][:], 'ofin%d' % ri_, obf[ri_], 'obf%d' % ri_, pst, osb[ri_], 'osb%d' % ri_,
                        io['OT1'][h * 128:(h + 1) * 128, i * 128:(i + 1) * 128])
            P.fence()


WNAMES = ["l0_norm_mix_pre", "l0_w_in", "l0_cmp_pe_k", "l0_cmp_w1_k", "l0_cmp_w2_k", "l0_cmp_pe_v", "l0_cmp_w1_v", "l0_cmp_w2_v",
          "l0_mla_q_norm", "l0_mla_w_uq", "l0_mla_kv_norm", "l0_mla_w_ukv", "l0_w_out", "l0_norm_mix_post", "l0_norm_ffn_pre",
          "l0_w_ff1", "l0_w_ff2", "l0_norm_ffn_post", "l1_norm_mix_pre", "l1_w_in", "l1_w_out", "l1_norm_mix_post",
          "l1_norm_ffn_pre", "l1_w_ff1", "l1_w_ff2", "l1_norm_ffn_post"]


def build_fused(L, wshapes, phases=(1, 2, 3, 4, 5, 6)):
    G = Geo(L)
    nc = bass.Bass("TRN2", target_bir_lowering=False)
    io = {}
    def ein(name, shape, dt=F32):
        io[name] = nc.dram_tensor(name, list(shape), dt, kind="ExternalInput").ap()
    def scr(name, shape, dt):
        io[name] = nc.dram_tensor(name, list(shape), dt).ap()
    ein('xT', [D, L]); ein('pos', [1, L]); ein('ridx', [128, 4]); ein('postm', [128, L // 128])
    ein('cpos', [128, ((L - 32) // 16 + 1 + 127) // 128]); ein('f0', [1, L // 64])
    ein('posq', [128, G.QN // 128]); ein('posd4', [128, 4 * (G.QN // 512)]); ein('posd16', [128, 16 * (G.QN // 2048)])
    for n in WNAMES:
        ein(n, wshapes[n])
    io['out'] = nc.dram_tensor("out", [D, G.OWN], F32, kind="ExternalOutput").ap()
    scr('QA', [1024, G.QN], BF16); scr('GATE', [G.QN, 24], F32); scr('KFM', [1024, L], BF16)
    scr('VS', [L, 256], BF16); scr('VW', [L, 256], BF16); scr('KPE', [64, L], BF16)
    scr('QMN', [1024, G.QN], BF16); scr('QMP', [512, G.QN], BF16); scr('KMN', [1024, L], BF16); scr('VM', [L, 1024], BF16)
    scr('OT', [2048, G.QN], BF16); scr('H2T', [D, G.QN], F32); scr('Z1', [6144, G.QN], BF16); scr('V1', [G.QN, 3072], BF16)
    scr('RAW', [G.OWN, 8, 129], F32); scr('OT1', [1024, G.OWN], BF16)
    scr('WB0', [108, 128, 4096], BF16); scr('WB1', [72, 128, 4096], BF16)
    with contextlib.ExitStack() as st:
        P = Prog(nc, st)
        if 1 in phases: phase1(P, nc, G, io)
        if 2 in phases: phase2(P, nc, G, io)
        if 3 in phases: phase3(P, nc, G, io)
        if 4 in phases: phase_post(P, nc, G, io, 0)
        if 5 in phases: phase5(P, nc, G, io)
        if 6 in phases: phase_post(P, nc, G, io, 1)
        P.emit()
    return nc, G


def fused_inputs(x, weights, L_total_S):
    B, S, _ = x.shape
    L = S
    G = Geo(L)
    maps = []
    for c in range(8):
        b, k = c // 4, c % 4
        end = (k + 1) * G.OWN
        shift = L - end
        xT = np.zeros((D, L), np.float32)
        xT[:, shift:] = x[b, :end].T
        pos = (np.arange(L) - shift).astype(np.float32)
        d = dict(xT=xT, pos=pos[None, :].copy(), ridx=ridx_np(),
                 postm=np.ascontiguousarray(pos.reshape(L // 128, 128).T))
        ncc = ((L - 32) // 16 + 1 + 127) // 128
        cp = np.full(ncc * 128, -1.0, np.float32)
        nvalid = min(ncc * 128, L // 16)
        cp[:nvalid] = pos[0:16 * nvalid:16]
        ncmp = (L - 32) // 16 + 1
        cp[ncmp:] = -1.0
        d['cpos'] = np.ascontiguousarray(cp.reshape(ncc, 128).T)
        f0 = np.zeros((1, L // 64), np.float32)
        f0[0, shift // 64] = 1.0
        d['f0'] = f0
        pq = pos[G.HAL0:]
        d['posq'] = np.ascontiguousarray(pq.reshape(G.QN // 128, 128).T)
        d['posd4'] = np.ascontiguousarray(pq.reshape(G.QN // 512, 128, 4).transpose(1, 2, 0).reshape(128, -1))
        d['posd16'] = np.ascontiguousarray(pq.reshape(G.QN // 2048, 128, 16).transpose(1, 2, 0).reshape(128, -1))
        for n in WNAMES:
            d[n] = weights[n]
        maps.append(d)
    return maps, G


def kernel(**inp):
    inp = {k: np.asarray(v) for k, v in inp.items()}
    x = inp["x"]
    B, S, _ = x.shape
    weights = {n: inp[n] for n in WNAMES}
    nc, G = build_fused(S, {n: weights[n].shape for n in WNAMES})
    maps, G = fused_inputs(x, weights, S)
    res = run_bass_kernel_spmd(nc, maps, core_ids=list(range(8)))
    del maps
    out = np.empty((B, S, D), np.float32)
    for c in range(8):
        b, k = c // 4, c % 4
        out[b, k * G.OWN:(k + 1) * G.OWN] = np.asarray(res.results[c]["out"]).T
    return out
```
